# Optimizing a Trainium2 kernel written in Bass

```python
import math
import jax, jax.numpy as jnp
from jax import lax
import numpy as np

D_MODEL = 1024
BATCH = 16
SEQ = 2048
DEPTH = 4

HEAD_DIM = 64
D_MIX = D_MODEL
DIFF_HEADS = (3 * D_MIX) // (8 * HEAD_DIM)
DIFF_DV = HEAD_DIM
DIFF_DK = HEAD_DIM // 2
DSA_HEADS = (3 * D_MIX) // (8 * HEAD_DIM)
DSA_DH = HEAD_DIM
DSA_LATENT = 2 * HEAD_DIM
N_IDX_HEADS = 8
D_IDX = 64
DSA_TOPK_MAX = 256
RET_HEADS = D_MIX // HEAD_DIM - DIFF_HEADS - DSA_HEADS
RET_DK = HEAD_DIM
RET_DV = HEAD_DIM
RET_CHUNK = 128
Q_BLOCK = 128
ROPE_BASE = 10000.0
NORM_EPS = 1e-6
NEG = -1e30
IDX_SCALE = (N_IDX_HEADS ** -0.5) * (D_IDX ** -0.5)

IN_SIZES = (
    DIFF_HEADS * 2 * DIFF_DK,
    DIFF_HEADS * 2 * DIFF_DK,
    DIFF_HEADS * DIFF_DV,
    DSA_HEADS * DSA_DH,
    DSA_LATENT,
    N_IDX_HEADS * D_IDX,
    D_IDX,
    N_IDX_HEADS,
    RET_HEADS * RET_DK,
    RET_HEADS * RET_DK,
    RET_HEADS * RET_DV,
    D_MIX,
)
IN_COLS = sum(IN_SIZES)

kernel_name = "hymba_style_diff_dsa_retention_trunk"


def rms_norm(x, g):
    xf = x.astype(jnp.float32)
    y = xf * lax.rsqrt(jnp.mean(xf * xf, axis=-1, keepdims=True) + NORM_EPS)
    return (y * g.astype(jnp.float32)).astype(x.dtype)


def to_blocks(a):
    B, S = a.shape[:2]
    return jnp.moveaxis(a.reshape((B, S // Q_BLOCK, Q_BLOCK) + a.shape[2:]), 1, 0)


def from_blocks(a):
    a = jnp.moveaxis(a, 0, 1)
    return a.reshape((a.shape[0], a.shape[1] * a.shape[2]) + a.shape[3:])


def rotary(x, cos, sin):
    x1, x2 = jnp.split(x.astype(jnp.float32), 2, axis=-1)
    c = cos[None, :, None, :]
    s = sin[None, :, None, :]
    return jnp.concatenate([x1 * c - x2 * s, x1 * s + x2 * c], axis=-1).astype(x.dtype)


def diff_attention(q, k, v, lam_vecs, norm_g, lam_init):
    S = q.shape[1]
    lf = lam_vecs.astype(jnp.float32)
    lam = jnp.exp(jnp.sum(lf[0] * lf[1])) - jnp.exp(jnp.sum(lf[2] * lf[3])) + lam_init
    scale = q.shape[-1] ** -0.5
    kpos = jnp.arange(S)

    def block(args):
        qb, start = args
        qpos = start + jnp.arange(Q_BLOCK)
        causal = kpos[None, :] <= qpos[:, None]
        s = jnp.einsum('bqhcd,bkhcd->bhcqk', qb, k).astype(jnp.float32) * scale
        p = jax.nn.softmax(jnp.where(causal, s, NEG), axis=-1)
        a = p[:, :, 0] - lam * p[:, :, 1]
        return jnp.einsum('bhqk,bkhd->bqhd', a.astype(v.dtype), v)

    starts = jnp.arange(S // Q_BLOCK) * Q_BLOCK
    o = from_blocks(lax.map(block, (to_blocks(q), starts)))
    return rms_norm(o, norm_g) * (1.0 - lam_init)


def dsa_attention(q, c_kv, iq, ik, iw, w_uk, w_uv, top_k):
    S = q.shape[1]
    scale = DSA_DH ** -0.5
    q_lat = jnp.einsum('bshd,hdr->bshr', q, w_uk)
    kpos = jnp.arange(S)

    def block(args):
        qlb, iqb, iwb, start = args
        qpos = start + jnp.arange(Q_BLOCK)
        causal = kpos[None, :] <= qpos[:, None]
        rel = jax.nn.relu(jnp.einsum('bqhd,bsd->bqhs', iqb, ik).astype(jnp.float32))
        score = jnp.einsum('bqhs,bqh->bqs', rel, iwb.astype(jnp.float32)) * IDX_SCALE
        score = jnp.where(causal[None], score, NEG)
        _, sel = lax.top_k(score, top_k)
        valid = sel <= qpos[None, :, None]
        c_sel = jax.vmap(lambda c, s: c[s])(c_kv, sel)
        s = jnp.einsum('bqhr,bqkr->bhqk', qlb, c_sel).astype(jnp.float32) * scale
        p = jax.nn.softmax(jnp.where(valid[:, None], s, NEG), axis=-1)
        o_lat = jnp.einsum('bhqk,bqkr->bqhr', p.astype(c_sel.dtype), c_sel)
        return jnp.einsum('bqhr,hrd->bqhd', o_lat, w_uv)

    starts = jnp.arange(S // Q_BLOCK) * Q_BLOCK
    o = lax.map(block, (to_blocks(q_lat), to_blocks(iq), to_blocks(iw), starts))
    return from_blocks(o)


def retention(q, k, v, norm_g):
    B, S, H, DK = q.shape
    DV = v.shape[-1]
    C = RET_CHUNK
    log_g = jnp.log(1.0 - 2.0 ** (-5.0 - jnp.arange(H, dtype=jnp.float32)))
    pos = jnp.arange(C, dtype=jnp.float32)
    diff = pos[:, None] - pos[None, :]
    d_intra = jnp.where(diff >= 0, jnp.exp(jnp.maximum(diff, 0.0)[None] * log_g[:, None, None]), 0.0)
    xi = jnp.exp((pos + 1.0)[:, None] * log_g[None, :])
    zeta = jnp.exp((C - 1.0 - pos)[:, None] * log_g[None, :])
    g_chunk = jnp.exp(C * log_g)

    def chunks(a):
        return jnp.moveaxis(a.astype(jnp.float32).reshape((B, S // C, C) + a.shape[2:]), 1, 0)

    qc_all = chunks(q)
    kc_all = chunks(k) * (DK ** -0.5)
    vc_all = chunks(v)

    def step(R, inp):
        qc, kc, vc = inp
        att = jnp.einsum('bqhd,bkhd->bhqk', qc, kc) * d_intra[None]
        inner = jnp.einsum('bhqk,bkhe->bqhe', att, vc)
        cross = jnp.einsum('bqhd,bhde->bqhe', qc, R) * xi[None, :, :, None]
        R_new = g_chunk[None, :, None, None] * R + jnp.einsum('bkhd,bkhe->bhde', kc * zeta[None, :, :, None], vc)
        return R_new, inner + cross

    R0 = jnp.zeros((B, H, DK, DV), jnp.float32)
    _, o = lax.scan(step, R0, (qc_all, kc_all, vc_all))
    o = jnp.moveaxis(o, 0, 1).reshape(B, S, H, DV).astype(v.dtype)
    return rms_norm(o, norm_g)


def setup_inputs(seed: int = 0) -> dict:
    key = jax.random.key(seed)
    ks = jax.random.split(key, 11)
    f32 = jnp.float32
    x = jax.random.normal(ks[0], (BATCH, SEQ, D_MODEL), f32)
    attn_norm = 1.0 + 0.02 * jax.random.normal(ks[1], (DEPTH, D_MODEL), f32)
    w_in = jax.random.normal(ks[2], (DEPTH, D_MODEL, IN_COLS), f32) * D_MODEL ** -0.5
    diff_lambda = 0.1 * jax.random.normal(ks[3], (DEPTH, 4, DIFF_DK), f32)
    diff_norm = 1.0 + 0.02 * jax.random.normal(ks[4], (DEPTH, DIFF_DV), f32)
    kv_norm = 1.0 + 0.02 * jax.random.normal(ks[5], (DEPTH, DSA_LATENT), f32)
    w_uk = jax.random.normal(ks[6], (DEPTH, DSA_HEADS, DSA_DH, DSA_LATENT), f32) * DSA_LATENT ** -0.5
    w_uv = jax.random.normal(ks[7], (DEPTH, DSA_HEADS, DSA_LATENT, DSA_DH), f32) * DSA_LATENT ** -0.5
    ret_norm = 1.0 + 0.02 * jax.random.normal(ks[8], (DEPTH, RET_DV), f32)
    w_out = jax.random.normal(ks[9], (DEPTH, D_MIX, D_MODEL), f32) * (D_MIX ** -0.5) * (2 * DEPTH) ** -0.5
    final_norm = 1.0 + 0.02 * jax.random.normal(ks[10], (D_MODEL,), f32)
    return {"x": x, "attn_norm": attn_norm, "w_in": w_in, "diff_lambda": diff_lambda,
            "diff_norm": diff_norm, "kv_norm": kv_norm, "w_uk": w_uk, "w_uv": w_uv,
            "ret_norm": ret_norm, "w_out": w_out, "final_norm": final_norm}


def reference(x, attn_norm, w_in, diff_lambda, diff_norm, kv_norm, w_uk, w_uv, ret_norm, w_out, final_norm):
    B, S, _ = x.shape
    top_k = min(DSA_TOPK_MAX, S // 4)
    split_points = np.cumsum(np.array(IN_SIZES))[:-1].tolist()
    inv_freq = ROPE_BASE ** (-jnp.arange(RET_DK // 2, dtype=jnp.float32) / (RET_DK // 2))
    ang = jnp.arange(S, dtype=jnp.float32)[:, None] * inv_freq[None, :]
    cos, sin = jnp.cos(ang), jnp.sin(ang)

    h = x
    for layer in range(DEPTH):
        u = rms_norm(h, attn_norm[layer])
        proj = jnp.einsum('bsd,dc->bsc', u, w_in[layer])
        (dq, dk, dv, sq, ckv, iq, ik, iw, rq, rk, rv, gate) = jnp.split(proj, split_points, axis=-1)

        lam_init = 0.8 - 0.6 * math.exp(-0.3 * layer)
        o_a = diff_attention(dq.reshape(B, S, DIFF_HEADS, 2, DIFF_DK),
                             dk.reshape(B, S, DIFF_HEADS, 2, DIFF_DK),
                             dv.reshape(B, S, DIFF_HEADS, DIFF_DV),
                             diff_lambda[layer], diff_norm[layer], lam_init)

        o_b = dsa_attention(sq.reshape(B, S, DSA_HEADS, DSA_DH),
                            rms_norm(ckv, kv_norm[layer]),
                            iq.reshape(B, S, N_IDX_HEADS, D_IDX), ik, iw,
                            w_uk[layer], w_uv[layer], top_k)

        o_c = retention(rotary(rq.reshape(B, S, RET_HEADS, RET_DK), cos, sin),
                        rotary(rk.reshape(B, S, RET_HEADS, RET_DK), cos, sin),
                        rv.reshape(B, S, RET_HEADS, RET_DV), ret_norm[layer])

        mixed = jnp.concatenate([o_a.reshape(B, S, -1), o_b.reshape(B, S, -1), o_c.reshape(B, S, -1)], axis=-1)
        y = jax.nn.silu(gate) * mixed
        h = h + jnp.einsum('bsc,cd->bsd', y, w_out[layer])

    return rms_norm(h, final_norm)
```

```python
import math
import numpy as np
from contextlib import ExitStack
import concourse.bass as bass
import concourse.mybir as mybir
from concourse.bass_utils import run_bass_kernel_spmd

F32 = mybir.dt.float32
BF16 = mybir.dt.bfloat16
ALU = mybir.AluOpType
AF = mybir.ActivationFunctionType
AX = mybir.AxisListType

S = 2048
D = 1024
NB = S // 128
DEPTH = 4
NCORES = 8
EPS = 1e-6
NBIS = 16
NEGM = -30000.0
import os
_SKIP = os.environ.get('KSKIP', '')


class Prog:
    ENG = ("pe", "act", "dve", "pool", "sp")
    NDS = 12

    def __init__(self, nc, stack):
        self.nc = nc
        self.q = {e: [] for e in self.ENG}
        self.cnt = {e: 0 for e in self.ENG}
        self.sem = {e: stack.enter_context(nc.semaphore("prog_" + e)) for e in self.ENG}
        self.seen = {e: {f: 0 for f in self.ENG} for e in self.ENG}
        self.dseen = {e: {} for e in self.ENG}
        self.lastw = {}
        self.readers = {}
        self.dsem = {}
        self.drot = {}
        for qn in ("sp", "pool"):
            for i in range(self.NDS):
                nm = "%s%d" % (qn, i)
                self.dsem[nm] = [stack.enter_context(nc.semaphore("d_" + nm)), 0]
            self.drot[qn] = 0

    def _need(self, eng, dep, waits):
        if dep is None:
            return
        if dep[0] == "e":
            _, f, idx = dep
            if self.seen[eng][f] < idx:
                self.seen[eng][f] = idx
                waits[("e", f)] = (self.sem[f], idx)
        else:
            _, name, val = dep
            if self.dseen[eng].get(name, 0) < val:
                self.dseen[eng][name] = val
                waits[("d", name)] = (self.dsem[name][0], val)

    def _deps(self, eng, reads, writes):
        waits = {}
        for k in reads:
            self._need(eng, self.lastw.get(k), waits)
        for k in writes:
            lw = self.lastw.get(k)
            if lw is not None and not (lw[0] == "e" and lw[1] == eng):
                self._need(eng, lw, waits)
            for dep in self.readers.get(k, {}).values():
                if not (dep[0] == "e" and dep[1] == eng):
                    self._need(eng, dep, waits)
        return list(waits.values())

    def op(self, eng, fn, reads=(), writes=()):
        writes = list(writes) + [k for k in reads if isinstance(k, tuple) and k[0] == "ps" and k not in writes]
        waits = self._deps(eng, reads, writes)
        self.cnt[eng] += 1
        idx = self.cnt[eng]
        self.q[eng].append((waits, fn, (self.sem[eng], 1)))
        dep = ("e", eng, idx)
        for k in reads:
            self.readers.setdefault(k, {})[eng] = dep
        for k in writes:
            self.lastw[k] = dep
            self.readers[k] = {}
        return dep

    def dma(self, queue, fn, reads=(), writes=()):
        name = "%s%d" % (queue, self.drot[queue])
        self.drot[queue] = (self.drot[queue] + 1) % self.NDS
        waits = {}
        self._need(queue, ("d", name, self.dsem[name][1]), waits) if self.dsem[name][1] else None
        w2 = self._deps(queue, reads, writes)
        allw = list(waits.values()) + w2
        self.dsem[name][1] += 16
        val = self.dsem[name][1]
        self.q[queue].append((allw, fn, (self.dsem[name][0], 16)))
        dep = ("d", name, val)
        for k in writes:
            self.lastw[k] = dep
            self.readers[k] = {}
        for k in reads:
            self.readers.setdefault(k, {})["dma:" + name] = dep
        return dep

    def barrier(self):
        for e in self.ENG:
            waits = {}
            for f in self.ENG:
                if f != e and self.cnt[f]:
                    self._need(e, ("e", f, self.cnt[f]), waits)
            for name, (h, val) in self.dsem.items():
                if val:
                    self._need(e, ("d", name, val), waits)
            self.q[e].append((list(waits.values()), None, None))

    def emit(self):
        nc = self.nc
        q = self.q
        with nc.Block() as block:
            def replay(name):
                def run(e):
                    for waits, fn, inc in q[name]:
                        for s, v in waits:
                            e.wait_ge(s, v)
                        if fn is not None:
                            fn(e).then_inc(inc[0], inc[1])
                return run
            block.sync(replay("sp"))
            block.tensor(replay("pe"))
            block.scalar(replay("act"))
            block.vector(replay("dve"))
            block.gpsimd(replay("pool"))


class Rot:
    def __init__(self, items):
        self.items = list(items)
        self.i = 0

    def next(self):
        v = self.items[self.i % len(self.items)]
        self.i += 1
        return v


def _col_maps():
    o_dq, o_dk, o_dv, o_sq, o_ckv, o_iq, o_ik, o_iw, o_rq, o_rk, o_rv, o_gate = (
        0, 384, 768, 1152, 1536, 1664, 2176, 2240, 2248, 2504, 2760, 3016)
    colsF = []
    for base in (o_dq, o_dk, o_sq):
        for c in range(3):
            colsF.append(np.arange(base + 128 * c, base + 128 * c + 128))
    for c in range(4):
        colsF.append(np.arange(o_iq + 128 * c, o_iq + 128 * c + 128))
    colsF.append(np.concatenate([np.arange(o_ik, o_ik + 64)] * 2))

    def swap(base, c):
        out = []
        for hl in range(2):
            b = base + 128 * c + 64 * hl
            out += [np.arange(b + 32, b + 64), np.arange(b, b + 32)]
        return np.concatenate(out)
    for base in (o_rq, o_rk):
        for c in range(2):
            colsF.append(np.arange(base + 128 * c, base + 128 * c + 128))
        for c in range(2):
            colsF.append(swap(base, c))
    colsF = np.concatenate(colsF)
    colsT = np.concatenate([np.arange(o_dv, o_dv + 384), np.arange(o_ckv, o_ckv + 128),
                            np.arange(o_iw, o_iw + 8), np.arange(o_rv, o_rv + 256),
                            np.arange(o_gate, o_gate + 1024)])
    return colsF, colsT


NF = 22
T_DV = (0, 384)
T_CKV = (384, 520)
T_RV = (520, 776)
T_GATE = (776, 1800)
NT = 1800


def _host_consts():
    f32 = np.float32
    c = {}
    c["ident"] = np.eye(128, dtype=f32)
    r = np.arange(128)
    c["tri"] = (r[None, :] >= r[:, None]).astype(f32)
    c["cneg"] = np.where(r[None, :] <= r[:, None], 0.0, -1e30).astype(f32)
    inv_freq = (f32(10000.0) ** (-(np.arange(32, dtype=f32)) / f32(32))).astype(f32)
    ang = (np.arange(S, dtype=f32)[:, None] * inv_freq[None, :]).astype(f32)
    cos, sin = np.cos(ang).astype(f32), np.sin(ang).astype(f32)
    rr = np.arange(128)
    cosT = cos[:, rr % 32].T.copy()
    sgn = np.where((rr % 64) < 32, -1.0, 1.0).astype(f32)
    sinT = (sin[:, rr % 32].T * sgn[:, None]).astype(f32)
    c["cosT"], c["sinT"] = np.ascontiguousarray(cosT), np.ascontiguousarray(sinT)
    H = 4
    log_g = np.log(f32(1.0) - f32(2.0) ** (f32(-5.0) - np.arange(H, dtype=f32))).astype(f32)
    pos = np.arange(128, dtype=f32)
    diff = pos[:, None] - pos[None, :]
    d_intra = np.where(diff >= 0, np.exp(np.maximum(diff, 0.0)[None] * log_g[:, None, None]), 0.0).astype(f32)
    xi = np.exp((pos + 1.0)[:, None] * log_g[None, :]).astype(f32)
    zeta = np.exp((127.0 - pos)[:, None] * log_g[None, :]).astype(f32)
    gch = np.exp(128.0 * log_g).astype(f32)
    sc = f32(64.0 ** -0.5)
    c["dintraT"] = np.ascontiguousarray(np.transpose(d_intra, (2, 0, 1)) * sc).astype(f32)
    xiT = np.zeros((128, 2, 128), f32)
    zt = np.zeros((128, 2, 128), f32)
    gc = np.zeros((128, 2), f32)
    for cc in range(2):
        for hl in range(2):
            h = 2 * cc + hl
            xiT[hl * 64:(hl + 1) * 64, cc, :] = xi[None, :, h]
            zt[:, cc, hl * 64:(hl + 1) * 64] = (zeta[:, h] * sc)[:, None]
            gc[hl * 64:(hl + 1) * 64, cc] = gch[h]
    c["xiT"], c["zeta"], c["gch"] = xiT, zt, gc
    return c


_CONST_SHAPES = {"ident": [128, 128], "tri": [128, 128], "cneg": [128, 128], "cosT": [128, S], "sinT": [128, S],
                 "dintraT": [128, 4, 128], "xiT": [128, 2, 128], "zeta": [128, 2, 128], "gch": [128, 2]}


def _host_weights(attn_norm, w_in, diff_lambda, diff_norm, kv_norm, w_uk, w_uv, ret_norm, w_out, final_norm):
    L = w_in.shape[0]
    colsF, colsT = _col_maps()
    w = {}
    wf = w_in[:, :, colsF].reshape(L, 8, 128, NF, 128)
    w["wF"] = np.ascontiguousarray(np.transpose(wf, (0, 3, 2, 1, 4)))
    wt = w_in[:, :, colsT].reshape(L, 8, 128, NT)
    w["wT"] = np.ascontiguousarray(np.transpose(wt, (0, 2, 1, 3)))
    w["wout"] = np.ascontiguousarray(np.transpose(w_out.reshape(L, 8, 128, D), (0, 2, 1, 3)))
    uk = w_uk.reshape(L, 3, 2, 64, 128)
    w["wuk"] = np.ascontiguousarray(np.transpose(uk, (0, 2, 3, 1, 4)).reshape(L, 128, 3, 128))
    w["wuv"] = np.ascontiguousarray(np.transpose(w_uv, (0, 2, 1, 3)).reshape(L, 128, 384))
    w["gattn"] = np.ascontiguousarray(np.transpose(attn_norm.reshape(L, 8, 128), (0, 2, 1)))
    w["gkv"] = np.ascontiguousarray(kv_norm.reshape(L, 128, 1))
    w["gdiff"] = np.ascontiguousarray(diff_norm)
    w["gret"] = np.ascontiguousarray(ret_norm)
    w["gfin"] = np.ascontiguousarray(final_norm)
    w["dlam"] = np.ascontiguousarray(diff_lambda.reshape(L, 128))
    return w


def build_program(NL=DEPTH, NS=2, debug=False):
    nc = bass.Bass("TRN2", target_bir_lowering=False)
    L = NL
    dt = lambda name, shape, dtype=F32, kind="ExternalInput": nc.dram_tensor(name, shape, dtype, kind=kind).ap()
    x_d = dt("x", [NS, S, D])
    wF_d = dt("wF", [L, NF, 128, 8, 128])
    wT_d = dt("wT", [L, 128, 8, NT])
    wout_d = dt("wout", [L, 128, 8, D])
    wuk_d = dt("wuk", [L, 128, 3, 128])
    wuv_d = dt("wuv", [L, 128, 384])
    gattn_d = dt("gattn", [L, 128, 8])
    gkv_d = dt("gkv", [L, 128, 1])
    gdiff_d = dt("gdiff", [L, 64])
    gret_d = dt("gret", [L, 64])
    gfin_d = dt("gfin", [D])
    dlam_d = dt("dlam", [L, 128])
    cd = {k: dt("c_" + k, shp) for k, shp in _CONST_SHAPES.items()}
    out_d = dt("out", [NS, S, D], F32, "ExternalOutput")
    hbuf_d = dt("hbuf", [NS, S, D], F32, "Internal")
    mixed_d = dt("mixed", [NS, S, D], BF16, "Internal")
    dbg_d = dt("dbg", [NS, S, D], F32, "ExternalOutput") if debug else None

    with ExitStack() as st:
        P = Prog(nc, st)
        _uid = [0]

        def sbuf(stack, name, shape, dtype=F32):
            _uid[0] += 1
            return stack.enter_context(nc.sbuf_tensor("s%d_%s" % (_uid[0], name), shape, dtype))
        ps = [st.enter_context(nc.psum_tensor("ps%d" % i, [128, 512], F32)) for i in range(8)]
        pk = lambda b: ("ps", b)

        def mm(out, lhsT, rhs, start, stop, reads, writes, **kw):
            P.op("pe", lambda e: e.matmul(out, lhsT=lhsT, rhs=rhs, start=start, stop=stop,
                                          skip_group_check=True, **kw), reads, writes)

        def tr(out, in_, reads, writes):
            P.op("pe", lambda e: e.transpose(out, in_, ident_b[:, :]), list(reads) + ["ident_b"], writes)

        def act(out, in_, func, reads, writes, **kw):
            P.op("act", lambda e: e.activation(out=out, in_=in_, func=func, **kw), reads, writes)

        def ts(eng, out, in0, s1, s2, op0, op1, reads, writes, **kw):
            if op1 is None:
                P.op(eng, lambda e: e.tensor_scalar(out, in0, s1, None, op0=op0, **kw), reads, writes)
            else:
                P.op(eng, lambda e: e.tensor_scalar(out, in0, s1, s2, op0=op0, op1=op1, **kw), reads, writes)

        def tt(eng, out, in0, in1, op, reads, writes):
            P.op(eng, lambda e: e.tensor_tensor(out, in0, in1, op=op), reads, writes)

        def stt(eng, out, in0, scalar, in1, op0, op1, reads, writes):
            P.op(eng, lambda e: e.scalar_tensor_tensor(out, in0, scalar, in1, op0=op0, op1=op1), reads, writes)

        def cp(eng, out, in_, reads, writes):
            if eng == "act":
                P.op("act", lambda e: e.copy(out, in_), reads, writes)
            else:
                P.op(eng, lambda e: e.tensor_copy(out, in_), reads, writes)

        def recip(out, in_, reads, writes):
            P.op("dve", lambda e: e.reciprocal(out, in_), reads, writes)

        def reduce(out, in_, op, reads, writes):
            P.op("dve", lambda e: e.tensor_reduce(out, in_, axis=AX.X, op=op), reads, writes)

        def memset(eng, ap, val, writes):
            P.op(eng, lambda e: e.memset(ap, val), [], writes)

        def dma(queue, out, in_, reads, writes):
            return P.dma(queue, lambda e: e.dma_start(out=out, in_=in_), reads, writes)

        def rsqrt_mean(eng_unused, out, ss, n, key_out, key_ss):
            ts("dve", out, ss, 1.0 / n, EPS, ALU.mult, ALU.add, [key_ss], [key_out])
            act(out, out, AF.Sqrt, [key_out], [key_out])
            P.op("dve", lambda e: e.reciprocal(out, out), [key_out], [key_out])

        ident_b = sbuf(st, "ident_b", [128, 128], BF16)
        tri_b = sbuf(st, "tri_b", [128, 128], BF16)
        cneg = sbuf(st, "cneg", [128, 128])
        dintraT = sbuf(st, "dintraT", [128, 4, 128])
        xiT = sbuf(st, "xiT", [128, 2, 128])
        zeta = sbuf(st, "zeta", [128, 2, 128])
        gch = sbuf(st, "gch", [128, 2])
        gfin = sbuf(st, "gfin", [128, D])
        gattn = sbuf(st, "gattn", [128, L, 8])
        gkv = sbuf(st, "gkv", [128, L, 1])
        gdm = sbuf(st, "gdm", [128, L, 64])
        gret = sbuf(st, "gret", [128, L, 64])
        dlam = sbuf(st, "dlam", [128, L, 128])
        lamneg = sbuf(st, "lamneg", [128, L, 1])
        lamt = sbuf(st, "lamt", [128, 4])
        wuk_b = sbuf(st, "wuk_b", [128, L, 3, 128], BF16)
        wuv_b = sbuf(st, "wuv_b", [128, L, 384], BF16)
        uT = sbuf(st, "uT", [128, 8, S], BF16)

        dma("pool", ident_b[:, :], cd["ident"][:, :], [], ["ident_b"])
        dma("pool", tri_b[:, :], cd["tri"][:, :], [], ["tri_b"])
        for nm, t in (("cneg", cneg), ("gch", gch)):
            dma("sp", t[:, :], cd[nm][:, :], [], [nm])
        for nm, t in (("dintraT", dintraT), ("xiT", xiT), ("zeta", zeta)):
            dma("sp", t[:, :, :], cd[nm][:, :, :], [], [nm])
        dma("sp", gfin[:, :], gfin_d.partition_broadcast(128), [], ["gfin"])
        for l in range(L):
            dma("sp", gattn[:, l, :], gattn_d[l, :, :], [], ["gattn"])
            dma("sp", gkv[:, l, :], gkv_d[l, :, :], [], ["gkv"])
            dma("sp", gdm[:, l, :], gdiff_d[l, :].partition_broadcast(128), [], ["gdm"])
            dma("sp", gret[:, l, :], gret_d[l, :].partition_broadcast(128), [], ["gret"])
            dma("sp", dlam[:, l, :], dlam_d[l, :].partition_broadcast(128), [], ["dlam"])
            dma("pool", wuk_b[:, l, :, :], wuk_d[l, :, :, :], [], ["wuk_b"])
            dma("pool", wuv_b[:, l, :], wuv_d[l, :, :], [], ["wuv_b"])
        for l in range(L):
            lam_init = 0.8 - 0.6 * math.exp(-0.3 * l)
            junk = dlam[:, l, 0:32]
            tt("dve", dlam[:, l, 0:32], dlam[:, l, 0:32], dlam[:, l, 32:64], ALU.mult, ["dlam"], ["dlam"])
            tt("dve", dlam[:, l, 64:96], dlam[:, l, 64:96], dlam[:, l, 96:128], ALU.mult, ["dlam"], ["dlam"])
            reduce(lamt[:, 0:1], dlam[:, l, 0:32], ALU.add, ["dlam"], ["lamt"])
            reduce(lamt[:, 1:2], dlam[:, l, 64:96], ALU.add, ["dlam", "lamt"], ["lamt"])
            act(lamt[:, 2:4], lamt[:, 0:2], AF.Exp, ["lamt"], ["lamt"])
            tt("dve", lamt[:, 0:1], lamt[:, 3:4], lamt[:, 2:3], ALU.subtract, ["lamt"], ["lamt"])
            ts("dve", lamneg[:, l, :], lamt[:, 0:1], -lam_init, None, ALU.add, None, ["lamt"], ["lamneg"])
            ts("dve", gdm[:, l, :], gdm[:, l, :], 1.0 - lam_init, None, ALU.mult, None, ["gdm"], ["gdm"])

        wrotF = Rot(range(3))
        wrotT = Rot(range(2))
        evrot = Rot(["act", "dve"])

        def h_src(l, s, t):
            src = x_d if l == 0 else hbuf_d
            return src[s, t * 128:(t + 1) * 128, :]

        def hkey(s, t):
            return ("h", s, t)

        for l in range(L):
            last_layer = (l == L - 1)
            for s in range(NS):
                with ExitStack() as ph:
                    hb = [sbuf(ph, "p0_h%d" % i, [128, D]) for i in range(2)]
                    hn = [sbuf(ph, "p0_hn%d" % i, [128, D], BF16) for i in range(2)]
                    sq_junk = sbuf(ph, "p0_junk", [128, D], BF16)
                    st0 = [sbuf(ph, "p0_st%d" % i, [128, 2]) for i in range(2)]
                    brot = Rot(range(8))
                    for t in range(NB):
                        i = t % 2
                        dma("sp", hb[i][:, :], h_src(l, s, t), [hkey(s, t)], [("hb", i)])
                        act(sq_junk[:, :], hb[i][:, :], AF.Square, [("hb", i)], ["sqj", ("st0", i)], accum_out=st0[i][:, 0:1])
                        rsqrt_mean("dve", st0[i][:, 1:2], st0[i][:, 0:1], D, ("st0", i), ("st0", i))
                        ts("pool", hn[i][:, :], hb[i][:, :], st0[i][:, 1:2], None, ALU.mult, None, [("hb", i), ("st0", i)], [("hn", i)])
                        b = brot.next()
                        pv = ps[b][:, :].bitcast(BF16)
                        for k in range(8):
                            tr(pv[:, k * 128:(k + 1) * 128], hn[i][:, k * 128:(k + 1) * 128], [("hn", i)], [pk(b)])
                        for k in range(8):
                            ev = evrot.next()
                            if ev == "act":
                                act(uT[:, k, t * 128:(t + 1) * 128], pv[:, k * 128:(k + 1) * 128], AF.Copy, [pk(b), "gattn"], ["uT"], scale=gattn[:, l, k:k + 1])
                            else:
                                ts("dve", uT[:, k, t * 128:(t + 1) * 128], pv[:, k * 128:(k + 1) * 128], gattn[:, l, k:k + 1], None, ALU.mult, None, [pk(b), "gattn"], ["uT"])
                P.barrier()

                def load_wF(wtiles, f):
                    i = wrotF.next()
                    dma("pool", wtiles[i][:, :, :], wF_d[l, f, :, :, :], [], [("wF", i)])
                    return i

                def proj_F(wtiles, f, evac):
                    i = load_wF(wtiles, f)
                    for tg in range(4):
                        b = brotP.next()
                        for k in range(8):
                            mm(ps[b][:, :], wtiles[i][:, k, :], uT[:, k, tg * 512:(tg + 1) * 512], k == 0, k == 7,
                               [("wF", i), "uT"], [pk(b)])
                        evac(tg, ps[b][:, :], b)

                def proj_T(wt, wkey, ncols, t, c0=0):
                    b = brotP.next()
                    for k in range(8):
                        mm(ps[b][:, 0:ncols], uT[:, k, t * 128:(t + 1) * 128], wt[:, k, c0:c0 + ncols], k == 0, k == 7,
                           [wkey, "uT"], [pk(b)])
                    return b

                def evac_copy(dst3, c):
                    def f(tg, pa, b):
                        ev = evrot.next()
                        cp(ev, dst3[:, c, tg * 512:(tg + 1) * 512], pa, [pk(b)], [dst3.name if hasattr(dst3, "name") else "x"])
                    return f

                with ExitStack() as ph:
                  if "A" in _SKIP:
                    pass
                  else:
                      brotP = Rot(range(8))
                      wFt = [sbuf(ph, "a_wF%d" % i, [128, 8, 128], BF16) for i in range(3)]
                      wTt = sbuf(ph, "a_wT", [128, 8, 384], BF16)
                      dqT = sbuf(ph, "a_dqT", [128, 3, S], BF16)
                      dkT = sbuf(ph, "a_dkT", [128, 3, S], BF16)
                      dva = sbuf(ph, "a_dva", [128, NB, 6, 65], BF16)
                      pt = [sbuf(ph, "a_pt%d" % i, [128, 512], BF16) for i in range(4)]
                      rr = [sbuf(ph, "a_rr%d" % i, [128, 4, 1]) for i in range(3)]
                      tA = sbuf(ph, "a_tA", [128, 4, 64])
                      tB = sbuf(ph, "a_tB", [128, 4, 64])
                      tO = sbuf(ph, "a_tO", [128, 4, 64])
                      tS = sbuf(ph, "a_tS", [128, 4, 64])
                      ss4 = sbuf(ph, "a_ss4", [128, 4, 1])
                      oa = [sbuf(ph, "a_oa%d" % i, [128, 4, 384], BF16) for i in range(2)]

                      dma("pool", wTt[:, :, :], wT_d[l, :, :, T_DV[0]:T_DV[1]], [], ["a_wT"])
                      memset("pool", dva[:, :, :, 64:65], 1.0, ["dva"])
                      for c in range(3):
                          def ev_q(tg, pa, b, c=c):
                              cp(evrot.next(), dqT[:, c, tg * 512:(tg + 1) * 512], pa, [pk(b)], ["dqT"])
                          proj_F(wFt, c, ev_q)
                      for c in range(3):
                          def ev_k(tg, pa, b, c=c):
                              cp(evrot.next(), dkT[:, c, tg * 512:(tg + 1) * 512], pa, [pk(b)], ["dkT"])
                          proj_F(wFt, 3 + c, ev_k)
                      for t in range(NB):
                          b = proj_T(wTt, "a_wT", 384, t)
                          cp(evrot.next(), dva[:, t, :, 0:64], ps[b][:, 0:384].rearrange("p (h e) -> p h e", e=64), [pk(b)], ["dva"])

                      scrot = Rot([0, 1, 2, 3])
                      accrot = Rot([(4, 5), (6, 7)])
                      ptrot = Rot(range(4))
                      scale = 32.0 ** -0.5
                      for I in range(4):
                          oab = oa[I % 2]
                          oak = ("oa", I % 2)
                          for h in range(6):
                              banks = accrot.next()
                              for c2 in range(2):
                                  ab = banks[c2]
                                  r0 = (h % 2) * 64 + c2 * 32
                                  kw = dict(tile_position=(96, 0)) if r0 == 96 else {}
                                  first = True
                                  for j in range(4 * I + 4):
                                      i0 = max(j, 4 * I)
                                      w = (4 * I + 4 - i0) * 128
                                      sb_ = scrot.next()
                                      mm(ps[sb_][:, 0:w], dkT[r0:r0 + 32, h // 2, j * 128:(j + 1) * 128],
                                         dqT[r0:r0 + 32, h // 2, i0 * 128:(4 * I + 4) * 128], True, True,
                                         ["dkT", "dqT"], [pk(sb_)], **kw)
                                      pi = ptrot.next()
                                      act(pt[pi][:, 0:w], ps[sb_][:, 0:w], AF.Exp, [pk(sb_)], [("pt", pi)], scale=scale)
                                      if j >= 4 * I:
                                          tt("pool", pt[pi][:, 0:128], pt[pi][:, 0:128], tri_b[:, :], ALU.mult, [("pt", pi), "tri_b"], [("pt", pi)])
                                      for i in range(i0, 4 * I + 4):
                                          mm(ps[ab][:, (i - 4 * I) * 65:(i - 4 * I) * 65 + 65], pt[pi][:, (i - i0) * 128:(i - i0 + 1) * 128],
                                             dva[:, j, h, :], first, j == i, [("pt", pi), "dva"], [pk(ab)])
                                          first = False
                              a0 = ps[banks[0]][:, 0:260].rearrange("p (i e) -> p i e", e=65)
                              a1 = ps[banks[1]][:, 0:260].rearrange("p (i e) -> p i e", e=65)
                              recip(rr[0][:, :, :], a0[:, :, 64:65], [pk(banks[0])], ["rr0"])
                              recip(rr[1][:, :, :], a1[:, :, 64:65], [pk(banks[1])], ["rr1"])
                              ts("dve", rr[2][:, :, :], rr[1][:, :, :], lamneg[:, l, :], None, ALU.mult, None, ["rr1", "lamneg"], ["rr2"])
                              tt("dve", tA[:, :, :], a0[:, :, 0:64], rr[0][:, :, :].to_broadcast([128, 4, 64]), ALU.mult, [pk(banks[0]), "rr0"], ["tA"])
                              tt("dve", tB[:, :, :], a1[:, :, 0:64], rr[2][:, :, :].to_broadcast([128, 4, 64]), ALU.mult, [pk(banks[1]), "rr2"], ["tB"])
                              tt("pool", tO[:, :, :], tA[:, :, :], tB[:, :, :], ALU.add, ["tA", "tB"], ["tO"])
                              tt("pool", tS[:, :, :], tO[:, :, :], tO[:, :, :], ALU.mult, ["tO"], ["tS"])
                              reduce(ss4[:, :, :], tS[:, :, :], ALU.add, ["tS"], ["ss4"])
                              rsqrt_mean("dve", ss4[:, :, :], ss4[:, :, :], 64, "ss4", "ss4")
                              tt("pool", tO[:, :, :], tO[:, :, :], ss4[:, :, :].to_broadcast([128, 4, 64]), ALU.mult, ["tO", "ss4"], ["tO"])
                              tt("pool", oab[:, :, h * 64:(h + 1) * 64], tO[:, :, :], gdm[:, l:l + 1, :].to_broadcast([128, 4, 64]), ALU.mult, ["tO", "gdm"], [oak])
                          for i4 in range(4):
                              t = 4 * I + i4
                              dma("sp", mixed_d[s, t * 128:(t + 1) * 128, 0:384], oab[:, i4, :], [oak], [("mx", s, t, 0)])
                P.barrier()

                with ExitStack() as ph:
                  if "B" in _SKIP:
                    pass
                  else:
                      brotP = Rot(range(8))
                      wFt = [sbuf(ph, "b_wF%d" % i, [128, 8, 128], BF16) for i in range(3)]
                      wTt = sbuf(ph, "b_wT", [128, 8, 256], BF16)
                      qlT = sbuf(ph, "b_qlT", [128, 6, S], BF16)
                      ckvT = sbuf(ph, "b_ckvT", [128, S], BF16)
                      vpa = sbuf(ph, "b_vpa", [128, NB, 6, 65], BF16)
                      iqT = sbuf(ph, "b_iqT", [128, 4, S], BF16)
                      ikT = sbuf(ph, "b_ikT", [128, S], BF16)
                      iw = sbuf(ph, "b_iw", [128, NB, 8])
                      sqt = [sbuf(ph, "b_sqt%d" % i, [128, 512], BF16) for i in range(2)]
                      ckn = [sbuf(ph, "b_ckn%d" % i, [128, 128], BF16) for i in range(2)]
                      cst = [sbuf(ph, "b_cst%d" % i, [128, 2]) for i in range(2)]
                      cjunk = sbuf(ph, "b_cjunk", [128, 128], BF16)
                      score = [sbuf(ph, "b_score%d" % i, [128, S]) for i in range(4)]
                      negm = [sbuf(ph, "b_negm%d" % i, [128, S], BF16) for i in range(4)]
                      rel = [sbuf(ph, "b_rel%d" % i, [128, 512], BF16) for i in range(4)]
                      dg = [sbuf(ph, "b_dg%d" % i, [128, 8, 128], BF16) for i in range(2)]
                      bis = [sbuf(ph, "b_bis%d" % i, [128, 8]) for i in range(4)]
                      bjunk = sbuf(ph, "b_bjunk", [128, S], BF16)
                      pt = [sbuf(ph, "b_pt%d" % i, [128, 512], BF16) for i in range(4)]
                      rr = sbuf(ph, "b_rr", [128, 4, 1])
                      ob = [sbuf(ph, "b_ob%d" % i, [128, 4, 384], BF16) for i in range(2)]

                      dma("pool", wTt[:, :, :], wT_d[l, :, :, T_CKV[0]:T_CKV[0] + 256], [], ["b_wT"])
                      memset("pool", vpa[:, :, :, 64:65], 1.0, ["vpa"])
                      sqrot = Rot(range(2))
                      for c in range(3):
                          def ev_sq(tg, pa, b, c=c):
                              si = sqrot.next()
                              cp(evrot.next(), sqt[si][:, :], pa, [pk(b)], [("sqt", si)])
                              for hl in range(2):
                                  b2 = brotP.next()
                                  mm(ps[b2][:, :], wuk_b[hl * 64:(hl + 1) * 64, l, c, :], sqt[si][hl * 64:(hl + 1) * 64, :], True, True,
                                     ["wuk_b", ("sqt", si)], [pk(b2)])
                                  cp(evrot.next(), qlT[:, 2 * c + hl, tg * 512:(tg + 1) * 512], ps[b2][:, :], [pk(b2)], ["qlT"])
                          if '1' not in _SKIP:
                              proj_F(wFt, 6 + c, ev_sq)
                      for c in range(4):
                          def ev_iq(tg, pa, b, c=c):
                              cp(evrot.next(), iqT[:, c, tg * 512:(tg + 1) * 512], pa, [pk(b)], ["iqT"])
                          if '2' not in _SKIP:
                              proj_F(wFt, 9 + c, ev_iq)

                      def ev_ik(tg, pa, b):
                          cp(evrot.next(), ikT[:, tg * 512:(tg + 1) * 512], pa, [pk(b)], ["ikT"])
                      if '3' not in _SKIP:
                          proj_F(wFt, 13, ev_ik)
                      for t in range(NB if '4' not in _SKIP else 0):
                          i = t % 2
                          b = proj_T(wTt, "b_wT", 136, t)
                          cp("dve", iw[:, t, :], ps[b][:, 128:136], [pk(b)], ["iw"])
                          act(cjunk[:, :], ps[b][:, 0:128], AF.Square, [pk(b)], ["cjunk", ("cst", i)], accum_out=cst[i][:, 0:1])
                          rsqrt_mean("dve", cst[i][:, 1:2], cst[i][:, 0:1], 128, ("cst", i), ("cst", i))
                          ts("dve", ckn[i][:, :], ps[b][:, 0:128], cst[i][:, 1:2], None, ALU.mult, None, [pk(b), ("cst", i)], [("ckn", i)])
                          b2 = brotP.next()
                          pv = ps[b2][:, :].bitcast(BF16)
                          tr(pv[:, 0:128], ckn[i][:, :], [("ckn", i)], [pk(b2)])
                          act(ckvT[:, t * 128:(t + 1) * 128], pv[:, 0:128], AF.Copy, [pk(b2), "gkv"], ["ckvT"], scale=gkv[:, l, :])
                          b3 = brotP.next()
                          mm(ps[b3][:, 0:384], ckvT[:, t * 128:(t + 1) * 128], wuv_b[:, l, :], True, True, ["ckvT", "wuv_b"], [pk(b3)])
                          cp(evrot.next(), vpa[:, t, :, 0:64], ps[b3][:, 0:384].rearrange("p (h e) -> p h e", e=64), [pk(b3)], ["vpa"])

                      relbank = Rot([0, 1])
                      idxacc = Rot([2, 3])
                      relrot = Rot(range(4))
                      scrot = Rot([4, 5])
                      accrot = Rot([6, 7])
                      ptrot = Rot(range(4))
                      scale = 64.0 ** -0.5

                      def index_phase(I):
                          for i4 in range(4):
                              i = 4 * I + i4
                              n = (i + 1) * 128
                              sk = ("score", i4)
                              bk = ("bis", i4)
                              bs = bis[i4]
                              if i < 2 or 'i' in _SKIP:
                                  memset("dve", score[i4][:, 0:n], 0.0, [sk])
                                  tt("dve", score[i4][:, n - 128:n], score[i4][:, n - 128:n], cneg[:, :], ALU.add, [sk, "cneg"], [sk])
                                  memset("dve", bs[:, 0:1], -1e29, [bk])
                              else:
                                  dgi = i % 2
                                  for hh in range(8):
                                      ts("pool", dg[dgi][:, hh, :], ident_b[:, :], iw[:, i, hh:hh + 1], None, ALU.mult, None, ["ident_b", "iw"], [("dg", dgi)])
                                  for kc in range((n + 511) // 512):
                                      w = min(512, n - kc * 512)
                                      ab = idxacc.next()
                                      for hh in range(8):
                                          rb = relbank.next()
                                          r0 = (hh % 2) * 64
                                          mm(ps[rb][:, 0:w], iqT[r0:r0 + 64, hh // 2, i * 128:(i + 1) * 128], ikT[r0:r0 + 64, kc * 512:kc * 512 + w],
                                             True, True, ["iqT", "ikT"], [pk(rb)])
                                          ri = relrot.next()
                                          if hh % 2 == 0:
                                              act(rel[ri][:, 0:w], ps[rb][:, 0:w], AF.Relu, [pk(rb)], [("rel", ri)])
                                          else:
                                              ts("dve", rel[ri][:, 0:w], ps[rb][:, 0:w], 0.0, None, ALU.max, None, [pk(rb)], [("rel", ri)])
                                          mm(ps[ab][:, 0:w], dg[dgi][:, hh, :], rel[ri][:, 0:w], hh == 0, hh == 7, [("dg", dgi), ("rel", ri)], [pk(ab)])
                                      cp("act", score[i4][:, kc * 512:kc * 512 + w], ps[ab][:, 0:w], [pk(ab)], [sk])
                                  tt("dve", score[i4][:, n - 128:n], score[i4][:, n - 128:n], cneg[:, :], ALU.add, [sk, "cneg"], [sk])
                                  reduce(bs[:, 5:6], score[i4][:, 0:n], ALU.max, [sk], [bk])
                                  reduce(bs[:, 0:1], score[i4][:, 0:n - 128], ALU.min, [sk, bk], [bk])
                                  tt("dve", bs[:, 1:2], bs[:, 5:6], bs[:, 0:1], ALU.subtract, [bk], [bk])
                                  for it in range(NBIS):
                                      ts("dve", bs[:, 2:3], bs[:, 1:2], 0.5 ** (it + 1), bs[:, 0:1], ALU.mult, ALU.add, [bk], [bk])
                                      ts("dve", bjunk[:, 0:n], score[i4][:, 0:n], bs[:, 2:3], 0.0, ALU.is_ge, ALU.add, [sk, bk], ["bjunk", bk], accum_out=bs[:, 3:4])
                                      ts("dve", bs[:, 4:5], bs[:, 3:4], 255.5, 1e30, ALU.is_lt, ALU.mult, [bk], [bk])
                                      stt("dve", bs[:, 0:1], bs[:, 2:3], bs[:, 4:5], bs[:, 0:1], ALU.subtract, ALU.max, [bk], [bk])
                              ts("dve", negm[i4][:, 0:n], score[i4][:, 0:n], bs[:, 0:1], NEGM, ALU.is_lt, ALU.mult, [sk, bk], [("negm", i4)])

                      def dsa_phase(I):
                          obb = ob[I % 2]
                          obk = ("ob", I % 2)
                          for h in range(6):
                              ab = accrot.next()
                              first = True
                              for j in range(4 * I + 4):
                                  i0 = max(j, 4 * I)
                                  w = (4 * I + 4 - i0) * 128
                                  sb_ = scrot.next()
                                  mm(ps[sb_][:, 0:w], ckvT[:, j * 128:(j + 1) * 128], qlT[:, h, i0 * 128:(4 * I + 4) * 128], True, False,
                                     ["ckvT", "qlT"], [pk(sb_)])
                                  for i in range(i0, 4 * I + 4):
                                      i4 = i - 4 * I
                                      mm(ps[sb_][:, (i - i0) * 128:(i - i0 + 1) * 128], negm[i4][:, j * 128:(j + 1) * 128], ident_b[:, :], False,
                                         i == 4 * I + 3, [("negm", i4), "ident_b"], [pk(sb_)])
                                  pi = ptrot.next()
                                  act(pt[pi][:, 0:w], ps[sb_][:, 0:w], AF.Exp, [pk(sb_)], [("pt", pi)], scale=scale)
                                  for i in range(i0, 4 * I + 4):
                                      mm(ps[ab][:, (i - 4 * I) * 65:(i - 4 * I) * 65 + 65], pt[pi][:, (i - i0) * 128:(i - i0 + 1) * 128],
                                         vpa[:, j, h, :], first, j == i, [("pt", pi), "vpa"], [pk(ab)])
                                      first = False
                              a0 = ps[ab][:, 0:260].rearrange("p (i e) -> p i e", e=65)
                              recip(rr[:, :, :], a0[:, :, 64:65], [pk(ab)], ["rrb"])
                              tt("dve", obb[:, :, h * 64:(h + 1) * 64], a0[:, :, 0:64], rr[:, :, :].to_broadcast([128, 4, 64]), ALU.mult, [pk(ab), "rrb"], [obk])
                          for i4 in range(4):
                              t = 4 * I + i4
                              dma("sp", mixed_d[s, t * 128:(t + 1) * 128, 384:768], obb[:, i4, :], [obk], [("mx", s, t, 1)])

                      for I in range(4):
                          if 'x' not in _SKIP:
                              index_phase(I)
                          if 'd' not in _SKIP:
                              dsa_phase(I)
                P.barrier()

                with ExitStack() as ph:
                  if "C" in _SKIP:
                    pass
                  else:
                      brotP = Rot(range(8))
                      wFt = [sbuf(ph, "c_wF%d" % i, [128, 8, 128], BF16) for i in range(3)]
                      wTt = sbuf(ph, "c_wT", [128, 8, 256], BF16)
                      rT = [sbuf(ph, "c_rqT", [128, 2, S], BF16), sbuf(ph, "c_rkT", [128, 2, S], BF16)]
                      rv = sbuf(ph, "c_rv", [128, NB, 256], BF16)
                      t1 = sbuf(ph, "c_t1", [128, 2, S])
                      t2 = [sbuf(ph, "c_t2_%d" % i, [128, 512]) for i in range(2)]
                      kz = [sbuf(ph, "c_kz%d" % i, [128, 128], BF16) for i in range(2)]
                      qxi = [sbuf(ph, "c_qxi%d" % i, [128, 128], BF16) for i in range(2)]
                      attD = [sbuf(ph, "c_attD%d" % i, [128, 128], BF16) for i in range(4)]
                      Rf = sbuf(ph, "c_Rf", [128, 2, 64])
                      Rb = sbuf(ph, "c_Rb", [128, 2, 64], BF16)
                      oc = [sbuf(ph, "c_oc%d" % i, [128, 4, 64]) for i in range(2)]
                      ocs = sbuf(ph, "c_ocs", [128, 4, 64])
                      ocb = [sbuf(ph, "c_ocb%d" % i, [128, 4, 64], BF16) for i in range(2)]
                      ss4 = sbuf(ph, "c_ss4", [128, 4, 1])

                      dma("pool", wTt[:, :, :], wT_d[l, :, :, T_RV[0]:T_RV[1]], [], ["c_wT"])
                      cosT = sbuf(ph, "c_cosT", [128, S])
                      sinT = sbuf(ph, "c_sinT", [128, S])
                      dma("sp", cosT[:, :], cd["cosT"][:, :], [], ["cosT"])
                      dma("sp", sinT[:, :], cd["sinT"][:, :], [], ["sinT"])
                      for qk in range(2):
                          for c in range(2):
                              def ev_x(tg, pa, b, c=c, qk=qk):
                                  i = tg % 2
                                  tt("dve", t1[:, c, tg * 512:(tg + 1) * 512], pa, cosT[:, tg * 512:(tg + 1) * 512], ALU.mult, [pk(b), "cosT"], [("t1", tg, c)])
                              proj_F(wFt, 14 + 4 * qk + c, ev_x)
                          for c in range(2):
                              def ev_xs(tg, pa, b, c=c, qk=qk):
                                  i = tg % 2
                                  tt("dve", t2[i][:, :], pa, sinT[:, tg * 512:(tg + 1) * 512], ALU.mult, [pk(b), "sinT"], [("t2", i)])
                                  tt("pool", rT[qk][:, c, tg * 512:(tg + 1) * 512], t1[:, c, tg * 512:(tg + 1) * 512], t2[i][:, :], ALU.add, [("t1", tg, c), ("t2", i)], [("rT", qk)])
                              proj_F(wFt, 16 + 4 * qk + c, ev_xs)
                      for t in range(NB):
                          b = proj_T(wTt, "c_wT", 256, t)
                          cp(evrot.next(), rv[:, t, :], ps[b][:, 0:256], [pk(b)], ["rv"])

                      memset("dve", Rf[:, :, :], 0.0, ["Rf"])
                      memset("dve", Rb[:, :, :], 0.0, ["Rb"])
                      brot = Rot([0, 1, 2, 3])
                      obrot = Rot([4, 5])
                      rbrot = Rot([6, 7])
                      adrot = Rot(range(4))
                      for t in range(NB):
                          tsl = slice(t * 128, (t + 1) * 128)
                          ob_ = obrot.next()
                          first = True
                          rbanks = []
                          for c in range(2):
                              b = brot.next()
                              pv = ps[b][:, :].bitcast(BF16)
                              tr(pv[:, 0:128], rT[1][:, c, tsl], [("rT", 1)], [pk(b)])
                              tt("dve", kz[c][:, :], pv[:, 0:128], zeta[:, c, :], ALU.mult, [pk(b), "zeta"], [("kz", c)])
                              tt("pool", qxi[c][:, :], rT[0][:, c, tsl], xiT[:, c, :], ALU.mult, [("rT", 0), "xiT"], [("qxi", c)])
                              for hl in range(2):
                                  h = 2 * c + hl
                                  rows = slice(hl * 64, (hl + 1) * 64)
                                  b = brot.next()
                                  mm(ps[b][:, 0:128], rT[1][rows, c, tsl], rT[0][rows, c, tsl], True, True, [("rT", 1), ("rT", 0)], [pk(b)])
                                  ai = adrot.next()
                                  tt("dve", attD[ai][:, :], ps[b][:, 0:128], dintraT[:, h, :], ALU.mult, [pk(b), "dintraT"], [("attD", ai)])
                                  mm(ps[ob_][:, h * 64:(h + 1) * 64], attD[ai][:, :], rv[:, t, h * 64:(h + 1) * 64], first, t == 0,
                                     [("attD", ai), "rv"], [pk(ob_)])
                                  first = False
                                  if t > 0:
                                      mm(ps[ob_][:, h * 64:(h + 1) * 64], qxi[c][rows, :], Rb[rows, c, :], False, True,
                                         [("qxi", c), "Rb"], [pk(ob_)])
                              if t < NB - 1:
                                  rb_ = rbrot.next()
                                  mm(ps[rb_][:, 0:128], kz[c][:, :], rv[:, t, c * 128:(c + 1) * 128], True, True, [("kz", c), "rv"], [pk(rb_)])
                                  rbanks.append(rb_)
                          if t < NB - 1:
                              for c in range(2):
                                  for hl in range(2):
                                      rows = slice(hl * 64, (hl + 1) * 64)
                                      stt("dve", Rf[rows, c, :], Rf[rows, c, :], gch[rows, c:c + 1], ps[rbanks[c]][rows, hl * 64:(hl + 1) * 64],
                                          ALU.mult, ALU.add, ["Rf", "gch", pk(rbanks[c])], ["Rf"])
                              cp("pool", Rb[:, :, :], Rf[:, :, :], ["Rf"], ["Rb"])
                          oi = t % 2
                          cp("act", oc[oi][:, :, :], ps[ob_][:, 0:256].rearrange("p (h e) -> p h e", e=64), [pk(ob_)], [("oc", oi)])
                          tt("pool", ocs[:, :, :], oc[oi][:, :, :], oc[oi][:, :, :], ALU.mult, [("oc", oi)], ["ocs"])
                          reduce(ss4[:, :, :], ocs[:, :, :], ALU.add, ["ocs"], ["c_ss4"])
                          rsqrt_mean("dve", ss4[:, :, :], ss4[:, :, :], 64, "c_ss4", "c_ss4")
                          tt("pool", oc[oi][:, :, :], oc[oi][:, :, :], ss4[:, :, :].to_broadcast([128, 4, 64]), ALU.mult, [("oc", oi), "c_ss4"], [("oc", oi)])
                          tt("pool", ocb[oi][:, :, :], oc[oi][:, :, :], gret[:, l:l + 1, :].to_broadcast([128, 4, 64]), ALU.mult, [("oc", oi), "gret"], [("ocb", oi)])
                          dma("sp", mixed_d[s, tsl, 768:1024], ocb[oi][:, :, :].rearrange("p h e -> p (h e)"), [("ocb", oi)], [("mx", s, t, 2)])
                P.barrier()

                with ExitStack() as ph:
                  if "D" in _SKIP:
                    pass
                  else:
                      wg = sbuf(ph, "d_wg", [128, 8, D], BF16)
                      wo = sbuf(ph, "d_wo", [128, 8, D], BF16)
                      hb = [sbuf(ph, "d_h%d" % i, [128, D]) for i in range(2)]
                      mx = [sbuf(ph, "d_mx%d" % i, [128, D], BF16) for i in range(2)]
                      sg = [sbuf(ph, "d_sg%d" % i, [128, D], BF16) for i in range(2)]
                      yb = [sbuf(ph, "d_y%d" % i, [128, D], BF16) for i in range(2)]
                      yT = [sbuf(ph, "d_yT%d" % i, [128, 8, 128], BF16) for i in range(2)]
                      hn = [sbuf(ph, "d_hn%d" % i, [128, D]) for i in range(2)]
                      fo = [sbuf(ph, "d_fo%d" % i, [128, D]) for i in range(2)]
                      fjunk = sbuf(ph, "d_fjunk", [128, D], BF16)
                      fst = [sbuf(ph, "d_fst%d" % i, [128, 2]) for i in range(2)]
                      for kh in range(2):
                          dma("pool", wg[:, :, kh * 512:(kh + 1) * 512], wT_d[l, :, :, T_GATE[0] + kh * 512:T_GATE[0] + (kh + 1) * 512], [], ["d_wg"])
                          dma("pool", wo[:, :, kh * 512:(kh + 1) * 512], wout_d[l, :, :, kh * 512:(kh + 1) * 512], [], ["d_wo"])
                      gb = Rot([0, 1, 2, 3])
                      tb = Rot([4, 5])
                      obk = Rot([6, 7, 0, 1, 2, 3])
                      for t in range(NB):
                          i = t % 2
                          tsl = slice(t * 128, (t + 1) * 128)
                          dma("sp", hb[i][:, :], h_src(l, s, t), [hkey(s, t)], [("dhb", i)])
                          dma("sp", mx[i][:, :], mixed_d[s, tsl, :], [("mx", s, t, 0), ("mx", s, t, 1), ("mx", s, t, 2)], [("dmx", i)])
                          for kh in range(2):
                              b = gb.next()
                              for k in range(8):
                                  mm(ps[b][:, :], uT[:, k, tsl], wg[:, k, kh * 512:(kh + 1) * 512], k == 0, k == 7, ["uT", "d_wg"], [pk(b)])
                              act(sg[i][:, kh * 512:(kh + 1) * 512], ps[b][:, :], AF.Silu, [pk(b)], [("sg", i)])
                          tt("pool", yb[i][:, :], sg[i][:, :], mx[i][:, :], ALU.mult, [("sg", i), ("dmx", i)], [("yb", i)])
                          b = tb.next()
                          pv = ps[b][:, :].bitcast(BF16)
                          for k in range(8):
                              tr(pv[:, k * 128:(k + 1) * 128], yb[i][:, k * 128:(k + 1) * 128], [("yb", i)], [pk(b)])
                          cp("dve", yT[i][:, :, :], pv[:, :].rearrange("p (k q) -> p k q", q=128), [pk(b)], [("yT", i)])
                          for kh in range(2):
                              b = gb.next()
                              for k in range(8):
                                  mm(ps[b][:, :], yT[i][:, k, :], wo[:, k, kh * 512:(kh + 1) * 512], k == 0, k == 7, [("yT", i), "d_wo"], [pk(b)])
                              tt("dve", hn[i][:, kh * 512:(kh + 1) * 512], ps[b][:, :], hb[i][:, kh * 512:(kh + 1) * 512], ALU.add, [pk(b), ("dhb", i)], [("dhn", i)])
                          if not last_layer:
                              dma("sp", hbuf_d[s, tsl, :], hn[i][:, :], [("dhn", i)], [hkey(s, t)])
                          else:
                              act(fjunk[:, :], hn[i][:, :], AF.Square, [("dhn", i)], ["fjunk", ("fst", i)], accum_out=fst[i][:, 0:1])
                              rsqrt_mean("dve", fst[i][:, 1:2], fst[i][:, 0:1], D, ("fst", i), ("fst", i))
                              stt("dve", fo[i][:, :], hn[i][:, :], fst[i][:, 1:2], gfin[:, :], ALU.mult, ALU.mult, [("dhn", i), ("fst", i), "gfin"], [("fo", i)])
                              dma("sp", out_d[s, tsl, :], fo[i][:, :], [("fo", i)], [("out", s, t)])
                P.barrier()
        P.barrier()
        P.emit()
    return nc


_PROG_CACHE = {}


def kernel(x, attn_norm, w_in, diff_lambda, diff_norm, kv_norm, w_uk, w_uv, ret_norm, w_out, final_norm):
    x = np.asarray(x, dtype=np.float32)
    args = [np.asarray(a, dtype=np.float32) for a in (attn_norm, w_in, diff_lambda, diff_norm, kv_norm, w_uk, w_uv, ret_norm, w_out, final_norm)]
    w = _host_weights(*args)
    consts = _host_consts()
    B = x.shape[0]
    ns = B // NCORES
    if "nc" not in _PROG_CACHE:
        _PROG_CACHE["nc"] = build_program(DEPTH, ns)
    nc = _PROG_CACHE["nc"]
    in_maps = []
    for c in range(NCORES):
        m = {"x": np.ascontiguousarray(x[c * ns:(c + 1) * ns])}
        m.update(w)
        for k, v in consts.items():
            m["c_" + k] = v
        in_maps.append(m)
    res = run_bass_kernel_spmd(nc, in_maps, core_ids=list(range(NCORES)))
    return np.concatenate([r["out"] for r in res.results], axis=0)
```

```python
import math
import numpy as np
from contextlib import ExitStack
import concourse.bass as bass
import concourse.mybir as mybir
from concourse.bass_utils import run_bass_kernel_spmd

F32 = mybir.dt.float32
BF16 = mybir.dt.bfloat16
ALU = mybir.AluOpType
AF = mybir.ActivationFunctionType
AX = mybir.AxisListType

S = 2048
D = 1024
NB = S // 128
DEPTH = 4
NCORES = 8
EPS = 1e-6
NBIS = 16
NEGM = -30000.0
import os
_SKIP = os.environ.get('KSKIP', '')


class Prog:
    ENG = ("pe", "act", "dve", "pool", "sp")
    NDS = 12

    def __init__(self, nc, stack):
        self.nc = nc
        self.q = {e: [] for e in self.ENG}
        self.cnt = {e: 0 for e in self.ENG}
        self.sem = {e: stack.enter_context(nc.semaphore("prog_" + e)) for e in self.ENG}
        self.seen = {e: {f: 0 for f in self.ENG} for e in self.ENG}
        self.dseen = {e: {} for e in self.ENG}
        self.lastw = {}
        self.readers = {}
        self.dsem = {}
        self.drot = {}
        for qn in ("sp", "pool"):
            for i in range(self.NDS):
                nm = "%s%d" % (qn, i)
                self.dsem[nm] = [stack.enter_context(nc.semaphore("d_" + nm)), 0]
            self.drot[qn] = 0

    def _need(self, eng, dep, waits):
        if dep is None:
            return
        if dep[0] == "e":
            _, f, idx = dep
            if self.seen[eng][f] < idx:
                self.seen[eng][f] = idx
                waits[("e", f)] = (self.sem[f], idx)
        else:
            _, name, val = dep
            if self.dseen[eng].get(name, 0) < val:
                self.dseen[eng][name] = val
                waits[("d", name)] = (self.dsem[name][0], val)

    def _deps(self, eng, reads, writes):
        waits = {}
        for k in reads:
            self._need(eng, self.lastw.get(k), waits)
        for k in writes:
            lw = self.lastw.get(k)
            if lw is not None and not (lw[0] == "e" and lw[1] == eng):
                self._need(eng, lw, waits)
            for dep in self.readers.get(k, {}).values():
                if not (dep[0] == "e" and dep[1] == eng):
                    self._need(eng, dep, waits)
        return list(waits.values())

    def op(self, eng, fn, reads=(), writes=()):
        writes = list(writes) + [k for k in reads if isinstance(k, tuple) and k[0] == "ps" and k not in writes]
        waits = self._deps(eng, reads, writes)
        self.cnt[eng] += 1
        idx = self.cnt[eng]
        self.q[eng].append((waits, fn, (self.sem[eng], 1)))
        dep = ("e", eng, idx)
        for k in reads:
            self.readers.setdefault(k, {})[eng] = dep
        for k in writes:
            self.lastw[k] = dep
            self.readers[k] = {}
        return dep

    def dma(self, queue, fn, reads=(), writes=()):
        name = "%s%d" % (queue, self.drot[queue])
        self.drot[queue] = (self.drot[queue] + 1) % self.NDS
        waits = {}
        self._need(queue, ("d", name, self.dsem[name][1]), waits) if self.dsem[name][1] else None
        w2 = self._deps(queue, reads, writes)
        allw = list(waits.values()) + w2
        self.dsem[name][1] += 16
        val = self.dsem[name][1]
        self.q[queue].append((allw, fn, (self.dsem[name][0], 16)))
        dep = ("d", name, val)
        for k in writes:
            self.lastw[k] = dep
            self.readers[k] = {}
        for k in reads:
            self.readers.setdefault(k, {})["dma:" + name] = dep
        return dep

    def barrier(self):
        for e in self.ENG:
            waits = {}
            for f in self.ENG:
                if f != e and self.cnt[f]:
                    self._need(e, ("e", f, self.cnt[f]), waits)
            for name, (h, val) in self.dsem.items():
                if val:
                    self._need(e, ("d", name, val), waits)
            self.q[e].append((list(waits.values()), None, None))

    def emit(self):
        nc = self.nc
        q = self.q
        with nc.Block() as block:
            def replay(name):
                def run(e):
                    for waits, fn, inc in q[name]:
                        for s, v in waits:
                            e.wait_ge(s, v)
                        if fn is not None:
                            fn(e).then_inc(inc[0], inc[1])
                return run
            block.sync(replay("sp"))
            block.tensor(replay("pe"))
            block.scalar(replay("act"))
            block.vector(replay("dve"))
            block.gpsimd(replay("pool"))


class Rot:
    def __init__(self, items):
        self.items = list(items)
        self.i = 0

    def next(self):
        v = self.items[self.i % len(self.items)]
        self.i += 1
        return v


def _col_maps():
    o_dq, o_dk, o_dv, o_sq, o_ckv, o_iq, o_ik, o_iw, o_rq, o_rk, o_rv, o_gate = (
        0, 384, 768, 1152, 1536, 1664, 2176, 2240, 2248, 2504, 2760, 3016)
    colsF = []
    for base in (o_dq, o_dk, o_sq):
        for c in range(3):
            colsF.append(np.arange(base + 128 * c, base + 128 * c + 128))
    for c in range(4):
        colsF.append(np.arange(o_iq + 128 * c, o_iq + 128 * c + 128))
    colsF.append(np.concatenate([np.arange(o_ik, o_ik + 64)] * 2))

    def swap(base, c):
        out = []
        for hl in range(2):
            b = base + 128 * c + 64 * hl
            out += [np.arange(b + 32, b + 64), np.arange(b, b + 32)]
        return np.concatenate(out)
    for base in (o_rq, o_rk):
        for c in range(2):
            colsF.append(np.arange(base + 128 * c, base + 128 * c + 128))
        for c in range(2):
            colsF.append(swap(base, c))
    colsF = np.concatenate(colsF)
    colsT = np.concatenate([np.arange(o_dv, o_dv + 384), np.arange(o_ckv, o_ckv + 128),
                            np.arange(o_iw, o_iw + 8), np.arange(o_rv, o_rv + 256),
                            np.arange(o_gate, o_gate + 1024)])
    return colsF, colsT


NF = 22
T_DV = (0, 384)
T_CKV = (384, 520)
T_RV = (520, 776)
T_GATE = (776, 1800)
NT = 1800


def _host_consts():
    f32 = np.float32
    c = {}
    c["ident"] = np.eye(128, dtype=f32)
    r = np.arange(128)
    c["tri"] = (r[None, :] >= r[:, None]).astype(f32)
    c["cneg"] = np.where(r[None, :] <= r[:, None], 0.0, -1e30).astype(f32)
    inv_freq = (f32(10000.0) ** (-(np.arange(32, dtype=f32)) / f32(32))).astype(f32)
    ang = (np.arange(S, dtype=f32)[:, None] * inv_freq[None, :]).astype(f32)
    cos, sin = np.cos(ang).astype(f32), np.sin(ang).astype(f32)
    rr = np.arange(128)
    cosT = cos[:, rr % 32].T.copy()
    sgn = np.where((rr % 64) < 32, -1.0, 1.0).astype(f32)
    sinT = (sin[:, rr % 32].T * sgn[:, None]).astype(f32)
    c["cosT"], c["sinT"] = np.ascontiguousarray(cosT), np.ascontiguousarray(sinT)
    H = 4
    log_g = np.log(f32(1.0) - f32(2.0) ** (f32(-5.0) - np.arange(H, dtype=f32))).astype(f32)
    pos = np.arange(128, dtype=f32)
    diff = pos[:, None] - pos[None, :]
    d_intra = np.where(diff >= 0, np.exp(np.maximum(diff, 0.0)[None] * log_g[:, None, None]), 0.0).astype(f32)
    xi = np.exp((pos + 1.0)[:, None] * log_g[None, :]).astype(f32)
    zeta = np.exp((127.0 - pos)[:, None] * log_g[None, :]).astype(f32)
    gch = np.exp(128.0 * log_g).astype(f32)
    sc = f32(64.0 ** -0.5)
    c["dintraT"] = np.ascontiguousarray(np.transpose(d_intra, (2, 0, 1)) * sc).astype(f32)
    xiT = np.zeros((128, 2, 128), f32)
    zt = np.zeros((128, 2, 128), f32)
    gc = np.zeros((128, 2), f32)
    for cc in range(2):
        for hl in range(2):
            h = 2 * cc + hl
            xiT[hl * 64:(hl + 1) * 64, cc, :] = xi[None, :, h]
            zt[:, cc, hl * 64:(hl + 1) * 64] = (zeta[:, h] * sc)[:, None]
            gc[hl * 64:(hl + 1) * 64, cc] = gch[h]
    c["xiT"], c["zeta"], c["gch"] = xiT, zt, gc
    return c


_CONST_SHAPES = {"ident": [128, 128], "tri": [128, 128], "cneg": [128, 128], "cosT": [128, S], "sinT": [128, S],
                 "dintraT": [128, 4, 128], "xiT": [128, 2, 128], "zeta": [128, 2, 128], "gch": [128, 2]}


def _host_weights(attn_norm, w_in, diff_lambda, diff_norm, kv_norm, w_uk, w_uv, ret_norm, w_out, final_norm):
    L = w_in.shape[0]
    colsF, colsT = _col_maps()
    w = {}
    wf = w_in[:, :, colsF].reshape(L, 8, 128, NF, 128)
    w["wF"] = np.ascontiguousarray(np.transpose(wf, (0, 3, 2, 1, 4)))
    wt = w_in[:, :, colsT].reshape(L, 8, 128, NT)
    w["wT"] = np.ascontiguousarray(np.transpose(wt, (0, 2, 1, 3)))
    w["wout"] = np.ascontiguousarray(np.transpose(w_out.reshape(L, 8, 128, D), (0, 2, 1, 3)))
    uk = w_uk.reshape(L, 3, 2, 64, 128)
    w["wuk"] = np.ascontiguousarray(np.transpose(uk, (0, 2, 3, 1, 4)).reshape(L, 128, 3, 128))
    w["wuv"] = np.ascontiguousarray(np.transpose(w_uv, (0, 2, 1, 3)).reshape(L, 128, 384))
    w["gattn"] = np.ascontiguousarray(np.transpose(attn_norm.reshape(L, 8, 128), (0, 2, 1)))
    w["gkv"] = np.ascontiguousarray(kv_norm.reshape(L, 128, 1))
    w["gdiff"] = np.ascontiguousarray(diff_norm)
    w["gret"] = np.ascontiguousarray(ret_norm)
    w["gfin"] = np.ascontiguousarray(final_norm)
    w["dlam"] = np.ascontiguousarray(diff_lambda.reshape(L, 128))
    return w


def build_program(NL=DEPTH, NS=2, debug=False):
    nc = bass.Bass("TRN2", target_bir_lowering=False)
    L = NL
    dt = lambda name, shape, dtype=F32, kind="ExternalInput": nc.dram_tensor(name, shape, dtype, kind=kind).ap()
    x_d = dt("x", [NS, S, D])
    wF_d = dt("wF", [L, NF, 128, 8, 128])
    wT_d = dt("wT", [L, 128, 8, NT])
    wout_d = dt("wout", [L, 128, 8, D])
    wuk_d = dt("wuk", [L, 128, 3, 128])
    wuv_d = dt("wuv", [L, 128, 384])
    gattn_d = dt("gattn", [L, 128, 8])
    gkv_d = dt("gkv", [L, 128, 1])
    gdiff_d = dt("gdiff", [L, 64])
    gret_d = dt("gret", [L, 64])
    gfin_d = dt("gfin", [D])
    dlam_d = dt("dlam", [L, 128])
    cd = {k: dt("c_" + k, shp) for k, shp in _CONST_SHAPES.items()}
    out_d = dt("out", [NS, S, D], F32, "ExternalOutput")
    hbuf_d = dt("hbuf", [NS, S, D], F32, "Internal")
    mixed_d = dt("mixed", [NS, S, D], BF16, "Internal")
    dbg_d = dt("dbg", [NS, S, D], F32, "ExternalOutput") if debug else None

    with ExitStack() as st:
        P = Prog(nc, st)
        _uid = [0]

        def sbuf(stack, name, shape, dtype=F32):
            _uid[0] += 1
            return stack.enter_context(nc.sbuf_tensor("s%d_%s" % (_uid[0], name), shape, dtype))
        ps = [st.enter_context(nc.psum_tensor("ps%d" % i, [128, 512], F32)) for i in range(8)]
        pk = lambda b: ("ps", b)

        def mm(out, lhsT, rhs, start, stop, reads, writes, **kw):
            P.op("pe", lambda e: e.matmul(out, lhsT=lhsT, rhs=rhs, start=start, stop=stop,
                                          skip_group_check=True, **kw), reads, writes)

        def tr(out, in_, reads, writes):
            P.op("pe", lambda e: e.transpose(out, in_, ident_b[:, :]), list(reads) + ["ident_b"], writes)

        def act(out, in_, func, reads, writes, **kw):
            P.op("act", lambda e: e.activation(out=out, in_=in_, func=func, **kw), reads, writes)

        def ts(eng, out, in0, s1, s2, op0, op1, reads, writes, **kw):
            if op1 is None:
                P.op(eng, lambda e: e.tensor_scalar(out, in0, s1, None, op0=op0, **kw), reads, writes)
            else:
                P.op(eng, lambda e: e.tensor_scalar(out, in0, s1, s2, op0=op0, op1=op1, **kw), reads, writes)

        def tt(eng, out, in0, in1, op, reads, writes):
            P.op(eng, lambda e: e.tensor_tensor(out, in0, in1, op=op), reads, writes)

        def stt(eng, out, in0, scalar, in1, op0, op1, reads, writes):
            P.op(eng, lambda e: e.scalar_tensor_tensor(out, in0, scalar, in1, op0=op0, op1=op1), reads, writes)

        def cp(eng, out, in_, reads, writes):
            if eng == "act":
                P.op("act", lambda e: e.copy(out, in_), reads, writes)
            else:
                P.op(eng, lambda e: e.tensor_copy(out, in_), reads, writes)

        def recip(out, in_, reads, writes):
            P.op("dve", lambda e: e.reciprocal(out, in_), reads, writes)

        def reduce(out, in_, op, reads, writes):
            P.op("dve", lambda e: e.tensor_reduce(out, in_, axis=AX.X, op=op), reads, writes)

        def memset(eng, ap, val, writes):
            P.op(eng, lambda e: e.memset(ap, val), [], writes)

        def dma(queue, out, in_, reads, writes):
            return P.dma(queue, lambda e: e.dma_start(out=out, in_=in_), reads, writes)

        def rsqrt_mean(eng_unused, out, ss, n, key_out, key_ss):
            ts("dve", out, ss, 1.0 / n, EPS, ALU.mult, ALU.add, [key_ss], [key_out])
            act(out, out, AF.Sqrt, [key_out], [key_out])
            P.op("dve", lambda e: e.reciprocal(out, out), [key_out], [key_out])

        ident_b = sbuf(st, "ident_b", [128, 128], BF16)
        tri_b = sbuf(st, "tri_b", [128, 128], BF16)
        cneg = sbuf(st, "cneg", [128, 128])
        dintraT = sbuf(st, "dintraT", [128, 4, 128])
        xiT = sbuf(st, "xiT", [128, 2, 128])
        zeta = sbuf(st, "zeta", [128, 2, 128])
        gch = sbuf(st, "gch", [128, 2])
        gattn = sbuf(st, "gattn", [128, L, 8])
        gkv = sbuf(st, "gkv", [128, L, 1])
        gdm = sbuf(st, "gdm", [128, L, 64])
        gret = sbuf(st, "gret", [128, L, 64])
        dlam = sbuf(st, "dlam", [128, L, 128])
        lamneg = sbuf(st, "lamneg", [128, L, 1])
        lamt = sbuf(st, "lamt", [128, 4])
        uT = sbuf(st, "uT", [128, 8, S], BF16)

        dma("pool", ident_b[:, :], cd["ident"][:, :], [], ["ident_b"])
        dma("pool", tri_b[:, :], cd["tri"][:, :], [], ["tri_b"])
        for nm, t in (("cneg", cneg), ("gch", gch)):
            dma("sp", t[:, :], cd[nm][:, :], [], [nm])
        for nm, t in (("dintraT", dintraT), ("xiT", xiT), ("zeta", zeta)):
            dma("sp", t[:, :, :], cd[nm][:, :, :], [], [nm])
        for l in range(L):
            dma("sp", gattn[:, l, :], gattn_d[l, :, :], [], ["gattn"])
            dma("sp", gkv[:, l, :], gkv_d[l, :, :], [], ["gkv"])
            dma("sp", gdm[:, l, :], gdiff_d[l, :].partition_broadcast(128), [], ["gdm"])
            dma("sp", gret[:, l, :], gret_d[l, :].partition_broadcast(128), [], ["gret"])
            dma("sp", dlam[:, l, :], dlam_d[l, :].partition_broadcast(128), [], ["dlam"])
        for l in range(L):
            lam_init = 0.8 - 0.6 * math.exp(-0.3 * l)
            junk = dlam[:, l, 0:32]
            tt("dve", dlam[:, l, 0:32], dlam[:, l, 0:32], dlam[:, l, 32:64], ALU.mult, ["dlam"], ["dlam"])
            tt("dve", dlam[:, l, 64:96], dlam[:, l, 64:96], dlam[:, l, 96:128], ALU.mult, ["dlam"], ["dlam"])
            reduce(lamt[:, 0:1], dlam[:, l, 0:32], ALU.add, ["dlam"], ["lamt"])
            reduce(lamt[:, 1:2], dlam[:, l, 64:96], ALU.add, ["dlam", "lamt"], ["lamt"])
            act(lamt[:, 2:4], lamt[:, 0:2], AF.Exp, ["lamt"], ["lamt"])
            tt("dve", lamt[:, 0:1], lamt[:, 3:4], lamt[:, 2:3], ALU.subtract, ["lamt"], ["lamt"])
            ts("dve", lamneg[:, l, :], lamt[:, 0:1], -lam_init, None, ALU.add, None, ["lamt"], ["lamneg"])
            ts("dve", gdm[:, l, :], gdm[:, l, :], 1.0 - lam_init, None, ALU.mult, None, ["gdm"], ["gdm"])

        wrotF = Rot(range(3))
        wrotT = Rot(range(2))
        evrot = Rot(["act", "dve"])

        def h_src(l, s, t):
            src = x_d if l == 0 else hbuf_d
            return src[s, t * 128:(t + 1) * 128, :]

        def hkey(s, t):
            return ("h", s, t)

        for l in range(L):
            last_layer = (l == L - 1)
            for s in range(NS):
                with ExitStack() as ph:
                    hb = [sbuf(ph, "p0_h%d" % i, [128, D]) for i in range(2)]
                    hn = [sbuf(ph, "p0_hn%d" % i, [128, D], BF16) for i in range(2)]
                    sq_junk = sbuf(ph, "p0_junk", [128, D], BF16)
                    st0 = [sbuf(ph, "p0_st%d" % i, [128, 2]) for i in range(2)]
                    brot = Rot(range(8))
                    gat3 = sbuf(ph, "p0_g3", [128, 8, 1])
                    cp("dve", gat3[:, :, :].rearrange("p k o -> p (k o)"), gattn[:, l, :], ["gattn"], ["gat3"])

                    def p0_stage1(t):
                        i = t % 2
                        dma("sp", hb[i][:, :], h_src(l, s, t), [hkey(s, t)], [("hb", i)])
                        act(sq_junk[:, :], hb[i][:, :], AF.Square, [("hb", i)], ["sqj", ("st0", i)], accum_out=st0[i][:, 0:1])
                        rsqrt_mean("dve", st0[i][:, 1:2], st0[i][:, 0:1], D, ("st0", i), ("st0", i))
                        act(hn[i][:, :], hb[i][:, :], AF.Copy, [("hb", i), ("st0", i)], [("hn", i)], scale=st0[i][:, 1:2])

                    def p0_stage2(t):
                        i = t % 2
                        b = brot.next()
                        pv = ps[b][:, :].bitcast(BF16)
                        for k in range(8):
                            tr(pv[:, k * 128:(k + 1) * 128], hn[i][:, k * 128:(k + 1) * 128], [("hn", i)], [pk(b)])
                        tt("dve", uT[:, :, t * 128:(t + 1) * 128], pv[:, :].rearrange("p (k q) -> p k q", q=128),
                           gat3[:, :, :].to_broadcast([128, 8, 128]), ALU.mult, [pk(b), "gat3"], ["uT"])
                    for t in range(NB + 1):
                        if t < NB:
                            p0_stage1(t)
                        if t >= 1:
                            p0_stage2(t - 1)
                P.barrier()

                def load_wF(wtiles, f):
                    i = wrotF.next()
                    dma("pool", wtiles[i][:, :, :], wF_d[l, f, :, :, :], [], [("wF", i)])
                    return i

                def proj_F(wtiles, f, evac):
                    i = load_wF(wtiles, f)
                    for tg in range(4):
                        b = brotP.next()
                        for k in range(8):
                            mm(ps[b][:, :], wtiles[i][:, k, :], uT[:, k, tg * 512:(tg + 1) * 512], k == 0, k == 7,
                               [("wF", i), "uT"], [pk(b)])
                        evac(tg, ps[b][:, :], b)

                def proj_T(wt, wkey, ncols, t, c0=0):
                    b = brotP.next()
                    for k in range(8):
                        mm(ps[b][:, 0:ncols], uT[:, k, t * 128:(t + 1) * 128], wt[:, k, c0:c0 + ncols], k == 0, k == 7,
                           [wkey, "uT"], [pk(b)])
                    return b

                def evac_copy(dst3, c):
                    def f(tg, pa, b):
                        ev = evrot.next()
                        cp(ev, dst3[:, c, tg * 512:(tg + 1) * 512], pa, [pk(b)], [dst3.name if hasattr(dst3, "name") else "x"])
                    return f

                with ExitStack() as ph:
                  if "A" in _SKIP:
                    pass
                  else:
                      brotP = Rot(range(8))
                      wFt = [sbuf(ph, "a_wF%d" % i, [128, 8, 128], BF16) for i in range(3)]
                      wTt = sbuf(ph, "a_wT", [128, 8, 384], BF16)
                      dqT = sbuf(ph, "a_dqT", [128, 3, S], BF16)
                      dkT = sbuf(ph, "a_dkT", [128, 3, S], BF16)
                      dva = sbuf(ph, "a_dva", [128, NB, 6, 65], BF16)
                      pt = [sbuf(ph, "a_pt%d" % i, [128, 512], BF16) for i in range(4)]
                      rr = [sbuf(ph, "a_rr%d" % i, [128, 4, 1]) for i in range(3)]
                      tA = sbuf(ph, "a_tA", [128, 4, 64])
                      tB = sbuf(ph, "a_tB", [128, 4, 64])
                      tO = sbuf(ph, "a_tO", [128, 4, 64])
                      tS = sbuf(ph, "a_tS", [128, 4, 64])
                      ss4 = sbuf(ph, "a_ss4", [128, 4, 1])
                      oa = [sbuf(ph, "a_oa%d" % i, [128, 4, 384], BF16) for i in range(2)]

                      dma("pool", wTt[:, :, :], wT_d[l, :, :, T_DV[0]:T_DV[1]], [], ["a_wT"])
                      memset("pool", dva[:, :, :, 64:65], 1.0, ["dva"])
                      for c in range(3):
                          def ev_q(tg, pa, b, c=c):
                              cp(evrot.next(), dqT[:, c, tg * 512:(tg + 1) * 512], pa, [pk(b)], ["dqT"])
                          proj_F(wFt, c, ev_q)
                      for c in range(3):
                          def ev_k(tg, pa, b, c=c):
                              cp(evrot.next(), dkT[:, c, tg * 512:(tg + 1) * 512], pa, [pk(b)], ["dkT"])
                          proj_F(wFt, 3 + c, ev_k)
                      for t in range(NB):
                          b = proj_T(wTt, "a_wT", 384, t)
                          cp(evrot.next(), dva[:, t, :, 0:64], ps[b][:, 0:384].rearrange("p (h e) -> p h e", e=64), [pk(b)], ["dva"])

                      scrot = Rot([0, 1, 2, 3])
                      accrot = Rot([(4, 5), (6, 7)])
                      ptrot = Rot(range(4))
                      scale = 32.0 ** -0.5

                      def a_epilogue(I, h, banks):
                          oab = oa[I % 2]
                          oak = ("oa", I % 2)
                          a0 = ps[banks[0]][:, 0:260].rearrange("p (i e) -> p i e", e=65)
                          a1 = ps[banks[1]][:, 0:260].rearrange("p (i e) -> p i e", e=65)
                          recip(rr[0][:, :, :], a0[:, :, 64:65], [pk(banks[0])], ["rr0"])
                          recip(rr[1][:, :, :], a1[:, :, 64:65], [pk(banks[1])], ["rr1"])
                          ts("dve", rr[2][:, :, :], rr[1][:, :, :], lamneg[:, l, :], None, ALU.mult, None, ["rr1", "lamneg"], ["rr2"])
                          tt("dve", tA[:, :, :], a0[:, :, 0:64], rr[0][:, :, :].to_broadcast([128, 4, 64]), ALU.mult, [pk(banks[0]), "rr0"], ["tA"])
                          tt("dve", tB[:, :, :], a1[:, :, 0:64], rr[2][:, :, :].to_broadcast([128, 4, 64]), ALU.mult, [pk(banks[1]), "rr2"], ["tB"])
                          tt("pool", tO[:, :, :], tA[:, :, :], tB[:, :, :], ALU.add, ["tA", "tB"], ["tO"])
                          tt("pool", tS[:, :, :], tO[:, :, :], tO[:, :, :], ALU.mult, ["tO"], ["tS"])
                          reduce(ss4[:, :, :], tS[:, :, :], ALU.add, ["tS"], ["ss4"])
                          rsqrt_mean("dve", ss4[:, :, :], ss4[:, :, :], 64, "ss4", "ss4")
                          tt("pool", tO[:, :, :], tO[:, :, :], ss4[:, :, :].to_broadcast([128, 4, 64]), ALU.mult, ["tO", "ss4"], ["tO"])
                          tt("pool", oab[:, :, h * 64:(h + 1) * 64], tO[:, :, :], gdm[:, l:l + 1, :].to_broadcast([128, 4, 64]), ALU.mult, ["tO", "gdm"], [oak])
                          if h == 5:
                              for i4 in range(4):
                                  t = 4 * I + i4
                                  dma("sp", mixed_d[s, t * 128:(t + 1) * 128, 0:384], oab[:, i4, :], [oak], [("mx", s, t, 0)])

                      tiles = []
                      for I in range(4):
                          for h in range(6):
                              banks = accrot.next()
                              for c2 in range(2):
                                  nj = 4 * I + 4
                                  for j in range(nj):
                                      tiles.append(dict(I=I, h=h, c2=c2, j=j, ab=banks[c2], banks=banks, first=(j == 0),
                                                        last=(c2 == 1 and j == nj - 1)))

                      def a_score(T):
                          I, h, c2, j = T["I"], T["h"], T["c2"], T["j"]
                          r0 = (h % 2) * 64 + c2 * 32
                          kw = dict(tile_position=(96, 0)) if r0 == 96 else {}
                          i0 = max(j, 4 * I)
                          w = (4 * I + 4 - i0) * 128
                          sb_ = scrot.next()
                          pi = ptrot.next()
                          T.update(i0=i0, w=w, pi=pi)
                          mm(ps[sb_][:, 0:w], dkT[r0:r0 + 32, h // 2, j * 128:(j + 1) * 128],
                             dqT[r0:r0 + 32, h // 2, i0 * 128:(4 * I + 4) * 128], True, True,
                             ["dkT", "dqT"], [pk(sb_)], **kw)
                          act(pt[pi][:, 0:w], ps[sb_][:, 0:w], AF.Exp, [pk(sb_)], [("pt", pi)], scale=scale)
                          if j >= 4 * I:
                              tt("pool", pt[pi][:, 0:128], pt[pi][:, 0:128], tri_b[:, :], ALU.mult, [("pt", pi), "tri_b"], [("pt", pi)])

                      def a_av(T):
                          I, h, j, i0, pi, ab = T["I"], T["h"], T["j"], T["i0"], T["pi"], T["ab"]
                          for i in range(i0, 4 * I + 4):
                              mm(ps[ab][:, (i - 4 * I) * 65:(i - 4 * I) * 65 + 65], pt[pi][:, (i - i0) * 128:(i - i0 + 1) * 128],
                                 dva[:, j, h, :], T["first"] and i == i0, j == i, [("pt", pi), "dva"], [pk(ab)])
                          if T["last"]:
                              a_epilogue(I, h, T["banks"])

                      DEP = 2
                      for idx in range(len(tiles) + DEP):
                          if idx < len(tiles):
                              a_score(tiles[idx])
                          if idx >= DEP:
                              a_av(tiles[idx - DEP])
                P.barrier()

                with ExitStack() as ph:
                  if "B" in _SKIP:
                    pass
                  else:
                      brotP = Rot(range(8))
                      wFt = [sbuf(ph, "b_wF%d" % i, [128, 8, 128], BF16) for i in range(3)]
                      wTt = sbuf(ph, "b_wT", [128, 8, 256], BF16)
                      qlT = sbuf(ph, "b_qlT", [128, 6, S], BF16)
                      ckvT = sbuf(ph, "b_ckvT", [128, S], BF16)
                      vpa = sbuf(ph, "b_vpa", [128, NB, 6, 65], BF16)
                      iqT = sbuf(ph, "b_iqT", [128, 4, S], BF16)
                      ikT = sbuf(ph, "b_ikT", [128, S], BF16)
                      iw = sbuf(ph, "b_iw", [128, NB, 8])
                      sqt = [sbuf(ph, "b_sqt%d" % i, [128, 512], BF16) for i in range(2)]
                      ckn = [sbuf(ph, "b_ckn%d" % i, [128, 128], BF16) for i in range(2)]
                      cst = [sbuf(ph, "b_cst%d" % i, [128, 2]) for i in range(2)]
                      cjunk = sbuf(ph, "b_cjunk", [128, 128], BF16)
                      score = [sbuf(ph, "b_score%d" % i, [128, S]) for i in range(4)]
                      negm = [sbuf(ph, "b_negm%d" % i, [128, S], BF16) for i in range(8)]
                      rel = [sbuf(ph, "b_rel%d" % i, [128, 512], BF16) for i in range(4)]
                      dg = [sbuf(ph, "b_dg%d" % i, [128, 8, 128], BF16) for i in range(2)]
                      bis = [sbuf(ph, "b_bis%d" % i, [128, 8]) for i in range(4)]
                      bjunk = sbuf(ph, "b_bjunk", [128, S], BF16)
                      pt = [sbuf(ph, "b_pt%d" % i, [128, 512], BF16) for i in range(4)]
                      acs = [sbuf(ph, "b_acs%d" % i, [128, 4, 65]) for i in range(2)]
                      acsrot = Rot(range(2))
                      rrp = [sbuf(ph, "b_rrp%d" % i, [128, 4, 1]) for i in range(2)]
                      negone = sbuf(ph, "b_negone", [128, 4, 1])
                      memset("pool", negone[:, :, :], -1.0, ["negone"])
                      ob = [sbuf(ph, "b_ob%d" % i, [128, 4, 384], BF16) for i in range(2)]

                      dma("pool", wTt[:, :, :], wT_d[l, :, :, T_CKV[0]:T_CKV[0] + 256], [], ["b_wT"])
                      wuk_b = sbuf(ph, "b_wuk", [128, 1, 3, 128], BF16)
                      wuv_b = sbuf(ph, "b_wuv", [128, 1, 384], BF16)
                      dma("pool", wuk_b[:, 0, :, :], wuk_d[l, :, :, :], [], ["wuk_b"])
                      dma("pool", wuv_b[:, 0, :], wuv_d[l, :, :], [], ["wuv_b"])
                      memset("pool", vpa[:, :, :, 64:65], 1.0, ["vpa"])
                      sqrot = Rot(range(2))
                      for c in range(3):
                          def ev_sq(tg, pa, b, c=c):
                              si = sqrot.next()
                              cp(evrot.next(), sqt[si][:, :], pa, [pk(b)], [("sqt", si)])
                              for hl in range(2):
                                  b2 = brotP.next()
                                  mm(ps[b2][:, :], wuk_b[hl * 64:(hl + 1) * 64, 0, c, :], sqt[si][hl * 64:(hl + 1) * 64, :], True, True,
                                     ["wuk_b", ("sqt", si)], [pk(b2)])
                                  cp(evrot.next(), qlT[:, 2 * c + hl, tg * 512:(tg + 1) * 512], ps[b2][:, :], [pk(b2)], ["qlT"])
                          if '1' not in _SKIP:
                              proj_F(wFt, 6 + c, ev_sq)
                      for c in range(4):
                          def ev_iq(tg, pa, b, c=c):
                              cp(evrot.next(), iqT[:, c, tg * 512:(tg + 1) * 512], pa, [pk(b)], ["iqT"])
                          if '2' not in _SKIP:
                              proj_F(wFt, 9 + c, ev_iq)

                      def ev_ik(tg, pa, b):
                          cp(evrot.next(), ikT[:, tg * 512:(tg + 1) * 512], pa, [pk(b)], ["ikT"])
                      if '3' not in _SKIP:
                          proj_F(wFt, 13, ev_ik)
                      for t in range(NB if '4' not in _SKIP else 0):
                          i = t % 2
                          b = proj_T(wTt, "b_wT", 136, t)
                          cp("dve", iw[:, t, :], ps[b][:, 128:136], [pk(b)], ["iw"])
                          act(cjunk[:, :], ps[b][:, 0:128], AF.Square, [pk(b)], ["cjunk", ("cst", i)], accum_out=cst[i][:, 0:1])
                          rsqrt_mean("dve", cst[i][:, 1:2], cst[i][:, 0:1], 128, ("cst", i), ("cst", i))
                          ts("dve", ckn[i][:, :], ps[b][:, 0:128], cst[i][:, 1:2], None, ALU.mult, None, [pk(b), ("cst", i)], [("ckn", i)])
                          b2 = brotP.next()
                          pv = ps[b2][:, :].bitcast(BF16)
                          tr(pv[:, 0:128], ckn[i][:, :], [("ckn", i)], [pk(b2)])
                          act(ckvT[:, t * 128:(t + 1) * 128], pv[:, 0:128], AF.Copy, [pk(b2), "gkv"], ["ckvT"], scale=gkv[:, l, :])
                          b3 = brotP.next()
                          mm(ps[b3][:, 0:384], ckvT[:, t * 128:(t + 1) * 128], wuv_b[:, 0, :], True, True, ["ckvT", "wuv_b"], [pk(b3)])
                          cp(evrot.next(), vpa[:, t, :, 0:64], ps[b3][:, 0:384].rearrange("p (h e) -> p h e", e=64), [pk(b3)], ["vpa"])

                      relbank = Rot([0, 1])
                      idxacc = Rot([2, 3])
                      relrot = Rot(range(4))
                      scrot = Rot([4, 5])
                      accrot = Rot([6, 7])
                      ptrot = Rot(range(4))
                      scale = 64.0 ** -0.5

                      def index_phase(I):
                          par = I % 2
                          chains = []
                          for i4 in range(4):
                              i = 4 * I + i4
                              n = (i + 1) * 128
                              sk = ("score", i4)
                              bk = ("bis", i4)
                              bs = bis[i4]
                              if i < 2 or 'i' in _SKIP:
                                  memset("dve", score[i4][:, 0:n], 0.0, [sk])
                                  tt("dve", score[i4][:, n - 128:n], score[i4][:, n - 128:n], cneg[:, :], ALU.add, [sk, "cneg"], [sk])
                                  memset("dve", bs[:, 0:1], -1e29, [bk])
                              else:
                                  dgi = i % 2
                                  for hh in range(8):
                                      ts("pool", dg[dgi][:, hh, :], ident_b[:, :], iw[:, i, hh:hh + 1], None, ALU.mult, None, ["ident_b", "iw"], [("dg", dgi)])
                                  for kc in range((n + 511) // 512):
                                      w = min(512, n - kc * 512)
                                      ab = idxacc.next()
                                      pend = None
                                      for hh in range(9):
                                          if hh < 8:
                                              rb = relbank.next()
                                              r0 = (hh % 2) * 64
                                              mm(ps[rb][:, 0:w], iqT[r0:r0 + 64, hh // 2, i * 128:(i + 1) * 128], ikT[r0:r0 + 64, kc * 512:kc * 512 + w],
                                                 True, True, ["iqT", "ikT"], [pk(rb)])
                                              ri = relrot.next()
                                              act(rel[ri][:, 0:w], ps[rb][:, 0:w], AF.Relu, [pk(rb)], [("rel", ri)])
                                          if pend is not None:
                                              ph_, pri = pend
                                              mm(ps[ab][:, 0:w], dg[dgi][:, ph_, :], rel[pri][:, 0:w], ph_ == 0, ph_ == 7, [("dg", dgi), ("rel", pri)], [pk(ab)])
                                          pend = (hh, ri) if hh < 8 else None
                                      cp("act", score[i4][:, kc * 512:kc * 512 + w], ps[ab][:, 0:w], [pk(ab)], [sk])
                                  tt("dve", score[i4][:, n - 128:n], score[i4][:, n - 128:n], cneg[:, :], ALU.add, [sk, "cneg"], [sk])
                                  reduce(bs[:, 5:6], score[i4][:, 0:n], ALU.max, [sk], [bk])
                                  reduce(bs[:, 0:1], score[i4][:, 0:n - 128], ALU.min, [sk, bk], [bk])
                                  tt("dve", bs[:, 1:2], bs[:, 5:6], bs[:, 0:1], ALU.subtract, [bk], [bk])
                                  chains.append((i4, n, sk, bk, bs))
                          for it in range(NBIS):
                              for (i4, n, sk, bk, bs) in chains:
                                  ts("dve", bs[:, 2:3], bs[:, 1:2], 0.5 ** (it + 1), bs[:, 0:1], ALU.mult, ALU.add, [bk], [bk])
                              for (i4, n, sk, bk, bs) in chains:
                                  ts("dve", bjunk[:, 0:n], score[i4][:, 0:n], bs[:, 2:3], 0.0, ALU.is_ge, ALU.add, [sk, bk], ["bjunk", bk], accum_out=bs[:, 3:4])
                              for (i4, n, sk, bk, bs) in chains:
                                  ts("dve", bs[:, 4:5], bs[:, 3:4], 255.5, 1e30, ALU.is_lt, ALU.mult, [bk], [bk])
                              for (i4, n, sk, bk, bs) in chains:
                                  stt("dve", bs[:, 0:1], bs[:, 2:3], bs[:, 4:5], bs[:, 0:1], ALU.subtract, ALU.max, [bk], [bk])
                          for i4 in range(4):
                              n = (4 * I + i4 + 1) * 128
                              ts("dve", negm[par * 4 + i4][:, 0:n], score[i4][:, 0:n], bis[i4][:, 0:1], NEGM, ALU.is_lt, ALU.mult,
                                 [("score", i4), ("bis", i4)], [("negm", par * 4 + i4)])

                      def d_epilogue(I, h, ab):
                          obb = ob[I % 2]
                          obk = ("ob", I % 2)
                          a0 = ps[ab][:, 0:260].rearrange("p (i e) -> p i e", e=65)
                          ai = acsrot.next()
                          cp("act", acs[ai][:, :, :], a0, [pk(ab)], [("acs", ai)])
                          tt("pool", rrp[ai][:, :, :], acs[ai][:, :, 64:65], negone[:, :, :], ALU.pow, [("acs", ai), "negone"], [("rrp", ai)])
                          tt("pool", obb[:, :, h * 64:(h + 1) * 64], acs[ai][:, :, 0:64], rrp[ai][:, :, :].to_broadcast([128, 4, 64]), ALU.mult,
                             [("acs", ai), ("rrp", ai)], [obk])
                          if h == 5:
                              for i4 in range(4):
                                  t = 4 * I + i4
                                  dma("sp", mixed_d[s, t * 128:(t + 1) * 128, 384:768], obb[:, i4, :], [obk], [("mx", s, t, 1)])

                      def d_score(T):
                          I, h, j = T["I"], T["h"], T["j"]
                          par = I % 2
                          i0 = max(j, 4 * I)
                          w = (4 * I + 4 - i0) * 128
                          sb_ = scrot.next()
                          pi = ptrot.next()
                          T.update(i0=i0, w=w, pi=pi)
                          mm(ps[sb_][:, 0:w], ckvT[:, j * 128:(j + 1) * 128], qlT[:, h, i0 * 128:(4 * I + 4) * 128], True, False,
                             ["ckvT", "qlT"], [pk(sb_)])
                          for i in range(i0, 4 * I + 4):
                              i4 = i - 4 * I
                              mm(ps[sb_][:, (i - i0) * 128:(i - i0 + 1) * 128], negm[par * 4 + i4][:, j * 128:(j + 1) * 128], ident_b[:, :], False,
                                 i == 4 * I + 3, [("negm", par * 4 + i4), "ident_b"], [pk(sb_)])
                          act(pt[pi][:, 0:w], ps[sb_][:, 0:w], AF.Exp, [pk(sb_)], [("pt", pi)], scale=scale)

                      def d_av(T):
                          I, h, j, i0, pi, ab = T["I"], T["h"], T["j"], T["i0"], T["pi"], T["ab"]
                          for i in range(i0, 4 * I + 4):
                              mm(ps[ab][:, (i - 4 * I) * 65:(i - 4 * I) * 65 + 65], pt[pi][:, (i - i0) * 128:(i - i0 + 1) * 128],
                                 vpa[:, j, h, :], T["first"] and i == i0, j == i, [("pt", pi), "vpa"], [pk(ab)])
                          if T["last"]:
                              d_epilogue(I, h, ab)

                      def dsa_phase(I):
                          tiles = []
                          for h in range(6):
                              ab = accrot.next()
                              nj = 4 * I + 4
                              for j in range(nj):
                                  tiles.append(dict(I=I, h=h, j=j, ab=ab, first=(j == 0), last=(j == nj - 1)))
                          DEP = 1
                          for idx in range(len(tiles) + DEP):
                              if idx < len(tiles):
                                  d_score(tiles[idx])
                              if idx >= DEP:
                                  d_av(tiles[idx - DEP])

                      index_phase(0)
                      for I in range(4):
                          if I + 1 < 4:
                              index_phase(I + 1)
                          dsa_phase(I)
                P.barrier()

                with ExitStack() as ph:
                  if "C" in _SKIP:
                    pass
                  else:
                      brotP = Rot(range(8))
                      wFt = [sbuf(ph, "c_wF%d" % i, [128, 8, 128], BF16) for i in range(3)]
                      wTt = sbuf(ph, "c_wT", [128, 8, 256], BF16)
                      rT = [sbuf(ph, "c_rqT", [128, 2, S], BF16), sbuf(ph, "c_rkT", [128, 2, S], BF16)]
                      rv = sbuf(ph, "c_rv", [128, NB, 256], BF16)
                      t1 = sbuf(ph, "c_t1", [128, 2, S])
                      t2 = [sbuf(ph, "c_t2_%d" % i, [128, 512]) for i in range(2)]
                      kz = [sbuf(ph, "c_kz%d" % i, [128, 128], BF16) for i in range(2)]
                      qxi = [sbuf(ph, "c_qxi%d" % i, [128, 128], BF16) for i in range(2)]
                      attD = [sbuf(ph, "c_attD%d" % i, [128, 128], BF16) for i in range(4)]
                      Rf = sbuf(ph, "c_Rf", [128, 2, 64])
                      Rb = sbuf(ph, "c_Rb", [128, 2, 64], BF16)
                      oc = [sbuf(ph, "c_oc%d" % i, [128, 4, 64]) for i in range(2)]
                      ocs = sbuf(ph, "c_ocs", [128, 4, 64])
                      ocb = [sbuf(ph, "c_ocb%d" % i, [128, 4, 64], BF16) for i in range(2)]
                      ss4 = sbuf(ph, "c_ss4", [128, 4, 1])

                      dma("pool", wTt[:, :, :], wT_d[l, :, :, T_RV[0]:T_RV[1]], [], ["c_wT"])
                      cosT = sbuf(ph, "c_cosT", [128, S])
                      sinT = sbuf(ph, "c_sinT", [128, S])
                      dma("sp", cosT[:, :], cd["cosT"][:, :], [], ["cosT"])
                      dma("sp", sinT[:, :], cd["sinT"][:, :], [], ["sinT"])
                      for qk in range(2):
                          for c in range(2):
                              def ev_x(tg, pa, b, c=c, qk=qk):
                                  i = tg % 2
                                  tt("dve", t1[:, c, tg * 512:(tg + 1) * 512], pa, cosT[:, tg * 512:(tg + 1) * 512], ALU.mult, [pk(b), "cosT"], [("t1", tg, c)])
                              proj_F(wFt, 14 + 4 * qk + c, ev_x)
                          for c in range(2):
                              def ev_xs(tg, pa, b, c=c, qk=qk):
                                  i = tg % 2
                                  tt("dve", t2[i][:, :], pa, sinT[:, tg * 512:(tg + 1) * 512], ALU.mult, [pk(b), "sinT"], [("t2", i)])
                                  tt("pool", rT[qk][:, c, tg * 512:(tg + 1) * 512], t1[:, c, tg * 512:(tg + 1) * 512], t2[i][:, :], ALU.add, [("t1", tg, c), ("t2", i)], [("rT", qk)])
                              proj_F(wFt, 16 + 4 * qk + c, ev_xs)
                      for t in range(NB):
                          b = proj_T(wTt, "c_wT", 256, t)
                          cp(evrot.next(), rv[:, t, :], ps[b][:, 0:256], [pk(b)], ["rv"])

                      memset("dve", Rf[:, :, :], 0.0, ["Rf"])
                      memset("dve", Rb[:, :, :], 0.0, ["Rb"])
                      brot = Rot([0, 1, 2, 3])
                      obrot = Rot([4, 5])
                      rbrot = Rot([6, 7])
                      adrot = Rot(range(4))
                      for t in range(NB):
                          tsl = slice(t * 128, (t + 1) * 128)
                          ob_ = obrot.next()
                          first = True
                          rbanks = []
                          for c in range(2):
                              b = brot.next()
                              pv = ps[b][:, :].bitcast(BF16)
                              tr(pv[:, 0:128], rT[1][:, c, tsl], [("rT", 1)], [pk(b)])
                              tt("dve", kz[c][:, :], pv[:, 0:128], zeta[:, c, :], ALU.mult, [pk(b), "zeta"], [("kz", c)])
                              tt("pool", qxi[c][:, :], rT[0][:, c, tsl], xiT[:, c, :], ALU.mult, [("rT", 0), "xiT"], [("qxi", c)])
                              for hl in range(2):
                                  h = 2 * c + hl
                                  rows = slice(hl * 64, (hl + 1) * 64)
                                  b = brot.next()
                                  mm(ps[b][:, 0:128], rT[1][rows, c, tsl], rT[0][rows, c, tsl], True, True, [("rT", 1), ("rT", 0)], [pk(b)])
                                  ai = adrot.next()
                                  tt("dve", attD[ai][:, :], ps[b][:, 0:128], dintraT[:, h, :], ALU.mult, [pk(b), "dintraT"], [("attD", ai)])
                                  mm(ps[ob_][:, h * 64:(h + 1) * 64], attD[ai][:, :], rv[:, t, h * 64:(h + 1) * 64], first, t == 0,
                                     [("attD", ai), "rv"], [pk(ob_)])
                                  first = False
                                  if t > 0:
                                      mm(ps[ob_][:, h * 64:(h + 1) * 64], qxi[c][rows, :], Rb[rows, c, :], False, True,
                                         [("qxi", c), "Rb"], [pk(ob_)])
                              if t < NB - 1:
                                  rb_ = rbrot.next()
                                  mm(ps[rb_][:, 0:128], kz[c][:, :], rv[:, t, c * 128:(c + 1) * 128], True, True, [("kz", c), "rv"], [pk(rb_)])
                                  rbanks.append(rb_)
                          if t < NB - 1:
                              for c in range(2):
                                  for hl in range(2):
                                      rows = slice(hl * 64, (hl + 1) * 64)
                                      stt("dve", Rf[rows, c, :], Rf[rows, c, :], gch[rows, c:c + 1], ps[rbanks[c]][rows, hl * 64:(hl + 1) * 64],
                                          ALU.mult, ALU.add, ["Rf", "gch", pk(rbanks[c])], ["Rf"])
                              cp("pool", Rb[:, :, :], Rf[:, :, :], ["Rf"], ["Rb"])
                          oi = t % 2
                          cp("act", oc[oi][:, :, :], ps[ob_][:, 0:256].rearrange("p (h e) -> p h e", e=64), [pk(ob_)], [("oc", oi)])
                          tt("pool", ocs[:, :, :], oc[oi][:, :, :], oc[oi][:, :, :], ALU.mult, [("oc", oi)], ["ocs"])
                          reduce(ss4[:, :, :], ocs[:, :, :], ALU.add, ["ocs"], ["c_ss4"])
                          rsqrt_mean("dve", ss4[:, :, :], ss4[:, :, :], 64, "c_ss4", "c_ss4")
                          tt("pool", oc[oi][:, :, :], oc[oi][:, :, :], ss4[:, :, :].to_broadcast([128, 4, 64]), ALU.mult, [("oc", oi), "c_ss4"], [("oc", oi)])
                          tt("pool", ocb[oi][:, :, :], oc[oi][:, :, :], gret[:, l:l + 1, :].to_broadcast([128, 4, 64]), ALU.mult, [("oc", oi), "gret"], [("ocb", oi)])
                          dma("sp", mixed_d[s, tsl, 768:1024], ocb[oi][:, :, :].rearrange("p h e -> p (h e)"), [("ocb", oi)], [("mx", s, t, 2)])
                P.barrier()

                with ExitStack() as ph:
                  if "D" in _SKIP:
                    pass
                  else:
                      wg = sbuf(ph, "d_wg", [128, 8, D], BF16)
                      wo = sbuf(ph, "d_wo", [128, 8, D], BF16)
                      hb = [sbuf(ph, "d_h%d" % i, [128, D]) for i in range(2)]
                      mx = [sbuf(ph, "d_mx%d" % i, [128, D], BF16) for i in range(2)]
                      sg = [sbuf(ph, "d_sg%d" % i, [128, D], BF16) for i in range(2)]
                      yb = [sbuf(ph, "d_y%d" % i, [128, D], BF16) for i in range(2)]
                      yT = [sbuf(ph, "d_yT%d" % i, [128, 8, 128], BF16) for i in range(2)]
                      hn = [sbuf(ph, "d_hn%d" % i, [128, D]) for i in range(2)]
                      fo = [sbuf(ph, "d_fo%d" % i, [128, D]) for i in range(2)]
                      fjunk = sbuf(ph, "d_fjunk", [128, D], BF16)
                      if last_layer:
                          gfin = sbuf(ph, "d_gfin", [128, D])
                          dma("sp", gfin[:, :], gfin_d.partition_broadcast(128), [], ["gfin"])
                      fst = [sbuf(ph, "d_fst%d" % i, [128, 2]) for i in range(2)]
                      for kh in range(2):
                          dma("pool", wg[:, :, kh * 512:(kh + 1) * 512], wT_d[l, :, :, T_GATE[0] + kh * 512:T_GATE[0] + (kh + 1) * 512], [], ["d_wg"])
                          dma("pool", wo[:, :, kh * 512:(kh + 1) * 512], wout_d[l, :, :, kh * 512:(kh + 1) * 512], [], ["d_wo"])
                      gb = Rot([0, 1, 2, 3])
                      tb = Rot([4, 5])
                      ob2 = Rot([6, 7])
                      hb3 = hb + [sbuf(ph, "d_h2", [128, D])]

                      def d_gate(t):
                          i = t % 2
                          tsl = slice(t * 128, (t + 1) * 128)
                          dma("sp", hb3[t % 3][:, :], h_src(l, s, t), [hkey(s, t)], [("dhb", t % 3)])
                          dma("sp", mx[i][:, :], mixed_d[s, tsl, :], [("mx", s, t, 0), ("mx", s, t, 1), ("mx", s, t, 2)], [("dmx", i)])
                          for kh in range(2):
                              b = gb.next()
                              for k in range(8):
                                  mm(ps[b][:, :], uT[:, k, tsl], wg[:, k, kh * 512:(kh + 1) * 512], k == 0, k == 7, ["uT", "d_wg"], [pk(b)])
                              act(sg[i][:, kh * 512:(kh + 1) * 512], ps[b][:, :], AF.Silu, [pk(b)], [("sg", i)])
                          tt("dve", yb[i][:, :], sg[i][:, :], mx[i][:, :], ALU.mult, [("sg", i), ("dmx", i)], [("yb", i)])

                      def d_tr(t):
                          i = t % 2
                          b = tb.next()
                          pv = ps[b][:, :].bitcast(BF16)
                          for k in range(8):
                              tr(pv[:, k * 128:(k + 1) * 128], yb[i][:, k * 128:(k + 1) * 128], [("yb", i)], [pk(b)])
                          cp("act", yT[i][:, :, :], pv[:, :].rearrange("p (k q) -> p k q", q=128), [pk(b)], [("yT", i)])

                      def d_out(t):
                          i = t % 2
                          tsl = slice(t * 128, (t + 1) * 128)
                          for kh in range(2):
                              b = ob2.next()
                              for k in range(8):
                                  mm(ps[b][:, :], yT[i][:, k, :], wo[:, k, kh * 512:(kh + 1) * 512], k == 0, k == 7, [("yT", i), "d_wo"], [pk(b)])
                              tt("dve", hn[i][:, kh * 512:(kh + 1) * 512], ps[b][:, :], hb3[t % 3][:, kh * 512:(kh + 1) * 512], ALU.add, [pk(b), ("dhb", t % 3)], [("dhn", i)])
                          if not last_layer:
                              dma("sp", hbuf_d[s, tsl, :], hn[i][:, :], [("dhn", i)], [hkey(s, t)])
                          else:
                              act(fjunk[:, :], hn[i][:, :], AF.Square, [("dhn", i)], ["fjunk", ("fst", i)], accum_out=fst[i][:, 0:1])
                              rsqrt_mean("dve", fst[i][:, 1:2], fst[i][:, 0:1], D, ("fst", i), ("fst", i))
                              stt("dve", fo[i][:, :], hn[i][:, :], fst[i][:, 1:2], gfin[:, :], ALU.mult, ALU.mult, [("dhn", i), ("fst", i), "gfin"], [("fo", i)])
                              dma("sp", out_d[s, tsl, :], fo[i][:, :], [("fo", i)], [("out", s, t)])

                      for t in range(NB + 2):
                          if t < NB:
                              d_gate(t)
                          if 1 <= t <= NB:
                              d_tr(t - 1)
                          if t >= 2:
                              d_out(t - 2)
                P.barrier()
        P.barrier()
        P.emit()
    return nc


_PROG_CACHE = {}


def kernel(x, attn_norm, w_in, diff_lambda, diff_norm, kv_norm, w_uk, w_uv, ret_norm, w_out, final_norm):
    x = np.asarray(x, dtype=np.float32)
    args = [np.asarray(a, dtype=np.float32) for a in (attn_norm, w_in, diff_lambda, diff_norm, kv_norm, w_uk, w_uv, ret_norm, w_out, final_norm)]
    w = _host_weights(*args)
    consts = _host_consts()
    B = x.shape[0]
    ns = B // NCORES
    if "nc" not in _PROG_CACHE:
        _PROG_CACHE["nc"] = build_program(DEPTH, ns)
    nc = _PROG_CACHE["nc"]
    in_maps = []
    for c in range(NCORES):
        m = {"x": np.ascontiguousarray(x[c * ns:(c + 1) * ns])}
        m.update(w)
        for k, v in consts.items():
            m["c_" + k] = v
        in_maps.append(m)
    res = run_bass_kernel_spmd(nc, in_maps, core_ids=list(range(NCORES)))
    return np.concatenate([r["out"] for r in res.results], axis=0)
```

```python
import math
import numpy as np
from contextlib import ExitStack
import concourse.bass as bass
import concourse.mybir as mybir
from concourse.bass_utils import run_bass_kernel_spmd

F32 = mybir.dt.float32
BF16 = mybir.dt.bfloat16
ALU = mybir.AluOpType
AF = mybir.ActivationFunctionType
AX = mybir.AxisListType

S = 2048
D = 1024
NB = S // 128
DEPTH = 4
NCORES = 8
EPS = 1e-6
NBIS = 14
NEGM = -30000.0
import os
_SKIP = os.environ.get('KSKIP', '')


class Prog:
    ENG = ("pe", "act", "dve", "pool", "sp")
    NDS = 12

    def __init__(self, nc, stack):
        self.nc = nc
        self.q = {e: [] for e in self.ENG}
        self.cnt = {e: 0 for e in self.ENG}
        self.sem = {e: stack.enter_context(nc.semaphore("prog_" + e)) for e in self.ENG}
        self.seen = {e: {f: 0 for f in self.ENG} for e in self.ENG}
        self.dseen = {e: {} for e in self.ENG}
        self.lastw = {}
        self.readers = {}
        self.dsem = {}
        self.drot = {}
        for qn in ("sp", "pool"):
            for i in range(self.NDS):
                nm = "%s%d" % (qn, i)
                self.dsem[nm] = [stack.enter_context(nc.semaphore("d_" + nm)), 0]
            self.drot[qn] = 0

    def _need(self, eng, dep, waits):
        if dep is None:
            return
        if dep[0] == "e":
            _, f, idx = dep
            if self.seen[eng][f] < idx:
                self.seen[eng][f] = idx
                waits[("e", f)] = (self.sem[f], idx)
        else:
            _, name, val = dep
            if self.dseen[eng].get(name, 0) < val:
                self.dseen[eng][name] = val
                waits[("d", name)] = (self.dsem[name][0], val)

    def _deps(self, eng, reads, writes):
        waits = {}
        for k in reads:
            self._need(eng, self.lastw.get(k), waits)
        for k in writes:
            lw = self.lastw.get(k)
            if lw is not None and not (lw[0] == "e" and lw[1] == eng):
                self._need(eng, lw, waits)
            for dep in self.readers.get(k, {}).values():
                if not (dep[0] == "e" and dep[1] == eng):
                    self._need(eng, dep, waits)
        return list(waits.values())

    def op(self, eng, fn, reads=(), writes=()):
        writes = list(writes) + [k for k in reads if isinstance(k, tuple) and k[0] == "ps" and k not in writes]
        waits = self._deps(eng, reads, writes)
        self.cnt[eng] += 1
        idx = self.cnt[eng]
        self.q[eng].append((waits, fn, (self.sem[eng], 1)))
        dep = ("e", eng, idx)
        for k in reads:
            self.readers.setdefault(k, {})[eng] = dep
        for k in writes:
            self.lastw[k] = dep
            self.readers[k] = {}
        return dep

    def dma(self, queue, fn, reads=(), writes=()):
        name = "%s%d" % (queue, self.drot[queue])
        self.drot[queue] = (self.drot[queue] + 1) % self.NDS
        waits = {}
        self._need(queue, ("d", name, self.dsem[name][1]), waits) if self.dsem[name][1] else None
        w2 = self._deps(queue, reads, writes)
        allw = list(waits.values()) + w2
        self.dsem[name][1] += 16
        val = self.dsem[name][1]
        self.q[queue].append((allw, fn, (self.dsem[name][0], 16)))
        dep = ("d", name, val)
        for k in writes:
            self.lastw[k] = dep
            self.readers[k] = {}
        for k in reads:
            self.readers.setdefault(k, {})["dma:" + name] = dep
        return dep

    def barrier(self):
        for e in self.ENG:
            waits = {}
            for f in self.ENG:
                if f != e and self.cnt[f]:
                    self._need(e, ("e", f, self.cnt[f]), waits)
            for name, (h, val) in self.dsem.items():
                if val:
                    self._need(e, ("d", name, val), waits)
            self.q[e].append((list(waits.values()), None, None))

    def emit(self):
        nc = self.nc
        q = self.q
        with nc.Block() as block:
            def replay(name):
                def run(e):
                    for waits, fn, inc in q[name]:
                        for s, v in waits:
                            e.wait_ge(s, v)
                        if fn is not None:
                            fn(e).then_inc(inc[0], inc[1])
                return run
            block.sync(replay("sp"))
            block.tensor(replay("pe"))
            block.scalar(replay("act"))
            block.vector(replay("dve"))
            block.gpsimd(replay("pool"))


class Rot:
    def __init__(self, items):
        self.items = list(items)
        self.i = 0

    def next(self):
        v = self.items[self.i % len(self.items)]
        self.i += 1
        return v


def _col_maps():
    o_dq, o_dk, o_dv, o_sq, o_ckv, o_iq, o_ik, o_iw, o_rq, o_rk, o_rv, o_gate = (
        0, 384, 768, 1152, 1536, 1664, 2176, 2240, 2248, 2504, 2760, 3016)
    colsF = []
    for base in (o_dq, o_dk, o_sq):
        for c in range(3):
            colsF.append(np.arange(base + 128 * c, base + 128 * c + 128))
    for c in range(4):
        colsF.append(np.arange(o_iq + 128 * c, o_iq + 128 * c + 128))
    colsF.append(np.concatenate([np.arange(o_ik, o_ik + 64)] * 2))

    def swap(base, c):
        out = []
        for hl in range(2):
            b = base + 128 * c + 64 * hl
            out += [np.arange(b + 32, b + 64), np.arange(b, b + 32)]
        return np.concatenate(out)
    for base in (o_rq, o_rk):
        for c in range(2):
            colsF.append(np.arange(base + 128 * c, base + 128 * c + 128))
        for c in range(2):
            colsF.append(swap(base, c))
    colsF = np.concatenate(colsF)
    colsT = np.concatenate([np.arange(o_dv, o_dv + 384), np.arange(o_ckv, o_ckv + 128),
                            np.arange(o_iw, o_iw + 8), np.arange(o_rv, o_rv + 256),
                            np.arange(o_gate, o_gate + 1024)])
    return colsF, colsT


NF = 22
T_DV = (0, 384)
T_CKV = (384, 520)
T_RV = (520, 776)
T_GATE = (776, 1800)
NT = 1800


def _host_consts():
    f32 = np.float32
    c = {}
    c["ident"] = np.eye(128, dtype=f32)
    r = np.arange(128)
    c["tri"] = (r[None, :] >= r[:, None]).astype(f32)
    c["cneg"] = np.where(r[None, :] <= r[:, None], 0.0, -1e30).astype(f32)
    inv_freq = (f32(10000.0) ** (-(np.arange(32, dtype=f32)) / f32(32))).astype(f32)
    ang = (np.arange(S, dtype=f32)[:, None] * inv_freq[None, :]).astype(f32)
    cos, sin = np.cos(ang).astype(f32), np.sin(ang).astype(f32)
    rr = np.arange(128)
    cosT = cos[:, rr % 32].T.copy()
    sgn = np.where((rr % 64) < 32, -1.0, 1.0).astype(f32)
    sinT = (sin[:, rr % 32].T * sgn[:, None]).astype(f32)
    c["cosT"], c["sinT"] = np.ascontiguousarray(cosT), np.ascontiguousarray(sinT)
    H = 4
    log_g = np.log(f32(1.0) - f32(2.0) ** (f32(-5.0) - np.arange(H, dtype=f32))).astype(f32)
    pos = np.arange(128, dtype=f32)
    diff = pos[:, None] - pos[None, :]
    d_intra = np.where(diff >= 0, np.exp(np.maximum(diff, 0.0)[None] * log_g[:, None, None]), 0.0).astype(f32)
    xi = np.exp((pos + 1.0)[:, None] * log_g[None, :]).astype(f32)
    zeta = np.exp((127.0 - pos)[:, None] * log_g[None, :]).astype(f32)
    gch = np.exp(128.0 * log_g).astype(f32)
    sc = f32(64.0 ** -0.5)
    c["dintraT"] = np.ascontiguousarray(np.transpose(d_intra, (2, 0, 1)) * sc).astype(f32)
    xiT = np.zeros((128, 2, 128), f32)
    zt = np.zeros((128, 2, 128), f32)
    gc = np.zeros((128, 2), f32)
    for cc in range(2):
        for hl in range(2):
            h = 2 * cc + hl
            xiT[hl * 64:(hl + 1) * 64, cc, :] = xi[None, :, h]
            zt[:, cc, hl * 64:(hl + 1) * 64] = (zeta[:, h] * sc)[:, None]
            gc[hl * 64:(hl + 1) * 64, cc] = gch[h]
    c["xiT"], c["zeta"], c["gch"] = xiT, zt, gc
    return c


_CONST_SHAPES = {"ident": [128, 128], "tri": [128, 128], "cneg": [128, 128], "cosT": [128, S], "sinT": [128, S],
                 "dintraT": [128, 4, 128], "xiT": [128, 2, 128], "zeta": [128, 2, 128], "gch": [128, 2]}


def _host_weights(attn_norm, w_in, diff_lambda, diff_norm, kv_norm, w_uk, w_uv, ret_norm, w_out, final_norm):
    L = w_in.shape[0]
    colsF, colsT = _col_maps()
    w = {}
    wf = w_in[:, :, colsF].reshape(L, 8, 128, NF, 128)
    w["wF"] = np.ascontiguousarray(np.transpose(wf, (0, 3, 2, 1, 4)))
    wt = w_in[:, :, colsT].reshape(L, 8, 128, NT)
    w["wT"] = np.ascontiguousarray(np.transpose(wt, (0, 2, 1, 3)))
    w["wout"] = np.ascontiguousarray(np.transpose(w_out.reshape(L, 8, 128, D), (0, 2, 1, 3)))
    uk = w_uk.reshape(L, 3, 2, 64, 128)
    w["wuk"] = np.ascontiguousarray(np.transpose(uk, (0, 2, 3, 1, 4)).reshape(L, 128, 3, 128))
    w["wuv"] = np.ascontiguousarray(np.transpose(w_uv, (0, 2, 1, 3)).reshape(L, 128, 384))
    w["gattn"] = np.ascontiguousarray(np.transpose(attn_norm.reshape(L, 8, 128), (0, 2, 1)))
    w["gkv"] = np.ascontiguousarray(kv_norm.reshape(L, 128, 1))
    w["gdiff"] = np.ascontiguousarray(diff_norm)
    w["gret"] = np.ascontiguousarray(ret_norm)
    w["gfin"] = np.ascontiguousarray(final_norm)
    w["dlam"] = np.ascontiguousarray(diff_lambda.reshape(L, 128))
    return w


def build_program(NL=DEPTH, NS=2, debug=False):
    nc = bass.Bass("TRN2", target_bir_lowering=False)
    L = NL
    dt = lambda name, shape, dtype=F32, kind="ExternalInput": nc.dram_tensor(name, shape, dtype, kind=kind).ap()
    x_d = dt("x", [NS, S, D])
    wF_d = dt("wF", [L, NF, 128, 8, 128])
    wT_d = dt("wT", [L, 128, 8, NT])
    wout_d = dt("wout", [L, 128, 8, D])
    wuk_d = dt("wuk", [L, 128, 3, 128])
    wuv_d = dt("wuv", [L, 128, 384])
    gattn_d = dt("gattn", [L, 128, 8])
    gkv_d = dt("gkv", [L, 128, 1])
    gdiff_d = dt("gdiff", [L, 64])
    gret_d = dt("gret", [L, 64])
    gfin_d = dt("gfin", [D])
    dlam_d = dt("dlam", [L, 128])
    cd = {k: dt("c_" + k, shp) for k, shp in _CONST_SHAPES.items()}
    out_d = dt("out", [NS, S, D], F32, "ExternalOutput")
    hbuf_d = dt("hbuf", [NS, S, D], F32, "Internal")
    mixed_d = dt("mixed", [NS, S, D], BF16, "Internal")
    dbg_d = dt("dbg", [NS, S, D], F32, "ExternalOutput") if debug else None

    with ExitStack() as st:
        P = Prog(nc, st)
        _uid = [0]

        def sbuf(stack, name, shape, dtype=F32):
            _uid[0] += 1
            return stack.enter_context(nc.sbuf_tensor("s%d_%s" % (_uid[0], name), shape, dtype))
        ps = [st.enter_context(nc.psum_tensor("ps%d" % i, [128, 512], F32)) for i in range(8)]
        pk = lambda b: ("ps", b)

        def mm(out, lhsT, rhs, start, stop, reads, writes, **kw):
            P.op("pe", lambda e: e.matmul(out, lhsT=lhsT, rhs=rhs, start=start, stop=stop,
                                          skip_group_check=True, **kw), reads, writes)

        def tr(out, in_, reads, writes):
            P.op("pe", lambda e: e.transpose(out, in_, ident_b[:, :]), list(reads) + ["ident_b"], writes)

        def act(out, in_, func, reads, writes, **kw):
            P.op("act", lambda e: e.activation(out=out, in_=in_, func=func, **kw), reads, writes)

        def ts(eng, out, in0, s1, s2, op0, op1, reads, writes, **kw):
            if op1 is None:
                P.op(eng, lambda e: e.tensor_scalar(out, in0, s1, None, op0=op0, **kw), reads, writes)
            else:
                P.op(eng, lambda e: e.tensor_scalar(out, in0, s1, s2, op0=op0, op1=op1, **kw), reads, writes)

        def tt(eng, out, in0, in1, op, reads, writes):
            P.op(eng, lambda e: e.tensor_tensor(out, in0, in1, op=op), reads, writes)

        def stt(eng, out, in0, scalar, in1, op0, op1, reads, writes):
            P.op(eng, lambda e: e.scalar_tensor_tensor(out, in0, scalar, in1, op0=op0, op1=op1), reads, writes)

        def cp(eng, out, in_, reads, writes):
            if eng == "act":
                P.op("act", lambda e: e.copy(out, in_), reads, writes)
            else:
                P.op(eng, lambda e: e.tensor_copy(out, in_), reads, writes)

        def recip(out, in_, reads, writes):
            P.op("dve", lambda e: e.reciprocal(out, in_), reads, writes)

        def reduce(out, in_, op, reads, writes):
            P.op("dve", lambda e: e.tensor_reduce(out, in_, axis=AX.X, op=op), reads, writes)

        def memset(eng, ap, val, writes):
            P.op(eng, lambda e: e.memset(ap, val), [], writes)

        def dma(queue, out, in_, reads, writes):
            return P.dma(queue, lambda e: e.dma_start(out=out, in_=in_), reads, writes)

        def rsqrt_mean(eng_unused, out, ss, n, key_out, key_ss):
            ts("dve", out, ss, 1.0 / n, EPS, ALU.mult, ALU.add, [key_ss], [key_out])
            act(out, out, AF.Sqrt, [key_out], [key_out])
            P.op("dve", lambda e: e.reciprocal(out, out), [key_out], [key_out])

        ident_b = sbuf(st, "ident_b", [128, 128], BF16)
        tri_b = sbuf(st, "tri_b", [128, 128], BF16)
        cneg = sbuf(st, "cneg", [128, 128])
        dintraT = sbuf(st, "dintraT", [128, 4, 128])
        xiT = sbuf(st, "xiT", [128, 2, 128])
        zeta = sbuf(st, "zeta", [128, 2, 128])
        gch = sbuf(st, "gch", [128, 2])
        gattn = sbuf(st, "gattn", [128, L, 8])
        gkv = sbuf(st, "gkv", [128, L, 1])
        gdm = sbuf(st, "gdm", [128, L, 64])
        gret = sbuf(st, "gret", [128, L, 64])
        lamneg = sbuf(st, "lamneg", [128, L, 1])
        lamt = sbuf(st, "lamt", [128, 4])
        uT = sbuf(st, "uT", [128, 8, S], BF16)
        tmp0 = ExitStack()
        dlam = sbuf(tmp0, "dlam", [128, L, 128])

        dma("pool", ident_b[:, :], cd["ident"][:, :], [], ["ident_b"])
        dma("pool", tri_b[:, :], cd["tri"][:, :], [], ["tri_b"])
        for nm, t in (("cneg", cneg), ("gch", gch)):
            dma("sp", t[:, :], cd[nm][:, :], [], [nm])
        for nm, t in (("dintraT", dintraT), ("xiT", xiT), ("zeta", zeta)):
            dma("sp", t[:, :, :], cd[nm][:, :, :], [], [nm])
        for l in range(L):
            dma("sp", gattn[:, l, :], gattn_d[l, :, :], [], ["gattn"])
            dma("sp", gkv[:, l, :], gkv_d[l, :, :], [], ["gkv"])
            dma("sp", gdm[:, l, :], gdiff_d[l, :].partition_broadcast(128), [], ["gdm"])
            dma("sp", gret[:, l, :], gret_d[l, :].partition_broadcast(128), [], ["gret"])
            dma("sp", dlam[:, l, :], dlam_d[l, :].partition_broadcast(128), [], ["dlam"])
        for l in range(L):
            lam_init = 0.8 - 0.6 * math.exp(-0.3 * l)
            junk = dlam[:, l, 0:32]
            tt("dve", dlam[:, l, 0:32], dlam[:, l, 0:32], dlam[:, l, 32:64], ALU.mult, ["dlam"], ["dlam"])
            tt("dve", dlam[:, l, 64:96], dlam[:, l, 64:96], dlam[:, l, 96:128], ALU.mult, ["dlam"], ["dlam"])
            reduce(lamt[:, 0:1], dlam[:, l, 0:32], ALU.add, ["dlam"], ["lamt"])
            reduce(lamt[:, 1:2], dlam[:, l, 64:96], ALU.add, ["dlam", "lamt"], ["lamt"])
            act(lamt[:, 2:4], lamt[:, 0:2], AF.Exp, ["lamt"], ["lamt"])
            tt("dve", lamt[:, 0:1], lamt[:, 3:4], lamt[:, 2:3], ALU.subtract, ["lamt"], ["lamt"])
            ts("dve", lamneg[:, l, :], lamt[:, 0:1], -lam_init, None, ALU.add, None, ["lamt"], ["lamneg"])
            ts("dve", gdm[:, l, :], gdm[:, l, :], 1.0 - lam_init, None, ALU.mult, None, ["gdm"], ["gdm"])

        P.barrier()
        tmp0.close()
        wrotF = Rot(range(3))
        wrotT = Rot(range(2))
        evrot = Rot(["act", "dve"])

        def h_src(l, s, t):
            src = x_d if l == 0 else hbuf_d
            return src[s, t * 128:(t + 1) * 128, :]

        def hkey(s, t):
            return ("h", s, t)

        for l in range(L):
            last_layer = (l == L - 1)
            for s in range(NS):
                with ExitStack() as ph:
                    hb = [sbuf(ph, "p0_h%d" % i, [128, D]) for i in range(2)]
                    hn = [sbuf(ph, "p0_hn%d" % i, [128, D], BF16) for i in range(2)]
                    sq_junk = sbuf(ph, "p0_junk", [128, D], BF16)
                    st0 = [sbuf(ph, "p0_st%d" % i, [128, 2]) for i in range(2)]
                    brot = Rot(range(8))
                    gat3 = sbuf(ph, "p0_g3", [128, 8, 1])
                    cp("dve", gat3[:, :, :].rearrange("p k o -> p (k o)"), gattn[:, l, :], ["gattn"], ["gat3"])

                    def p0_stage1(t):
                        i = t % 2
                        dma("sp", hb[i][:, :], h_src(l, s, t), [hkey(s, t)], [("hb", i)])
                        act(sq_junk[:, :], hb[i][:, :], AF.Square, [("hb", i)], ["sqj", ("st0", i)], accum_out=st0[i][:, 0:1])
                        rsqrt_mean("dve", st0[i][:, 1:2], st0[i][:, 0:1], D, ("st0", i), ("st0", i))
                        act(hn[i][:, :], hb[i][:, :], AF.Copy, [("hb", i), ("st0", i)], [("hn", i)], scale=st0[i][:, 1:2])

                    def p0_stage2(t):
                        i = t % 2
                        b = brot.next()
                        pv = ps[b][:, :].bitcast(BF16)
                        for k in range(8):
                            tr(pv[:, k * 128:(k + 1) * 128], hn[i][:, k * 128:(k + 1) * 128], [("hn", i)], [pk(b)])
                        tt("dve", uT[:, :, t * 128:(t + 1) * 128], pv[:, :].rearrange("p (k q) -> p k q", q=128),
                           gat3[:, :, :].to_broadcast([128, 8, 128]), ALU.mult, [pk(b), "gat3"], ["uT"])
                    for t in range(NB + 1):
                        if t < NB:
                            p0_stage1(t)
                        if t >= 1:
                            p0_stage2(t - 1)
                P.barrier()

                def load_wF(wtiles, f):
                    i = wrotF.next()
                    dma("pool", wtiles[i][:, :, :], wF_d[l, f, :, :, :], [], [("wF", i)])
                    return i

                def proj_F(wtiles, f, evac):
                    i = load_wF(wtiles, f)
                    for tg in range(4):
                        b = brotP.next()
                        for k in range(8):
                            mm(ps[b][:, :], wtiles[i][:, k, :], uT[:, k, tg * 512:(tg + 1) * 512], k == 0, k == 7,
                               [("wF", i), "uT"], [pk(b)])
                        evac(tg, ps[b][:, :], b)

                def proj_T(wt, wkey, ncols, t, c0=0):
                    b = brotP.next()
                    for k in range(8):
                        mm(ps[b][:, 0:ncols], uT[:, k, t * 128:(t + 1) * 128], wt[:, k, c0:c0 + ncols], k == 0, k == 7,
                           [wkey, "uT"], [pk(b)])
                    return b

                def evac_copy(dst3, c):
                    def f(tg, pa, b):
                        ev = evrot.next()
                        cp(ev, dst3[:, c, tg * 512:(tg + 1) * 512], pa, [pk(b)], [dst3.name if hasattr(dst3, "name") else "x"])
                    return f

                with ExitStack() as ph:
                  if "A" in _SKIP:
                    pass
                  else:
                      brotP = Rot(range(8))
                      wFt = [sbuf(ph, "a_wF%d" % i, [128, 8, 128], BF16) for i in range(3)]
                      wTt = sbuf(ph, "a_wT", [128, 8, 384], BF16)
                      dqT = sbuf(ph, "a_dqT", [128, 3, S], BF16)
                      dkT = sbuf(ph, "a_dkT", [128, 3, S], BF16)
                      dva = sbuf(ph, "a_dva", [128, NB, 6, 65], BF16)
                      pt = [sbuf(ph, "a_pt%d" % i, [128, 512], BF16) for i in range(8)]
                      rr = [sbuf(ph, "a_rr%d" % i, [128, 4, 1]) for i in range(3)]
                      tA = sbuf(ph, "a_tA", [128, 4, 64])
                      tB = sbuf(ph, "a_tB", [128, 4, 64])
                      tO = sbuf(ph, "a_tO", [128, 4, 64])
                      tS = sbuf(ph, "a_tS", [128, 4, 64])
                      ss4 = sbuf(ph, "a_ss4", [128, 4, 1])
                      oa = [sbuf(ph, "a_oa%d" % i, [128, 4, 384], BF16) for i in range(2)]

                      dma("pool", wTt[:, :, :], wT_d[l, :, :, T_DV[0]:T_DV[1]], [], ["a_wT"])
                      memset("pool", dva[:, :, :, 64:65], 1.0, ["dva"])
                      for c in range(3):
                          def ev_q(tg, pa, b, c=c):
                              cp(evrot.next(), dqT[:, c, tg * 512:(tg + 1) * 512], pa, [pk(b)], ["dqT"])
                          proj_F(wFt, c, ev_q)
                      for c in range(3):
                          def ev_k(tg, pa, b, c=c):
                              cp(evrot.next(), dkT[:, c, tg * 512:(tg + 1) * 512], pa, [pk(b)], ["dkT"])
                          proj_F(wFt, 3 + c, ev_k)
                      for t in range(NB):
                          b = proj_T(wTt, "a_wT", 384, t)
                          cp(evrot.next(), dva[:, t, :, 0:64], ps[b][:, 0:384].rearrange("p (h e) -> p h e", e=64), [pk(b)], ["dva"])

                      scrot = Rot([0, 1, 2, 3])
                      accrot = Rot([(4, 5), (6, 7)])
                      ptrot = Rot(range(8))
                      scale = 32.0 ** -0.5

                      def a_epilogue(I, h, banks):
                          oab = oa[I % 2]
                          oak = ("oa", I % 2)
                          a0 = ps[banks[0]][:, 0:260].rearrange("p (i e) -> p i e", e=65)
                          a1 = ps[banks[1]][:, 0:260].rearrange("p (i e) -> p i e", e=65)
                          recip(rr[0][:, :, :], a0[:, :, 64:65], [pk(banks[0])], ["rr0"])
                          recip(rr[1][:, :, :], a1[:, :, 64:65], [pk(banks[1])], ["rr1"])
                          ts("dve", rr[2][:, :, :], rr[1][:, :, :], lamneg[:, l, :], None, ALU.mult, None, ["rr1", "lamneg"], ["rr2"])
                          tt("dve", tA[:, :, :], a0[:, :, 0:64], rr[0][:, :, :].to_broadcast([128, 4, 64]), ALU.mult, [pk(banks[0]), "rr0"], ["tA"])
                          tt("dve", tB[:, :, :], a1[:, :, 0:64], rr[2][:, :, :].to_broadcast([128, 4, 64]), ALU.mult, [pk(banks[1]), "rr2"], ["tB"])
                          tt("pool", tO[:, :, :], tA[:, :, :], tB[:, :, :], ALU.add, ["tA", "tB"], ["tO"])
                          tt("pool", tS[:, :, :], tO[:, :, :], tO[:, :, :], ALU.mult, ["tO"], ["tS"])
                          reduce(ss4[:, :, :], tS[:, :, :], ALU.add, ["tS"], ["ss4"])
                          rsqrt_mean("dve", ss4[:, :, :], ss4[:, :, :], 64, "ss4", "ss4")
                          tt("pool", tO[:, :, :], tO[:, :, :], ss4[:, :, :].to_broadcast([128, 4, 64]), ALU.mult, ["tO", "ss4"], ["tO"])
                          tt("pool", oab[:, :, h * 64:(h + 1) * 64], tO[:, :, :], gdm[:, l:l + 1, :].to_broadcast([128, 4, 64]), ALU.mult, ["tO", "gdm"], [oak])
                          if h == 5:
                              for i4 in range(4):
                                  t = 4 * I + i4
                                  dma("sp", mixed_d[s, t * 128:(t + 1) * 128, 0:384], oab[:, i4, :], [oak], [("mx", s, t, 0)])

                      tiles = []
                      for I in range(4):
                          for h in range(6):
                              banks = accrot.next()
                              nj = 4 * I + 4
                              for j in range(nj):
                                  for c2 in range(2):
                                      tiles.append(dict(I=I, h=h, c2=c2, j=j, ab=banks[c2], banks=banks, first=(j == 0),
                                                        last=(c2 == 1 and j == nj - 1)))

                      def a_score(T):
                          I, h, c2, j = T["I"], T["h"], T["c2"], T["j"]
                          r0 = (h % 2) * 64 + c2 * 32
                          kw = dict(tile_position=(96, 0)) if r0 == 96 else {}
                          i0 = max(j, 4 * I)
                          w = (4 * I + 4 - i0) * 128
                          sb_ = scrot.next()
                          pi = ptrot.next()
                          T.update(i0=i0, w=w, pi=pi)
                          mm(ps[sb_][:, 0:w], dkT[r0:r0 + 32, h // 2, j * 128:(j + 1) * 128],
                             dqT[r0:r0 + 32, h // 2, i0 * 128:(4 * I + 4) * 128], True, True,
                             ["dkT", "dqT"], [pk(sb_)], **kw)
                          act(pt[pi][:, 0:w], ps[sb_][:, 0:w], AF.Exp, [pk(sb_)], [("pt", pi)], scale=scale)
                          if j >= 4 * I:
                              tt("pool", pt[pi][:, 0:128], pt[pi][:, 0:128], tri_b[:, :], ALU.mult, [("pt", pi), "tri_b"], [("pt", pi)])

                      def a_av(T):
                          I, h, j, i0, pi, ab = T["I"], T["h"], T["j"], T["i0"], T["pi"], T["ab"]
                          for i in range(i0, 4 * I + 4):
                              mm(ps[ab][:, (i - 4 * I) * 65:(i - 4 * I) * 65 + 65], pt[pi][:, (i - i0) * 128:(i - i0 + 1) * 128],
                                 dva[:, j, h, :], T["first"] and i == i0, j == i, [("pt", pi), "dva"], [pk(ab)])
                          if T["last"]:
                              a_epilogue(I, h, T["banks"])

                      DEP = 4
                      for idx in range(len(tiles) + DEP):
                          if idx < len(tiles):
                              a_score(tiles[idx])
                          if idx >= DEP:
                              a_av(tiles[idx - DEP])
                P.barrier()

                with ExitStack() as ph:
                  if "B" in _SKIP:
                    pass
                  else:
                      brotP = Rot(range(8))
                      qlT = sbuf(ph, "b_qlT", [128, 6, S], BF16)
                      ckvT = sbuf(ph, "b_ckvT", [128, S], BF16)
                      vpa = sbuf(ph, "b_vpa", [128, NB, 6, 65], BF16)
                      iqT = sbuf(ph, "b_iqT", [128, 4, S], BF16)
                      ikT = sbuf(ph, "b_ikT", [128, S], BF16)
                      iw3 = sbuf(ph, "b_iw", [128, NB, 8, 1])
                      iw = iw3[:, :, :, :].rearrange("p t h o -> p t (h o)")
                      ident3 = sbuf(ph, "b_ident3", [128, 1, 128], BF16)
                      cp("pool", ident3[:, 0, :], ident_b[:, :], ["ident_b"], ["ident3"])
                      ph2 = ExitStack()
                      wFt = [sbuf(ph2, "b_wF%d" % i, [128, 8, 128], BF16) for i in range(3)]
                      wTt = sbuf(ph2, "b_wT", [128, 8, 256], BF16)
                      sqt = [sbuf(ph2, "b_sqt%d" % i, [128, 512], BF16) for i in range(2)]
                      ckn = [sbuf(ph2, "b_ckn%d" % i, [128, 128], BF16) for i in range(2)]
                      cst = [sbuf(ph2, "b_cst%d" % i, [128, 2]) for i in range(2)]
                      cjunk = sbuf(ph2, "b_cjunk", [128, 128], BF16)
                      wuk_b = sbuf(ph2, "b_wuk", [128, 1, 3, 128], BF16)
                      wuv_b = sbuf(ph2, "b_wuv", [128, 1, 384], BF16)

                      dma("pool", wTt[:, :, :], wT_d[l, :, :, T_CKV[0]:T_CKV[0] + 256], [], ["b_wT"])
                      dma("pool", wuk_b[:, 0, :, :], wuk_d[l, :, :, :], [], ["wuk_b"])
                      dma("pool", wuv_b[:, 0, :], wuv_d[l, :, :], [], ["wuv_b"])
                      memset("pool", vpa[:, :, :, 64:65], 1.0, ["vpa"])
                      sqrot = Rot(range(2))
                      for c in range(3):
                          def ev_sq(tg, pa, b, c=c):
                              si = sqrot.next()
                              cp(evrot.next(), sqt[si][:, :], pa, [pk(b)], [("sqt", si)])
                              for hl in range(2):
                                  b2 = brotP.next()
                                  mm(ps[b2][:, :], wuk_b[hl * 64:(hl + 1) * 64, 0, c, :], sqt[si][hl * 64:(hl + 1) * 64, :], True, True,
                                     ["wuk_b", ("sqt", si)], [pk(b2)])
                                  cp(evrot.next(), qlT[:, 2 * c + hl, tg * 512:(tg + 1) * 512], ps[b2][:, :], [pk(b2)], ["qlT"])
                          if '1' not in _SKIP:
                              proj_F(wFt, 6 + c, ev_sq)
                      for c in range(4):
                          def ev_iq(tg, pa, b, c=c):
                              cp(evrot.next(), iqT[:, c, tg * 512:(tg + 1) * 512], pa, [pk(b)], ["iqT"])
                          if '2' not in _SKIP:
                              proj_F(wFt, 9 + c, ev_iq)

                      def ev_ik(tg, pa, b):
                          cp(evrot.next(), ikT[:, tg * 512:(tg + 1) * 512], pa, [pk(b)], ["ikT"])
                      if '3' not in _SKIP:
                          proj_F(wFt, 13, ev_ik)
                      for t in range(NB if '4' not in _SKIP else 0):
                          i = t % 2
                          b = proj_T(wTt, "b_wT", 136, t)
                          cp("dve", iw[:, t, :], ps[b][:, 128:136], [pk(b)], ["iw"])
                          act(cjunk[:, :], ps[b][:, 0:128], AF.Square, [pk(b)], ["cjunk", ("cst", i)], accum_out=cst[i][:, 0:1])
                          rsqrt_mean("dve", cst[i][:, 1:2], cst[i][:, 0:1], 128, ("cst", i), ("cst", i))
                          ts("dve", ckn[i][:, :], ps[b][:, 0:128], cst[i][:, 1:2], None, ALU.mult, None, [pk(b), ("cst", i)], [("ckn", i)])
                          b2 = brotP.next()
                          pv = ps[b2][:, :].bitcast(BF16)
                          tr(pv[:, 0:128], ckn[i][:, :], [("ckn", i)], [pk(b2)])
                          act(ckvT[:, t * 128:(t + 1) * 128], pv[:, 0:128], AF.Copy, [pk(b2), "gkv"], ["ckvT"], scale=gkv[:, l, :])
                          b3 = brotP.next()
                          mm(ps[b3][:, 0:384], ckvT[:, t * 128:(t + 1) * 128], wuv_b[:, 0, :], True, True, ["ckvT", "wuv_b"], [pk(b3)])
                          cp(evrot.next(), vpa[:, t, :, 0:64], ps[b3][:, 0:384].rearrange("p (h e) -> p h e", e=64), [pk(b3)], ["vpa"])

                      P.barrier()
                      ph2.close()
                      SCW = [(8 + i + 1) * 128 for i in range(4)] + [(12 + i + 1) * 128 for i in range(4)]
                      score = [sbuf(ph, "b_score%d" % i, [128, SCW[i]]) for i in range(8)]
                      negm = [sbuf(ph, "b_negm%d" % i, [128, SCW[i]], BF16) for i in range(8)]
                      rel = [sbuf(ph, "b_rel%d" % i, [128, 512], BF16) for i in range(6)]
                      dg = [sbuf(ph, "b_dg%d" % i, [128, 8, 128], BF16) for i in range(4)]
                      bis = [sbuf(ph, "b_bis%d" % i, [128, 8]) for i in range(8)]
                      bjunk = sbuf(ph, "b_bjunk", [128, S], BF16)
                      pt = [sbuf(ph, "b_pt%d" % i, [128, 512], BF16) for i in range(4)]
                      acs = [sbuf(ph, "b_acs%d" % i, [128, 4, 65]) for i in range(2)]
                      acsrot = Rot(range(2))
                      rrp = [sbuf(ph, "b_rrp%d" % i, [128, 4, 1]) for i in range(2)]
                      negone = sbuf(ph, "b_negone", [128, 4, 1])
                      memset("pool", negone[:, :, :], -1.0, ["negone"])
                      ob = [sbuf(ph, "b_ob%d" % i, [128, 4, 384], BF16) for i in range(2)]
                      relbank = Rot([0, 1, 2, 3])
                      idxacc = Rot([4])
                      relrot = Rot(range(6))
                      scrot = Rot([5, 6])
                      accrot = Rot([7])
                      ptrot = Rot(range(4))
                      scale = 64.0 ** -0.5

                      def index_phase(I):
                          par = I % 2
                          chains = []
                          for i4 in range(4):
                              i = 4 * I + i4
                              if i >= 2:
                                  tt("pool", dg[i4][:, :, :], ident3[:, :, :].to_broadcast([128, 8, 128]), iw3[:, i, :, :].to_broadcast([128, 8, 128]),
                                     ALU.mult, ["ident3", "iw"], [("dg", i4)])
                          for i4 in range(4):
                              i = 4 * I + i4
                              n = (i + 1) * 128
                              si_ = par * 4 + i4
                              sk = ("score", si_)
                              bk = ("bis", si_)
                              bs = bis[si_]
                              if i < 2 or 'i' in _SKIP:
                                  memset("dve", score[si_][:, 0:n], 0.0, [sk])
                                  tt("dve", score[si_][:, n - 128:n], score[si_][:, n - 128:n], cneg[:, :], ALU.add, [sk, "cneg"], [sk])
                                  memset("dve", bs[:, 0:1], -1e29, [bk])
                              else:
                                  dgi = i4
                                  for kc in range((n + 511) // 512):
                                      w = min(512, n - kc * 512)
                                      ab = idxacc.next()
                                      pend = []
                                      for hp in range(5):
                                          cur = []
                                          if hp < 4:
                                              for hl in range(2):
                                                  hh = 2 * hp + hl
                                                  rb = relbank.next()
                                                  r0 = hl * 64
                                                  mm(ps[rb][:, 0:w], iqT[r0:r0 + 64, hp, i * 128:(i + 1) * 128], ikT[r0:r0 + 64, kc * 512:kc * 512 + w],
                                                     True, True, ["iqT", "ikT"], [pk(rb)])
                                                  ri = relrot.next()
                                                  act(rel[ri][:, 0:w], ps[rb][:, 0:w], AF.Relu, [pk(rb)], [("rel", ri)])
                                                  cur.append((hh, ri))
                                          for (ph_, pri) in pend:
                                              mm(ps[ab][:, 0:w], dg[dgi][:, ph_, :], rel[pri][:, 0:w], ph_ == 0, ph_ == 7, [("dg", dgi), ("rel", pri)], [pk(ab)])
                                          pend = cur
                                      cp("act", score[si_][:, kc * 512:kc * 512 + w], ps[ab][:, 0:w], [pk(ab)], [sk])
                                  tt("dve", score[si_][:, n - 128:n], score[si_][:, n - 128:n], cneg[:, :], ALU.add, [sk, "cneg"], [sk])
                                  reduce(bs[:, 5:6], score[si_][:, 0:n], ALU.max, [sk], [bk])
                                  reduce(bs[:, 0:1], score[si_][:, 0:n - 128], ALU.min, [sk, bk], [bk])
                                  tt("dve", bs[:, 1:2], bs[:, 5:6], bs[:, 0:1], ALU.subtract, [bk], [bk])
                                  chains.append((si_, n, sk, bk, bs))
                          for it in range(NBIS):
                              for (si_, n, sk, bk, bs) in chains:
                                  ts("dve", bs[:, 2:3], bs[:, 1:2], 0.5 ** (it + 1), bs[:, 0:1], ALU.mult, ALU.add, [bk], [bk])
                              for (si_, n, sk, bk, bs) in chains:
                                  ts("dve", bjunk[:, 0:n], score[si_][:, 0:n], bs[:, 2:3], 0.0, ALU.is_ge, ALU.add, [sk, bk], ["bjunk", bk], accum_out=bs[:, 3:4])
                              for (si_, n, sk, bk, bs) in chains:
                                  ts("dve", bs[:, 4:5], bs[:, 3:4], 255.5, 1e30, ALU.is_lt, ALU.mult, [bk], [bk])
                              for (si_, n, sk, bk, bs) in chains:
                                  stt("dve", bs[:, 0:1], bs[:, 2:3], bs[:, 4:5], bs[:, 0:1], ALU.subtract, ALU.max, [bk], [bk])
                          for i4 in range(4):
                              n = (4 * I + i4 + 1) * 128
                              si_ = par * 4 + i4
                              ts("dve", negm[si_][:, 0:n], score[si_][:, 0:n], bis[si_][:, 0:1], NEGM, ALU.is_lt, ALU.mult,
                                 [("score", si_), ("bis", si_)], [("negm", si_)])

                      def d_epilogue(I, h, ab):
                          obb = ob[I % 2]
                          obk = ("ob", I % 2)
                          a0 = ps[ab][:, 0:260].rearrange("p (i e) -> p i e", e=65)
                          ai = acsrot.next()
                          cp("act", acs[ai][:, :, :], a0, [pk(ab)], [("acs", ai)])
                          tt("pool", rrp[ai][:, :, :], acs[ai][:, :, 64:65], negone[:, :, :], ALU.pow, [("acs", ai), "negone"], [("rrp", ai)])
                          tt("pool", obb[:, :, h * 64:(h + 1) * 64], acs[ai][:, :, 0:64], rrp[ai][:, :, :].to_broadcast([128, 4, 64]), ALU.mult,
                             [("acs", ai), ("rrp", ai)], [obk])
                          if h == 5:
                              for i4 in range(4):
                                  t = 4 * I + i4
                                  dma("sp", mixed_d[s, t * 128:(t + 1) * 128, 384:768], obb[:, i4, :], [obk], [("mx", s, t, 1)])

                      def d_score(T):
                          I, h, j = T["I"], T["h"], T["j"]
                          par = I % 2
                          i0 = max(j, 4 * I)
                          w = (4 * I + 4 - i0) * 128
                          sb_ = scrot.next()
                          pi = ptrot.next()
                          T.update(i0=i0, w=w, pi=pi)
                          mm(ps[sb_][:, 0:w], ckvT[:, j * 128:(j + 1) * 128], qlT[:, h, i0 * 128:(4 * I + 4) * 128], True, False,
                             ["ckvT", "qlT"], [pk(sb_)])
                          for i in range(i0, 4 * I + 4):
                              i4 = i - 4 * I
                              mm(ps[sb_][:, (i - i0) * 128:(i - i0 + 1) * 128], negm[par * 4 + i4][:, j * 128:(j + 1) * 128], ident_b[:, :], False,
                                 i == 4 * I + 3, [("negm", par * 4 + i4), "ident_b"], [pk(sb_)])
                          act(pt[pi][:, 0:w], ps[sb_][:, 0:w], AF.Exp, [pk(sb_)], [("pt", pi)], scale=scale)

                      def d_av(T):
                          I, h, j, i0, pi, ab = T["I"], T["h"], T["j"], T["i0"], T["pi"], T["ab"]
                          for i in range(i0, 4 * I + 4):
                              mm(ps[ab][:, (i - 4 * I) * 65:(i - 4 * I) * 65 + 65], pt[pi][:, (i - i0) * 128:(i - i0 + 1) * 128],
                                 vpa[:, j, h, :], T["first"] and i == i0, j == i, [("pt", pi), "vpa"], [pk(ab)])
                          if T["last"]:
                              d_epilogue(I, h, ab)

                      def dsa_phase(I):
                          tiles = []
                          for h in range(6):
                              ab = accrot.next()
                              nj = 4 * I + 4
                              for j in range(nj):
                                  tiles.append(dict(I=I, h=h, j=j, ab=ab, first=(j == 0), last=(j == nj - 1)))
                          DEP = 1
                          for idx in range(len(tiles) + DEP):
                              if idx < len(tiles):
                                  d_score(tiles[idx])
                              if idx >= DEP:
                                  d_av(tiles[idx - DEP])

                      index_phase(3)
                      for I in (3, 2, 1, 0):
                          if I - 1 >= 0:
                              index_phase(I - 1)
                          dsa_phase(I)
                P.barrier()

                with ExitStack() as ph:
                  if "C" in _SKIP:
                    pass
                  else:
                      brotP = Rot(range(8))
                      wFt = [sbuf(ph, "c_wF%d" % i, [128, 8, 128], BF16) for i in range(3)]
                      wTt = sbuf(ph, "c_wT", [128, 8, 256], BF16)
                      rT = [sbuf(ph, "c_rqT", [128, 2, S], BF16), sbuf(ph, "c_rkT", [128, 2, S], BF16)]
                      rv = sbuf(ph, "c_rv", [128, NB, 256], BF16)
                      t1 = sbuf(ph, "c_t1", [128, 2, S])
                      t2 = [sbuf(ph, "c_t2_%d" % i, [128, 512]) for i in range(2)]
                      kz = [sbuf(ph, "c_kz%d" % i, [128, 128], BF16) for i in range(4)]
                      qxi = [sbuf(ph, "c_qxi%d" % i, [128, 128], BF16) for i in range(4)]
                      attD = [sbuf(ph, "c_attD%d" % i, [128, 128], BF16) for i in range(4)]
                      Rf = sbuf(ph, "c_Rf", [128, 2, 64])
                      Rb = sbuf(ph, "c_Rb", [128, 2, 64], BF16)
                      oc = [sbuf(ph, "c_oc%d" % i, [128, 4, 64]) for i in range(2)]
                      ocs = sbuf(ph, "c_ocs", [128, 4, 64])
                      ocb = [sbuf(ph, "c_ocb%d" % i, [128, 4, 64], BF16) for i in range(2)]
                      ss4 = sbuf(ph, "c_ss4", [128, 4, 1])

                      dma("pool", wTt[:, :, :], wT_d[l, :, :, T_RV[0]:T_RV[1]], [], ["c_wT"])
                      cosT = sbuf(ph, "c_cosT", [128, S])
                      sinT = sbuf(ph, "c_sinT", [128, S])
                      dma("sp", cosT[:, :], cd["cosT"][:, :], [], ["cosT"])
                      dma("sp", sinT[:, :], cd["sinT"][:, :], [], ["sinT"])
                      for qk in range(2):
                          for c in range(2):
                              def ev_x(tg, pa, b, c=c, qk=qk):
                                  i = tg % 2
                                  tt("dve", t1[:, c, tg * 512:(tg + 1) * 512], pa, cosT[:, tg * 512:(tg + 1) * 512], ALU.mult, [pk(b), "cosT"], [("t1", tg, c)])
                              proj_F(wFt, 14 + 4 * qk + c, ev_x)
                          for c in range(2):
                              def ev_xs(tg, pa, b, c=c, qk=qk):
                                  i = tg % 2
                                  tt("dve", t2[i][:, :], pa, sinT[:, tg * 512:(tg + 1) * 512], ALU.mult, [pk(b), "sinT"], [("t2", i)])
                                  tt("pool", rT[qk][:, c, tg * 512:(tg + 1) * 512], t1[:, c, tg * 512:(tg + 1) * 512], t2[i][:, :], ALU.add, [("t1", tg, c), ("t2", i)], [("rT", qk)])
                              proj_F(wFt, 16 + 4 * qk + c, ev_xs)
                      for t in range(NB):
                          b = proj_T(wTt, "c_wT", 256, t)
                          cp(evrot.next(), rv[:, t, :], ps[b][:, 0:256], [pk(b)], ["rv"])

                      memset("dve", Rf[:, :, :], 0.0, ["Rf"])
                      memset("dve", Rb[:, :, :], 0.0, ["Rb"])
                      brot = Rot([0, 1])
                      adrot = Rot(range(4))
                      obanks = [(4, 5), (2, 3)]
                      rbk = [6, 7]

                      def c_stage1(t):
                          tsl = slice(t * 128, (t + 1) * 128)
                          p2 = t % 2
                          obp = obanks[p2]
                          firsts = [True, True]
                          for c in range(2):
                              b = brot.next()
                              pv = ps[b][:, :].bitcast(BF16)
                              tr(pv[:, 0:128], rT[1][:, c, tsl], [("rT", 1)], [pk(b)])
                              tt("dve", kz[p2 * 2 + c][:, :], pv[:, 0:128], zeta[:, c, :], ALU.mult, [pk(b), "zeta"], [("kz", p2, c)])
                              tt("pool", qxi[p2 * 2 + c][:, :], rT[0][:, c, tsl], xiT[:, c, :], ALU.mult, [("rT", 0), "xiT"], [("qxi", p2, c)])
                              for hl in range(2):
                                  h = 2 * c + hl
                                  rows = slice(hl * 64, (hl + 1) * 64)
                                  b = brot.next()
                                  mm(ps[b][:, 0:128], rT[1][rows, c, tsl], rT[0][rows, c, tsl], True, True, [("rT", 1), ("rT", 0)], [pk(b)])
                                  ai = adrot.next()
                                  tt("dve", attD[ai][:, :], ps[b][:, 0:128], dintraT[:, h, :], ALU.mult, [pk(b), "dintraT"], [("attD", ai)])
                                  mm(ps[obp[hl]][:, c * 64:(c + 1) * 64], attD[ai][:, :], rv[:, t, h * 64:(h + 1) * 64], firsts[hl], t == 0,
                                     [("attD", ai), "rv"], [pk(obp[hl])])
                                  firsts[hl] = False
                              if t < NB - 1:
                                  rb_ = rbk[p2]
                                  mm(ps[rb_][:, c * 128:(c + 1) * 128], kz[p2 * 2 + c][:, :], rv[:, t, c * 128:(c + 1) * 128], True, True, [("kz", p2, c), "rv"], [pk(rb_)])

                      def c_stage2(t):
                          tsl = slice(t * 128, (t + 1) * 128)
                          p2 = t % 2
                          obp = obanks[p2]
                          if t > 0:
                              for hl in range(2):
                                  for c in range(2):
                                      rows = slice(hl * 64, (hl + 1) * 64)
                                      mm(ps[obp[hl]][:, c * 64:(c + 1) * 64], qxi[p2 * 2 + c][rows, :], Rb[rows, c, :], False, True,
                                         [("qxi", p2, c), "Rb"], [pk(obp[hl])])
                          oi = p2
                          ocv = oc[oi][:, :, :].rearrange("p (c hl) e -> p c hl e", hl=2)
                          for hl in range(2):
                              cp("act", ocv[:, :, hl, :], ps[obp[hl]][:, 0:128].rearrange("p (c e) -> p c e", e=64), [pk(obp[hl])], [("oc", oi)])
                          if t < NB - 1:
                              for c in range(2):
                                  for hl in range(2):
                                      rows = slice(hl * 64, (hl + 1) * 64)
                                      stt("dve", Rf[rows, c, :], Rf[rows, c, :], gch[rows, c:c + 1], ps[rbk[p2]][rows, c * 128 + hl * 64:c * 128 + (hl + 1) * 64],
                                          ALU.mult, ALU.add, ["Rf", "gch", pk(rbk[p2])], ["Rf"])
                              cp("act", Rb[:, :, :], Rf[:, :, :], ["Rf"], ["Rb"])
                          tt("pool", ocs[:, :, :], oc[oi][:, :, :], oc[oi][:, :, :], ALU.mult, [("oc", oi)], ["ocs"])
                          reduce(ss4[:, :, :], ocs[:, :, :], ALU.add, ["ocs"], ["c_ss4"])
                          rsqrt_mean("dve", ss4[:, :, :], ss4[:, :, :], 64, "c_ss4", "c_ss4")
                          tt("pool", oc[oi][:, :, :], oc[oi][:, :, :], ss4[:, :, :].to_broadcast([128, 4, 64]), ALU.mult, [("oc", oi), "c_ss4"], [("oc", oi)])
                          tt("pool", ocb[oi][:, :, :], oc[oi][:, :, :], gret[:, l:l + 1, :].to_broadcast([128, 4, 64]), ALU.mult, [("oc", oi), "gret"], [("ocb", oi)])
                          dma("sp", mixed_d[s, tsl, 768:1024], ocb[oi][:, :, :].rearrange("p h e -> p (h e)"), [("ocb", oi)], [("mx", s, t, 2)])

                      c_stage1(0)
                      for t in range(NB):
                          if t + 1 < NB:
                              c_stage1(t + 1)
                          c_stage2(t)
                P.barrier()

                with ExitStack() as ph:
                  if "D" in _SKIP:
                    pass
                  else:
                      wg = sbuf(ph, "d_wg", [128, 8, D], BF16)
                      wo = sbuf(ph, "d_wo", [128, 8, D], BF16)
                      hb = [sbuf(ph, "d_h%d" % i, [128, D]) for i in range(2)]
                      mx = [sbuf(ph, "d_mx%d" % i, [128, D], BF16) for i in range(2)]
                      sg = [sbuf(ph, "d_sg%d" % i, [128, D], BF16) for i in range(2)]
                      yb = [sbuf(ph, "d_y%d" % i, [128, D], BF16) for i in range(2)]
                      yT = [sbuf(ph, "d_yT%d" % i, [128, 8, 128], BF16) for i in range(2)]
                      hn = [sbuf(ph, "d_hn%d" % i, [128, D]) for i in range(2)]
                      fo = [sbuf(ph, "d_fo%d" % i, [128, D]) for i in range(2)]
                      fjunk = sbuf(ph, "d_fjunk", [128, D], BF16)
                      if last_layer:
                          gfin = sbuf(ph, "d_gfin", [128, D])
                          dma("sp", gfin[:, :], gfin_d.partition_broadcast(128), [], ["gfin"])
                      fst = [sbuf(ph, "d_fst%d" % i, [128, 2]) for i in range(2)]
                      for kh in range(2):
                          dma("pool", wg[:, :, kh * 512:(kh + 1) * 512], wT_d[l, :, :, T_GATE[0] + kh * 512:T_GATE[0] + (kh + 1) * 512], [], ["d_wg"])
                          dma("pool", wo[:, :, kh * 512:(kh + 1) * 512], wout_d[l, :, :, kh * 512:(kh + 1) * 512], [], ["d_wo"])
                      gb = Rot([0, 1, 2, 3])
                      tb = Rot([4, 5])
                      ob2 = Rot([6, 7])
                      hb3 = hb + [sbuf(ph, "d_h2", [128, D])]

                      def d_gate(t):
                          i = t % 2
                          tsl = slice(t * 128, (t + 1) * 128)
                          dma("sp", hb3[t % 3][:, :], h_src(l, s, t), [hkey(s, t)], [("dhb", t % 3)])
                          dma("sp", mx[i][:, :], mixed_d[s, tsl, :], [("mx", s, t, 0), ("mx", s, t, 1), ("mx", s, t, 2)], [("dmx", i)])
                          for kh in range(2):
                              b = gb.next()
                              for k in range(8):
                                  mm(ps[b][:, :], uT[:, k, tsl], wg[:, k, kh * 512:(kh + 1) * 512], k == 0, k == 7, ["uT", "d_wg"], [pk(b)])
                              act(sg[i][:, kh * 512:(kh + 1) * 512], ps[b][:, :], AF.Silu, [pk(b)], [("sg", i)])
                          tt("dve", yb[i][:, :], sg[i][:, :], mx[i][:, :], ALU.mult, [("sg", i), ("dmx", i)], [("yb", i)])

                      def d_tr(t):
                          i = t % 2
                          b = tb.next()
                          pv = ps[b][:, :].bitcast(BF16)
                          for k in range(8):
                              tr(pv[:, k * 128:(k + 1) * 128], yb[i][:, k * 128:(k + 1) * 128], [("yb", i)], [pk(b)])
                          cp("act", yT[i][:, :, :], pv[:, :].rearrange("p (k q) -> p k q", q=128), [pk(b)], [("yT", i)])

                      def d_out(t):
                          i = t % 2
                          tsl = slice(t * 128, (t + 1) * 128)
                          for kh in range(2):
                              b = ob2.next()
                              for k in range(8):
                                  mm(ps[b][:, :], yT[i][:, k, :], wo[:, k, kh * 512:(kh + 1) * 512], k == 0, k == 7, [("yT", i), "d_wo"], [pk(b)])
                              tt("dve", hn[i][:, kh * 512:(kh + 1) * 512], ps[b][:, :], hb3[t % 3][:, kh * 512:(kh + 1) * 512], ALU.add, [pk(b), ("dhb", t % 3)], [("dhn", i)])
                          if not last_layer:
                              dma("sp", hbuf_d[s, tsl, :], hn[i][:, :], [("dhn", i)], [hkey(s, t)])
                          else:
                              act(fjunk[:, :], hn[i][:, :], AF.Square, [("dhn", i)], ["fjunk", ("fst", i)], accum_out=fst[i][:, 0:1])
                              rsqrt_mean("dve", fst[i][:, 1:2], fst[i][:, 0:1], D, ("fst", i), ("fst", i))
                              stt("dve", fo[i][:, :], hn[i][:, :], fst[i][:, 1:2], gfin[:, :], ALU.mult, ALU.mult, [("dhn", i), ("fst", i), "gfin"], [("fo", i)])
                              dma("sp", out_d[s, tsl, :], fo[i][:, :], [("fo", i)], [("out", s, t)])

                      for t in range(NB + 2):
                          if t < NB:
                              d_gate(t)
                          if 1 <= t <= NB:
                              d_tr(t - 1)
                          if t >= 2:
                              d_out(t - 2)
                P.barrier()
        P.barrier()
        P.emit()
    return nc


_PROG_CACHE = {}


def kernel(x, attn_norm, w_in, diff_lambda, diff_norm, kv_norm, w_uk, w_uv, ret_norm, w_out, final_norm):
    x = np.asarray(x, dtype=np.float32)
    args = [np.asarray(a, dtype=np.float32) for a in (attn_norm, w_in, diff_lambda, diff_norm, kv_norm, w_uk, w_uv, ret_norm, w_out, final_norm)]
    w = _host_weights(*args)
    consts = _host_consts()
    B = x.shape[0]
    ns = B // NCORES
    if "nc" not in _PROG_CACHE:
        _PROG_CACHE["nc"] = build_program(DEPTH, ns)
    nc = _PROG_CACHE["nc"]
    in_maps = []
    for c in range(NCORES):
        m = {"x": np.ascontiguousarray(x[c * ns:(c + 1) * ns])}
        m.update(w)
        for k, v in consts.items():
            m["c_" + k] = v
        in_maps.append(m)
    res = run_bass_kernel_spmd(nc, in_maps, core_ids=list(range(NCORES)))
    return np.concatenate([r["out"] for r in res.results], axis=0)
```

```python
import math
import numpy as np
from contextlib import ExitStack
import concourse.bass as bass
import concourse.mybir as mybir
from concourse.bass_utils import run_bass_kernel_spmd

F32 = mybir.dt.float32
BF16 = mybir.dt.bfloat16
ALU = mybir.AluOpType
AF = mybir.ActivationFunctionType
AX = mybir.AxisListType

S = 2048
D = 1024
NB = S // 128
DEPTH = 4
NCORES = 8
EPS = 1e-6
NBIS = 14
NEGM = -30000.0
import os
_SKIP = os.environ.get('KSKIP', '')


class Prog:
    ENG = ("pe", "act", "dve", "pool", "sp")
    NDS = 12

    def __init__(self, nc, stack):
        self.nc = nc
        self.q = {e: [] for e in self.ENG}
        self.cnt = {e: 0 for e in self.ENG}
        self.sem = {e: stack.enter_context(nc.semaphore("prog_" + e)) for e in self.ENG}
        self.seen = {e: {f: 0 for f in self.ENG} for e in self.ENG}
        self.dseen = {e: {} for e in self.ENG}
        self.lastw = {}
        self.readers = {}
        self.dsem = {}
        self.drot = {}
        for qn in ("sp", "pool"):
            for i in range(self.NDS):
                nm = "%s%d" % (qn, i)
                self.dsem[nm] = [stack.enter_context(nc.semaphore("d_" + nm)), 0]
            self.drot[qn] = 0

    def _need(self, eng, dep, waits):
        if dep is None:
            return
        if dep[0] == "e":
            _, f, idx = dep
            if self.seen[eng][f] < idx:
                self.seen[eng][f] = idx
                waits[("e", f)] = (self.sem[f], idx)
        else:
            _, name, val = dep
            if self.dseen[eng].get(name, 0) < val:
                self.dseen[eng][name] = val
                waits[("d", name)] = (self.dsem[name][0], val)

    def _deps(self, eng, reads, writes):
        waits = {}
        for k in reads:
            self._need(eng, self.lastw.get(k), waits)
        for k in writes:
            lw = self.lastw.get(k)
            if lw is not None and not (lw[0] == "e" and lw[1] == eng):
                self._need(eng, lw, waits)
            for dep in self.readers.get(k, {}).values():
                if not (dep[0] == "e" and dep[1] == eng):
                    self._need(eng, dep, waits)
        return list(waits.values())

    def op(self, eng, fn, reads=(), writes=()):
        writes = list(writes) + [k for k in reads if isinstance(k, tuple) and k[0] == "ps" and k not in writes]
        waits = self._deps(eng, reads, writes)
        self.cnt[eng] += 1
        idx = self.cnt[eng]
        self.q[eng].append((waits, fn, (self.sem[eng], 1)))
        dep = ("e", eng, idx)
        for k in reads:
            self.readers.setdefault(k, {})[eng] = dep
        for k in writes:
            self.lastw[k] = dep
            self.readers[k] = {}
        return dep

    def dma(self, queue, fn, reads=(), writes=()):
        name = "%s%d" % (queue, self.drot[queue])
        self.drot[queue] = (self.drot[queue] + 1) % self.NDS
        waits = {}
        self._need(queue, ("d", name, self.dsem[name][1]), waits) if self.dsem[name][1] else None
        w2 = self._deps(queue, reads, writes)
        allw = list(waits.values()) + w2
        self.dsem[name][1] += 16
        val = self.dsem[name][1]
        self.q[queue].append((allw, fn, (self.dsem[name][0], 16)))
        dep = ("d", name, val)
        for k in writes:
            self.lastw[k] = dep
            self.readers[k] = {}
        for k in reads:
            self.readers.setdefault(k, {})["dma:" + name] = dep
        return dep

    def barrier(self):
        for e in self.ENG:
            waits = {}
            for f in self.ENG:
                if f != e and self.cnt[f]:
                    self._need(e, ("e", f, self.cnt[f]), waits)
            for name, (h, val) in self.dsem.items():
                if val:
                    self._need(e, ("d", name, val), waits)
            self.q[e].append((list(waits.values()), None, None))

    def emit(self):
        nc = self.nc
        q = self.q
        with nc.Block() as block:
            def replay(name):
                def run(e):
                    for waits, fn, inc in q[name]:
                        for s, v in waits:
                            e.wait_ge(s, v)
                        if fn is not None:
                            fn(e).then_inc(inc[0], inc[1])
                return run
            block.sync(replay("sp"))
            block.tensor(replay("pe"))
            block.scalar(replay("act"))
            block.vector(replay("dve"))
            block.gpsimd(replay("pool"))


class Rot:
    def __init__(self, items):
        self.items = list(items)
        self.i = 0

    def next(self):
        v = self.items[self.i % len(self.items)]
        self.i += 1
        return v


def _col_maps():
    o_dq, o_dk, o_dv, o_sq, o_ckv, o_iq, o_ik, o_iw, o_rq, o_rk, o_rv, o_gate = (
        0, 384, 768, 1152, 1536, 1664, 2176, 2240, 2248, 2504, 2760, 3016)
    colsF = []
    for base in (o_dq, o_dk, o_sq):
        for c in range(3):
            colsF.append(np.arange(base + 128 * c, base + 128 * c + 128))
    for c in range(4):
        colsF.append(np.arange(o_iq + 128 * c, o_iq + 128 * c + 128))
    colsF.append(np.concatenate([np.arange(o_ik, o_ik + 64)] * 2))

    def swap(base, c):
        out = []
        for hl in range(2):
            b = base + 128 * c + 64 * hl
            out += [np.arange(b + 32, b + 64), np.arange(b, b + 32)]
        return np.concatenate(out)
    for base in (o_rq, o_rk):
        for c in range(2):
            colsF.append(np.arange(base + 128 * c, base + 128 * c + 128))
        for c in range(2):
            colsF.append(swap(base, c))
    colsF = np.concatenate(colsF)
    colsT = np.concatenate([np.arange(o_dv, o_dv + 384), np.arange(o_ckv, o_ckv + 128),
                            np.arange(o_iw, o_iw + 8), np.arange(o_rv, o_rv + 256),
                            np.arange(o_gate, o_gate + 1024)])
    return colsF, colsT


NF = 22
T_DV = (0, 384)
T_CKV = (384, 520)
T_RV = (520, 776)
T_GATE = (776, 1800)
NT = 1800


def _host_consts():
    f32 = np.float32
    c = {}
    c["ident"] = np.eye(128, dtype=f32)
    r = np.arange(128)
    c["tri"] = (r[None, :] >= r[:, None]).astype(f32)
    c["cneg"] = np.where(r[None, :] <= r[:, None], 0.0, -1e30).astype(f32)
    inv_freq = (f32(10000.0) ** (-(np.arange(32, dtype=f32)) / f32(32))).astype(f32)
    ang = (np.arange(S, dtype=f32)[:, None] * inv_freq[None, :]).astype(f32)
    cos, sin = np.cos(ang).astype(f32), np.sin(ang).astype(f32)
    rr = np.arange(128)
    cosT = cos[:, rr % 32].T.copy()
    sgn = np.where((rr % 64) < 32, -1.0, 1.0).astype(f32)
    sinT = (sin[:, rr % 32].T * sgn[:, None]).astype(f32)
    c["cosT"], c["sinT"] = np.ascontiguousarray(cosT), np.ascontiguousarray(sinT)
    H = 4
    log_g = np.log(f32(1.0) - f32(2.0) ** (f32(-5.0) - np.arange(H, dtype=f32))).astype(f32)
    pos = np.arange(128, dtype=f32)
    diff = pos[:, None] - pos[None, :]
    d_intra = np.where(diff >= 0, np.exp(np.maximum(diff, 0.0)[None] * log_g[:, None, None]), 0.0).astype(f32)
    xi = np.exp((pos + 1.0)[:, None] * log_g[None, :]).astype(f32)
    zeta = np.exp((127.0 - pos)[:, None] * log_g[None, :]).astype(f32)
    gch = np.exp(128.0 * log_g).astype(f32)
    sc = f32(64.0 ** -0.5)
    c["dintraT"] = np.ascontiguousarray(np.transpose(d_intra, (2, 0, 1)) * sc).astype(f32)
    xiT = np.zeros((128, 2, 128), f32)
    zt = np.zeros((128, 2, 128), f32)
    gc = np.zeros((128, 2), f32)
    for cc in range(2):
        for hl in range(2):
            h = 2 * cc + hl
            xiT[hl * 64:(hl + 1) * 64, cc, :] = xi[None, :, h]
            zt[:, cc, hl * 64:(hl + 1) * 64] = (zeta[:, h] * sc)[:, None]
            gc[hl * 64:(hl + 1) * 64, cc] = gch[h]
    c["xiT"], c["zeta"], c["gch"] = xiT, zt, gc
    return c


_CONST_SHAPES = {"ident": [128, 128], "tri": [128, 128], "cneg": [128, 128], "cosT": [128, S], "sinT": [128, S],
                 "dintraT": [128, 4, 128], "xiT": [128, 2, 128], "zeta": [128, 2, 128], "gch": [128, 2]}


def _host_weights(attn_norm, w_in, diff_lambda, diff_norm, kv_norm, w_uk, w_uv, ret_norm, w_out, final_norm):
    L = w_in.shape[0]
    colsF, colsT = _col_maps()
    w = {}
    wf = w_in[:, :, colsF].reshape(L, 8, 128, NF, 128)
    w["wF"] = np.ascontiguousarray(np.transpose(wf, (0, 3, 2, 1, 4)))
    wt = w_in[:, :, colsT].reshape(L, 8, 128, NT)
    w["wT"] = np.ascontiguousarray(np.transpose(wt, (0, 2, 1, 3)))
    w["wout"] = np.ascontiguousarray(np.transpose(w_out.reshape(L, 8, 128, D), (0, 2, 1, 3)))
    uk = w_uk.reshape(L, 3, 2, 64, 128)
    w["wuk"] = np.ascontiguousarray(np.transpose(uk, (0, 2, 3, 1, 4)).reshape(L, 128, 3, 128))
    w["wuv"] = np.ascontiguousarray(np.transpose(w_uv, (0, 2, 1, 3)).reshape(L, 128, 384))
    w["gattn"] = np.ascontiguousarray(np.transpose(attn_norm.reshape(L, 8, 128), (0, 2, 1)))
    w["gkv"] = np.ascontiguousarray(kv_norm.reshape(L, 128, 1))
    w["gdiff"] = np.ascontiguousarray(diff_norm)
    w["gret"] = np.ascontiguousarray(ret_norm)
    w["gfin"] = np.ascontiguousarray(final_norm)
    w["dlam"] = np.ascontiguousarray(diff_lambda.reshape(L, 128))
    return w


def build_program(NL=DEPTH, NS=2, debug=False):
    nc = bass.Bass("TRN2", target_bir_lowering=False)
    L = NL
    dt = lambda name, shape, dtype=F32, kind="ExternalInput": nc.dram_tensor(name, shape, dtype, kind=kind).ap()
    x_d = dt("x", [NS, S, D])
    wF_d = dt("wF", [L, NF, 128, 8, 128])
    wT_d = dt("wT", [L, 128, 8, NT])
    wout_d = dt("wout", [L, 128, 8, D])
    wuk_d = dt("wuk", [L, 128, 3, 128])
    wuv_d = dt("wuv", [L, 128, 384])
    gattn_d = dt("gattn", [L, 128, 8])
    gkv_d = dt("gkv", [L, 128, 1])
    gdiff_d = dt("gdiff", [L, 64])
    gret_d = dt("gret", [L, 64])
    gfin_d = dt("gfin", [D])
    dlam_d = dt("dlam", [L, 128])
    cd = {k: dt("c_" + k, shp) for k, shp in _CONST_SHAPES.items()}
    out_d = dt("out", [NS, S, D], F32, "ExternalOutput")
    hbuf_d = dt("hbuf", [NS, S, D], F32, "Internal")
    mixed_d = dt("mixed", [NS, S, D], BF16, "Internal")
    dbg_d = dt("dbg", [NS, S, D], F32, "ExternalOutput") if debug else None

    with ExitStack() as st:
        P = Prog(nc, st)
        _uid = [0]

        def sbuf(stack, name, shape, dtype=F32):
            _uid[0] += 1
            return stack.enter_context(nc.sbuf_tensor("s%d_%s" % (_uid[0], name), shape, dtype))
        ps = [st.enter_context(nc.psum_tensor("ps%d" % i, [128, 512], F32)) for i in range(8)]
        pk = lambda b: ("ps", b)

        def mm(out, lhsT, rhs, start, stop, reads, writes, **kw):
            P.op("pe", lambda e: e.matmul(out, lhsT=lhsT, rhs=rhs, start=start, stop=stop,
                                          skip_group_check=True, **kw), reads, writes)

        def tr(out, in_, reads, writes):
            P.op("pe", lambda e: e.transpose(out, in_, ident_b[:, :]), list(reads) + ["ident_b"], writes)

        def act(out, in_, func, reads, writes, **kw):
            P.op("act", lambda e: e.activation(out=out, in_=in_, func=func, **kw), reads, writes)

        def ts(eng, out, in0, s1, s2, op0, op1, reads, writes, **kw):
            if op1 is None:
                P.op(eng, lambda e: e.tensor_scalar(out, in0, s1, None, op0=op0, **kw), reads, writes)
            else:
                P.op(eng, lambda e: e.tensor_scalar(out, in0, s1, s2, op0=op0, op1=op1, **kw), reads, writes)

        def tt(eng, out, in0, in1, op, reads, writes):
            P.op(eng, lambda e: e.tensor_tensor(out, in0, in1, op=op), reads, writes)

        def stt(eng, out, in0, scalar, in1, op0, op1, reads, writes):
            P.op(eng, lambda e: e.scalar_tensor_tensor(out, in0, scalar, in1, op0=op0, op1=op1), reads, writes)

        def cp(eng, out, in_, reads, writes):
            if eng == "act":
                P.op("act", lambda e: e.copy(out, in_), reads, writes)
            else:
                P.op(eng, lambda e: e.tensor_copy(out, in_), reads, writes)

        def recip(out, in_, reads, writes):
            P.op("dve", lambda e: e.reciprocal(out, in_), reads, writes)

        def reduce(out, in_, op, reads, writes):
            P.op("dve", lambda e: e.tensor_reduce(out, in_, axis=AX.X, op=op), reads, writes)

        def memset(eng, ap, val, writes):
            P.op(eng, lambda e: e.memset(ap, val), [], writes)

        def dma(queue, out, in_, reads, writes):
            return P.dma(queue, lambda e: e.dma_start(out=out, in_=in_), reads, writes)

        def rsqrt_mean(eng_unused, out, ss, n, key_out, key_ss):
            ts("dve", out, ss, 1.0 / n, EPS, ALU.mult, ALU.add, [key_ss], [key_out])
            act(out, out, AF.Sqrt, [key_out], [key_out])
            P.op("dve", lambda e: e.reciprocal(out, out), [key_out], [key_out])

        ident_b = sbuf(st, "ident_b", [128, 128], BF16)
        tri_b = sbuf(st, "tri_b", [128, 128], BF16)
        cneg = sbuf(st, "cneg", [128, 128])
        dintraT = sbuf(st, "dintraT", [128, 4, 128])
        xiT = sbuf(st, "xiT", [128, 2, 128])
        zeta = sbuf(st, "zeta", [128, 2, 128])
        gch = sbuf(st, "gch", [128, 2])
        gattn = sbuf(st, "gattn", [128, L, 8])
        gkv = sbuf(st, "gkv", [128, L, 1])
        gdm = sbuf(st, "gdm", [128, L, 64])
        gret = sbuf(st, "gret", [128, L, 64])
        lamneg = sbuf(st, "lamneg", [128, L, 1])
        lamt = sbuf(st, "lamt", [128, 4])
        uT = sbuf(st, "uT", [128, 8, S], BF16)
        tmp0 = ExitStack()
        dlam = sbuf(tmp0, "dlam", [128, L, 128])

        dma("pool", ident_b[:, :], cd["ident"][:, :], [], ["ident_b"])
        dma("pool", tri_b[:, :], cd["tri"][:, :], [], ["tri_b"])
        for nm, t in (("cneg", cneg), ("gch", gch)):
            dma("sp", t[:, :], cd[nm][:, :], [], [nm])
        for nm, t in (("dintraT", dintraT), ("xiT", xiT), ("zeta", zeta)):
            dma("sp", t[:, :, :], cd[nm][:, :, :], [], [nm])
        for l in range(L):
            dma("sp", gattn[:, l, :], gattn_d[l, :, :], [], ["gattn"])
            dma("sp", gkv[:, l, :], gkv_d[l, :, :], [], ["gkv"])
            dma("sp", gdm[:, l, :], gdiff_d[l, :].partition_broadcast(128), [], ["gdm"])
            dma("sp", gret[:, l, :], gret_d[l, :].partition_broadcast(128), [], ["gret"])
            dma("sp", dlam[:, l, :], dlam_d[l, :].partition_broadcast(128), [], ["dlam"])
        for l in range(L):
            lam_init = 0.8 - 0.6 * math.exp(-0.3 * l)
            junk = dlam[:, l, 0:32]
            tt("dve", dlam[:, l, 0:32], dlam[:, l, 0:32], dlam[:, l, 32:64], ALU.mult, ["dlam"], ["dlam"])
            tt("dve", dlam[:, l, 64:96], dlam[:, l, 64:96], dlam[:, l, 96:128], ALU.mult, ["dlam"], ["dlam"])
            reduce(lamt[:, 0:1], dlam[:, l, 0:32], ALU.add, ["dlam"], ["lamt"])
            reduce(lamt[:, 1:2], dlam[:, l, 64:96], ALU.add, ["dlam", "lamt"], ["lamt"])
            act(lamt[:, 2:4], lamt[:, 0:2], AF.Exp, ["lamt"], ["lamt"])
            tt("dve", lamt[:, 0:1], lamt[:, 3:4], lamt[:, 2:3], ALU.subtract, ["lamt"], ["lamt"])
            ts("dve", lamneg[:, l, :], lamt[:, 0:1], -lam_init, None, ALU.add, None, ["lamt"], ["lamneg"])
            ts("dve", gdm[:, l, :], gdm[:, l, :], 1.0 - lam_init, None, ALU.mult, None, ["gdm"], ["gdm"])

        P.barrier()
        tmp0.close()
        wrotF = Rot(range(3))
        wrotT = Rot(range(2))
        evrot = Rot(["act", "dve"])

        def h_src(l, s, t):
            src = x_d if l == 0 else hbuf_d
            return src[s, t * 128:(t + 1) * 128, :]

        def hkey(s, t):
            return ("h", s, t)

        for l in range(L):
            last_layer = (l == L - 1)
            for s in range(NS):
                with ExitStack() as ph:
                    hb = [sbuf(ph, "p0_h%d" % i, [128, D]) for i in range(2)]
                    hn = [sbuf(ph, "p0_hn%d" % i, [128, D], BF16) for i in range(2)]
                    sq_junk = sbuf(ph, "p0_junk", [128, D], BF16)
                    st0 = [sbuf(ph, "p0_st%d" % i, [128, 2]) for i in range(2)]
                    brot = Rot(range(8))
                    gat3 = sbuf(ph, "p0_g3", [128, 8, 1])
                    cp("dve", gat3[:, :, :].rearrange("p k o -> p (k o)"), gattn[:, l, :], ["gattn"], ["gat3"])

                    def p0_stage1(t):
                        i = t % 2
                        dma("sp", hb[i][:, :], h_src(l, s, t), [hkey(s, t)], [("hb", i)])
                        act(sq_junk[:, :], hb[i][:, :], AF.Square, [("hb", i)], ["sqj", ("st0", i)], accum_out=st0[i][:, 0:1])
                        rsqrt_mean("dve", st0[i][:, 1:2], st0[i][:, 0:1], D, ("st0", i), ("st0", i))
                        act(hn[i][:, :], hb[i][:, :], AF.Copy, [("hb", i), ("st0", i)], [("hn", i)], scale=st0[i][:, 1:2])

                    def p0_stage2(t):
                        i = t % 2
                        b = brot.next()
                        pv = ps[b][:, :].bitcast(BF16)
                        for k in range(8):
                            tr(pv[:, k * 128:(k + 1) * 128], hn[i][:, k * 128:(k + 1) * 128], [("hn", i)], [pk(b)])
                        tt("dve", uT[:, :, t * 128:(t + 1) * 128], pv[:, :].rearrange("p (k q) -> p k q", q=128),
                           gat3[:, :, :].to_broadcast([128, 8, 128]), ALU.mult, [pk(b), "gat3"], ["uT"])
                    for t in range(NB + 1):
                        if t < NB:
                            p0_stage1(t)
                        if t >= 1:
                            p0_stage2(t - 1)
                P.barrier()

                def load_wF(wtiles, f):
                    i = wrotF.next()
                    dma("pool", wtiles[i][:, :, :], wF_d[l, f, :, :, :], [], [("wF", i)])
                    return i

                def proj_F(wtiles, f, evac):
                    i = load_wF(wtiles, f)
                    for tg in range(4):
                        b = brotP.next()
                        for k in range(8):
                            mm(ps[b][:, :], wtiles[i][:, k, :], uT[:, k, tg * 512:(tg + 1) * 512], k == 0, k == 7,
                               [("wF", i), "uT"], [pk(b)])
                        evac(tg, ps[b][:, :], b)

                def proj_T(wt, wkey, ncols, t, c0=0):
                    b = brotP.next()
                    for k in range(8):
                        mm(ps[b][:, 0:ncols], uT[:, k, t * 128:(t + 1) * 128], wt[:, k, c0:c0 + ncols], k == 0, k == 7,
                           [wkey, "uT"], [pk(b)])
                    return b

                def evac_copy(dst3, c):
                    def f(tg, pa, b):
                        ev = evrot.next()
                        cp(ev, dst3[:, c, tg * 512:(tg + 1) * 512], pa, [pk(b)], [dst3.name if hasattr(dst3, "name") else "x"])
                    return f

                with ExitStack() as ph:
                  if "A" in _SKIP:
                    pass
                  else:
                      brotP = Rot(range(8))
                      wFt = [sbuf(ph, "a_wF%d" % i, [128, 8, 128], BF16) for i in range(3)]
                      wTt = sbuf(ph, "a_wT", [128, 8, 384], BF16)
                      dqT = sbuf(ph, "a_dqT", [128, 3, S], BF16)
                      dkT = sbuf(ph, "a_dkT", [128, 3, S], BF16)
                      dva = sbuf(ph, "a_dva", [128, NB, 6, 65], BF16)
                      pt = [sbuf(ph, "a_pt%d" % i, [128, 512], BF16) for i in range(8)]
                      rr = [sbuf(ph, "a_rr%d" % i, [128, 4, 1]) for i in range(3)]
                      tA = sbuf(ph, "a_tA", [128, 4, 64])
                      tB = sbuf(ph, "a_tB", [128, 4, 64])
                      tO = sbuf(ph, "a_tO", [128, 4, 64])
                      tS = sbuf(ph, "a_tS", [128, 4, 64])
                      ss4 = sbuf(ph, "a_ss4", [128, 4, 1])
                      oa = [sbuf(ph, "a_oa%d" % i, [128, 4, 384], BF16) for i in range(2)]

                      dma("pool", wTt[:, :, :], wT_d[l, :, :, T_DV[0]:T_DV[1]], [], ["a_wT"])
                      memset("pool", dva[:, :, :, 64:65], 1.0, ["dva"])
                      for c in range(3):
                          def ev_q(tg, pa, b, c=c):
                              cp(evrot.next(), dqT[:, c, tg * 512:(tg + 1) * 512], pa, [pk(b)], ["dqT"])
                          proj_F(wFt, c, ev_q)
                      for c in range(3):
                          def ev_k(tg, pa, b, c=c):
                              cp(evrot.next(), dkT[:, c, tg * 512:(tg + 1) * 512], pa, [pk(b)], ["dkT"])
                          proj_F(wFt, 3 + c, ev_k)
                      for t in range(NB):
                          b = proj_T(wTt, "a_wT", 384, t)
                          cp(evrot.next(), dva[:, t, :, 0:64], ps[b][:, 0:384].rearrange("p (h e) -> p h e", e=64), [pk(b)], ["dva"])

                      scrot = Rot([0, 1, 2, 3])
                      accrot = Rot([(4, 5), (6, 7)])
                      ptrot = Rot(range(8))
                      scale = 32.0 ** -0.5

                      def a_epilogue(I, h, banks):
                          oab = oa[I % 2]
                          oak = ("oa", I % 2)
                          a0 = ps[banks[0]][:, 0:260].rearrange("p (i e) -> p i e", e=65)
                          a1 = ps[banks[1]][:, 0:260].rearrange("p (i e) -> p i e", e=65)
                          recip(rr[0][:, :, :], a0[:, :, 64:65], [pk(banks[0])], ["rr0"])
                          recip(rr[1][:, :, :], a1[:, :, 64:65], [pk(banks[1])], ["rr1"])
                          ts("dve", rr[2][:, :, :], rr[1][:, :, :], lamneg[:, l, :], None, ALU.mult, None, ["rr1", "lamneg"], ["rr2"])
                          tt("dve", tA[:, :, :], a0[:, :, 0:64], rr[0][:, :, :].to_broadcast([128, 4, 64]), ALU.mult, [pk(banks[0]), "rr0"], ["tA"])
                          tt("dve", tB[:, :, :], a1[:, :, 0:64], rr[2][:, :, :].to_broadcast([128, 4, 64]), ALU.mult, [pk(banks[1]), "rr2"], ["tB"])
                          tt("pool", tO[:, :, :], tA[:, :, :], tB[:, :, :], ALU.add, ["tA", "tB"], ["tO"])
                          tt("pool", tS[:, :, :], tO[:, :, :], tO[:, :, :], ALU.mult, ["tO"], ["tS"])
                          reduce(ss4[:, :, :], tS[:, :, :], ALU.add, ["tS"], ["ss4"])
                          rsqrt_mean("dve", ss4[:, :, :], ss4[:, :, :], 64, "ss4", "ss4")
                          tt("pool", tO[:, :, :], tO[:, :, :], ss4[:, :, :].to_broadcast([128, 4, 64]), ALU.mult, ["tO", "ss4"], ["tO"])
                          tt("pool", oab[:, :, h * 64:(h + 1) * 64], tO[:, :, :], gdm[:, l:l + 1, :].to_broadcast([128, 4, 64]), ALU.mult, ["tO", "gdm"], [oak])
                          if h == 5:
                              for i4 in range(4):
                                  t = 4 * I + i4
                                  dma("sp", mixed_d[s, t * 128:(t + 1) * 128, 0:384], oab[:, i4, :], [oak], [("mx", s, t, 0)])

                      tiles = []
                      for I in range(4):
                          for h in range(6):
                              banks = accrot.next()
                              nj = 4 * I + 4
                              for j in range(nj):
                                  for c2 in range(2):
                                      tiles.append(dict(I=I, h=h, c2=c2, j=j, ab=banks[c2], banks=banks, first=(j == 0),
                                                        last=(c2 == 1 and j == nj - 1)))

                      def a_score(T):
                          I, h, c2, j = T["I"], T["h"], T["c2"], T["j"]
                          r0 = (h % 2) * 64 + c2 * 32
                          kw = dict(tile_position=(96, 0)) if r0 == 96 else {}
                          i0 = max(j, 4 * I)
                          w = (4 * I + 4 - i0) * 128
                          sb_ = scrot.next()
                          pi = ptrot.next()
                          T.update(i0=i0, w=w, pi=pi)
                          mm(ps[sb_][:, 0:w], dkT[r0:r0 + 32, h // 2, j * 128:(j + 1) * 128],
                             dqT[r0:r0 + 32, h // 2, i0 * 128:(4 * I + 4) * 128], True, True,
                             ["dkT", "dqT"], [pk(sb_)], **kw)
                          act(pt[pi][:, 0:w], ps[sb_][:, 0:w], AF.Exp, [pk(sb_)], [("pt", pi)], scale=scale)
                          if j >= 4 * I:
                              tt("pool", pt[pi][:, 0:128], pt[pi][:, 0:128], tri_b[:, :], ALU.mult, [("pt", pi), "tri_b"], [("pt", pi)])

                      def a_av(T):
                          I, h, j, i0, pi, ab = T["I"], T["h"], T["j"], T["i0"], T["pi"], T["ab"]
                          for i in range(i0, 4 * I + 4):
                              mm(ps[ab][:, (i - 4 * I) * 65:(i - 4 * I) * 65 + 65], pt[pi][:, (i - i0) * 128:(i - i0 + 1) * 128],
                                 dva[:, j, h, :], T["first"] and i == i0, j == i, [("pt", pi), "dva"], [pk(ab)])
                          if T["last"]:
                              a_epilogue(I, h, T["banks"])

                      DEP = 4
                      for idx in range(len(tiles) + DEP):
                          if idx < len(tiles):
                              a_score(tiles[idx])
                          if idx >= DEP:
                              a_av(tiles[idx - DEP])
                P.barrier()

                with ExitStack() as ph:
                  if "B" in _SKIP:
                    pass
                  else:
                      brotP = Rot(range(8))
                      qlT = sbuf(ph, "b_qlT", [128, 6, S], BF16)
                      ckvT = sbuf(ph, "b_ckvT", [128, S], BF16)
                      vpa = sbuf(ph, "b_vpa", [128, NB, 6, 65], BF16)
                      iqT = sbuf(ph, "b_iqT", [128, 4, S], BF16)
                      ikT = sbuf(ph, "b_ikT", [128, S], BF16)
                      iw3 = sbuf(ph, "b_iw", [128, NB, 8, 1])
                      iw = iw3[:, :, :, :].rearrange("p t h o -> p t (h o)")
                      ident3 = sbuf(ph, "b_ident3", [128, 1, 128], BF16)
                      cp("pool", ident3[:, 0, :], ident_b[:, :], ["ident_b"], ["ident3"])
                      ph2 = ExitStack()
                      wFt = [sbuf(ph2, "b_wF%d" % i, [128, 8, 128], BF16) for i in range(3)]
                      wTt = sbuf(ph2, "b_wT", [128, 8, 256], BF16)
                      sqt = [sbuf(ph2, "b_sqt%d" % i, [128, 512], BF16) for i in range(2)]
                      ckn = [sbuf(ph2, "b_ckn%d" % i, [128, 128], BF16) for i in range(3)]
                      cst = [sbuf(ph2, "b_cst%d" % i, [128, 2]) for i in range(3)]
                      cjunk = sbuf(ph2, "b_cjunk", [128, 128], BF16)
                      wuk_b = sbuf(ph2, "b_wuk", [128, 1, 3, 128], BF16)
                      wuv_b = sbuf(ph2, "b_wuv", [128, 1, 384], BF16)

                      dma("pool", wTt[:, :, :], wT_d[l, :, :, T_CKV[0]:T_CKV[0] + 256], [], ["b_wT"])
                      dma("pool", wuk_b[:, 0, :, :], wuk_d[l, :, :, :], [], ["wuk_b"])
                      dma("pool", wuv_b[:, 0, :], wuv_d[l, :, :], [], ["wuv_b"])
                      memset("pool", vpa[:, :, :, 64:65], 1.0, ["vpa"])
                      sqrot = Rot(range(2))
                      for c in range(3):
                          def ev_sq(tg, pa, b, c=c):
                              si = sqrot.next()
                              cp(evrot.next(), sqt[si][:, :], pa, [pk(b)], [("sqt", si)])
                              for hl in range(2):
                                  b2 = brotP.next()
                                  mm(ps[b2][:, :], wuk_b[hl * 64:(hl + 1) * 64, 0, c, :], sqt[si][hl * 64:(hl + 1) * 64, :], True, True,
                                     ["wuk_b", ("sqt", si)], [pk(b2)])
                                  cp(evrot.next(), qlT[:, 2 * c + hl, tg * 512:(tg + 1) * 512], ps[b2][:, :], [pk(b2)], ["qlT"])
                          if '1' not in _SKIP:
                              proj_F(wFt, 6 + c, ev_sq)
                      for c in range(4):
                          def ev_iq(tg, pa, b, c=c):
                              cp(evrot.next(), iqT[:, c, tg * 512:(tg + 1) * 512], pa, [pk(b)], ["iqT"])
                          if '2' not in _SKIP:
                              proj_F(wFt, 9 + c, ev_iq)

                      def ev_ik(tg, pa, b):
                          cp(evrot.next(), ikT[:, tg * 512:(tg + 1) * 512], pa, [pk(b)], ["ikT"])
                      if '3' not in _SKIP:
                          proj_F(wFt, 13, ev_ik)
                      def ckv_s1(t):
                          i = t % 3
                          b = proj_T(wTt, "b_wT", 136, t)
                          cp("dve", iw[:, t, :], ps[b][:, 128:136], [pk(b)], ["iw"])
                          act(cjunk[:, :], ps[b][:, 0:128], AF.Square, [pk(b)], ["cjunk", ("cst", i)], accum_out=cst[i][:, 0:1])
                          rsqrt_mean("dve", cst[i][:, 1:2], cst[i][:, 0:1], 128, ("cst", i), ("cst", i))
                          ts("dve", ckn[i][:, :], ps[b][:, 0:128], cst[i][:, 1:2], None, ALU.mult, None, [pk(b), ("cst", i)], [("ckn", i)])

                      def ckv_s2(t):
                          i = t % 3
                          b2 = brotP.next()
                          pv = ps[b2][:, :].bitcast(BF16)
                          tr(pv[:, 0:128], ckn[i][:, :], [("ckn", i)], [pk(b2)])
                          act(ckvT[:, t * 128:(t + 1) * 128], pv[:, 0:128], AF.Copy, [pk(b2), "gkv"], [("ckvT", t), "ckvT"], scale=gkv[:, l, :])

                      def ckv_s3(t):
                          b3 = brotP.next()
                          mm(ps[b3][:, 0:384], ckvT[:, t * 128:(t + 1) * 128], wuv_b[:, 0, :], True, True, [("ckvT", t), "wuv_b"], [pk(b3)])
                          cp(evrot.next(), vpa[:, t, :, 0:64], ps[b3][:, 0:384].rearrange("p (h e) -> p h e", e=64), [pk(b3)], ["vpa"])

                      for t in range(NB + 2):
                          if t < NB:
                              ckv_s1(t)
                          if 1 <= t <= NB:
                              ckv_s2(t - 1)
                          if t >= 2:
                              ckv_s3(t - 2)

                      P.barrier()
                      ph2.close()
                      SCW = [(8 + i + 1) * 128 for i in range(4)] + [(12 + i + 1) * 128 for i in range(4)]
                      score = [sbuf(ph, "b_score%d" % i, [128, SCW[i]]) for i in range(8)]
                      negm = [sbuf(ph, "b_negm%d" % i, [128, SCW[i]], BF16) for i in range(8)]
                      rel = [sbuf(ph, "b_rel%d" % i, [128, 512], BF16) for i in range(6)]
                      dg = [sbuf(ph, "b_dg%d" % i, [128, 8, 128], BF16) for i in range(4)]
                      bis = [sbuf(ph, "b_bis%d" % i, [128, 8]) for i in range(8)]
                      bjunk = sbuf(ph, "b_bjunk", [128, S], BF16)
                      pt = [sbuf(ph, "b_pt%d" % i, [128, 512], BF16) for i in range(4)]
                      acs = [sbuf(ph, "b_acs%d" % i, [128, 4, 65]) for i in range(2)]
                      acsrot = Rot(range(2))
                      rrp = [sbuf(ph, "b_rrp%d" % i, [128, 4, 1]) for i in range(2)]
                      negone = sbuf(ph, "b_negone", [128, 4, 1])
                      memset("pool", negone[:, :, :], -1.0, ["negone"])
                      ob = [sbuf(ph, "b_ob%d" % i, [128, 4, 384], BF16) for i in range(2)]
                      relbank = Rot([0, 1, 2, 3])
                      idxacc = Rot([4])
                      relrot = Rot(range(6))
                      scrot = Rot([5, 6])
                      accrot = Rot([7])
                      ptrot = Rot(range(4))
                      scale = 64.0 ** -0.5

                      QORD = [1, 3, 2, 0]
                      QPAR = {1: 0, 3: 1, 2: 0, 0: 1}

                      def index_phase(I):
                          par = QPAR[I]
                          chains = []
                          for i4 in range(4):
                              i = 4 * I + i4
                              if i >= 2:
                                  tt("pool", dg[i4][:, :, :], ident3[:, :, :].to_broadcast([128, 8, 128]), iw3[:, i, :, :].to_broadcast([128, 8, 128]),
                                     ALU.mult, ["ident3", "iw"], [("dg", i4)])
                          for i4 in range(4):
                              i = 4 * I + i4
                              n = (i + 1) * 128
                              si_ = par * 4 + i4
                              sk = ("score", si_)
                              bk = ("bis", si_)
                              bs = bis[si_]
                              if i < 2 or 'i' in _SKIP:
                                  memset("dve", score[si_][:, 0:n], 0.0, [sk])
                                  tt("dve", score[si_][:, n - 128:n], score[si_][:, n - 128:n], cneg[:, :], ALU.add, [sk, "cneg"], [sk])
                                  memset("dve", bs[:, 0:1], -1e29, [bk])
                              else:
                                  dgi = i4
                                  for kc in range((n + 511) // 512):
                                      w = min(512, n - kc * 512)
                                      ab = idxacc.next()
                                      pend = []
                                      for hp in range(5):
                                          cur = []
                                          if hp < 4:
                                              for hl in range(2):
                                                  hh = 2 * hp + hl
                                                  rb = relbank.next()
                                                  r0 = hl * 64
                                                  mm(ps[rb][:, 0:w], iqT[r0:r0 + 64, hp, i * 128:(i + 1) * 128], ikT[r0:r0 + 64, kc * 512:kc * 512 + w],
                                                     True, True, ["iqT", "ikT"], [pk(rb)])
                                                  ri = relrot.next()
                                                  act(rel[ri][:, 0:w], ps[rb][:, 0:w], AF.Relu, [pk(rb)], [("rel", ri)])
                                                  cur.append((hh, ri))
                                          for (ph_, pri) in pend:
                                              mm(ps[ab][:, 0:w], dg[dgi][:, ph_, :], rel[pri][:, 0:w], ph_ == 0, ph_ == 7, [("dg", dgi), ("rel", pri)], [pk(ab)])
                                          pend = cur
                                      cp("act", score[si_][:, kc * 512:kc * 512 + w], ps[ab][:, 0:w], [pk(ab)], [sk])
                                  tt("pool", score[si_][:, n - 128:n], score[si_][:, n - 128:n], cneg[:, :], ALU.add, [sk, "cneg"], [sk])
                                  reduce(bs[:, 5:6], score[si_][:, 0:n], ALU.max, [sk], [bk])
                                  reduce(bs[:, 0:1], score[si_][:, 0:256], ALU.min, [sk, bk], [bk])
                                  tt("dve", bs[:, 1:2], bs[:, 5:6], bs[:, 0:1], ALU.subtract, [bk], [bk])
                                  chains.append((si_, n, sk, bk, bs))
                          for it in range(NBIS):
                              for (si_, n, sk, bk, bs) in chains:
                                  ts("dve", bs[:, 2:3], bs[:, 1:2], 0.5 ** (it + 1), bs[:, 0:1], ALU.mult, ALU.add, [bk], [bk])
                              for (si_, n, sk, bk, bs) in chains:
                                  ts("dve", bjunk[:, 0:n], score[si_][:, 0:n], bs[:, 2:3], 0.0, ALU.is_ge, ALU.add, [sk, bk], ["bjunk", bk], accum_out=bs[:, 3:4])
                              for (si_, n, sk, bk, bs) in chains:
                                  ts("dve", bs[:, 4:5], bs[:, 3:4], 255.5, 1e30, ALU.is_lt, ALU.mult, [bk], [bk])
                              for (si_, n, sk, bk, bs) in chains:
                                  stt("dve", bs[:, 0:1], bs[:, 2:3], bs[:, 4:5], bs[:, 0:1], ALU.subtract, ALU.max, [bk], [bk])
                          for i4 in range(4):
                              n = (4 * I + i4 + 1) * 128
                              si_ = par * 4 + i4
                              ts("dve", negm[si_][:, 0:n], score[si_][:, 0:n], bis[si_][:, 0:1], NEGM, ALU.is_lt, ALU.mult,
                                 [("score", si_), ("bis", si_)], [("negm", si_)])

                      def d_epilogue(I, h, ab):
                          obb = ob[I % 2]
                          obk = ("ob", I % 2)
                          a0 = ps[ab][:, 0:260].rearrange("p (i e) -> p i e", e=65)
                          ai = acsrot.next()
                          cp("act", acs[ai][:, :, :], a0, [pk(ab)], [("acs", ai)])
                          tt("pool", rrp[ai][:, :, :], acs[ai][:, :, 64:65], negone[:, :, :], ALU.pow, [("acs", ai), "negone"], [("rrp", ai)])
                          tt("pool", obb[:, :, h * 64:(h + 1) * 64], acs[ai][:, :, 0:64], rrp[ai][:, :, :].to_broadcast([128, 4, 64]), ALU.mult,
                             [("acs", ai), ("rrp", ai)], [obk])
                          if h == 5:
                              for i4 in range(4):
                                  t = 4 * I + i4
                                  dma("sp", mixed_d[s, t * 128:(t + 1) * 128, 384:768], obb[:, i4, :], [obk], [("mx", s, t, 1)])

                      def d_score(T):
                          I, h, j = T["I"], T["h"], T["j"]
                          par = QPAR[I]
                          i0 = max(j, 4 * I)
                          w = (4 * I + 4 - i0) * 128
                          sb_ = scrot.next()
                          pi = ptrot.next()
                          T.update(i0=i0, w=w, pi=pi)
                          mm(ps[sb_][:, 0:w], ckvT[:, j * 128:(j + 1) * 128], qlT[:, h, i0 * 128:(4 * I + 4) * 128], True, False,
                             ["ckvT", "qlT"], [pk(sb_)])
                          for i in range(i0, 4 * I + 4):
                              i4 = i - 4 * I
                              mm(ps[sb_][:, (i - i0) * 128:(i - i0 + 1) * 128], negm[par * 4 + i4][:, j * 128:(j + 1) * 128], ident_b[:, :], False,
                                 i == 4 * I + 3, [("negm", par * 4 + i4), "ident_b"], [pk(sb_)])
                          act(pt[pi][:, 0:w], ps[sb_][:, 0:w], AF.Exp, [pk(sb_)], [("pt", pi)], scale=scale)

                      def d_av(T):
                          I, h, j, i0, pi, ab = T["I"], T["h"], T["j"], T["i0"], T["pi"], T["ab"]
                          for i in range(i0, 4 * I + 4):
                              mm(ps[ab][:, (i - 4 * I) * 65:(i - 4 * I) * 65 + 65], pt[pi][:, (i - i0) * 128:(i - i0 + 1) * 128],
                                 vpa[:, j, h, :], T["first"] and i == i0, j == i, [("pt", pi), "vpa"], [pk(ab)])
                          if T["last"]:
                              d_epilogue(I, h, ab)

                      def dsa_phase(I):
                          tiles = []
                          for h in range(6):
                              ab = accrot.next()
                              nj = 4 * I + 4
                              for j in range(nj):
                                  tiles.append(dict(I=I, h=h, j=j, ab=ab, first=(j == 0), last=(j == nj - 1)))
                          DEP = 1
                          for idx in range(len(tiles) + DEP):
                              if idx < len(tiles):
                                  d_score(tiles[idx])
                              if idx >= DEP:
                                  d_av(tiles[idx - DEP])

                      for pos, I in enumerate(QORD):
                          if pos == 0:
                              index_phase(I)
                          if pos + 1 < 4:
                              index_phase(QORD[pos + 1])
                          dsa_phase(I)
                P.barrier()

                with ExitStack() as ph:
                  if "C" in _SKIP:
                    pass
                  else:
                      brotP = Rot(range(8))
                      wFt = [sbuf(ph, "c_wF%d" % i, [128, 8, 128], BF16) for i in range(3)]
                      wTt = sbuf(ph, "c_wT", [128, 8, 256], BF16)
                      rT = [sbuf(ph, "c_rqT", [128, 2, S], BF16), sbuf(ph, "c_rkT", [128, 2, S], BF16)]
                      rv = sbuf(ph, "c_rv", [128, NB, 256], BF16)
                      t1 = sbuf(ph, "c_t1", [128, 2, S])
                      t2 = [sbuf(ph, "c_t2_%d" % i, [128, 512]) for i in range(2)]
                      kz = [sbuf(ph, "c_kz%d" % i, [128, 128], BF16) for i in range(4)]
                      qxi = [sbuf(ph, "c_qxi%d" % i, [128, 128], BF16) for i in range(4)]
                      attD = [sbuf(ph, "c_attD%d" % i, [128, 128], BF16) for i in range(4)]
                      Rf = sbuf(ph, "c_Rf", [128, 2, 64])
                      Rb = sbuf(ph, "c_Rb", [128, 2, 64], BF16)
                      oc = [sbuf(ph, "c_oc%d" % i, [128, 4, 64]) for i in range(2)]
                      ocs = sbuf(ph, "c_ocs", [128, 4, 64])
                      ocb = [sbuf(ph, "c_ocb%d" % i, [128, 4, 64], BF16) for i in range(2)]
                      ss4 = sbuf(ph, "c_ss4", [128, 4, 1])

                      dma("pool", wTt[:, :, :], wT_d[l, :, :, T_RV[0]:T_RV[1]], [], ["c_wT"])
                      cosT = sbuf(ph, "c_cosT", [128, S])
                      sinT = sbuf(ph, "c_sinT", [128, S])
                      dma("sp", cosT[:, :], cd["cosT"][:, :], [], ["cosT"])
                      dma("sp", sinT[:, :], cd["sinT"][:, :], [], ["sinT"])
                      for qk in range(2):
                          for c in range(2):
                              def ev_x(tg, pa, b, c=c, qk=qk):
                                  i = tg % 2
                                  tt("dve", t1[:, c, tg * 512:(tg + 1) * 512], pa, cosT[:, tg * 512:(tg + 1) * 512], ALU.mult, [pk(b), "cosT"], [("t1", tg, c)])
                              proj_F(wFt, 14 + 4 * qk + c, ev_x)
                          for c in range(2):
                              def ev_xs(tg, pa, b, c=c, qk=qk):
                                  i = tg % 2
                                  tt("dve", t2[i][:, :], pa, sinT[:, tg * 512:(tg + 1) * 512], ALU.mult, [pk(b), "sinT"], [("t2", i)])
                                  tt("pool", rT[qk][:, c, tg * 512:(tg + 1) * 512], t1[:, c, tg * 512:(tg + 1) * 512], t2[i][:, :], ALU.add, [("t1", tg, c), ("t2", i)], [("rT", qk)])
                              proj_F(wFt, 16 + 4 * qk + c, ev_xs)
                      for t in range(NB):
                          b = proj_T(wTt, "c_wT", 256, t)
                          cp(evrot.next(), rv[:, t, :], ps[b][:, 0:256], [pk(b)], ["rv"])

                      memset("dve", Rf[:, :, :], 0.0, ["Rf"])
                      memset("dve", Rb[:, :, :], 0.0, ["Rb"])
                      brot = Rot([0, 1])
                      adrot = Rot(range(4))
                      obanks = [(4, 5), (2, 3)]
                      rbk = [6, 7]

                      def c_stage1(t):
                          tsl = slice(t * 128, (t + 1) * 128)
                          p2 = t % 2
                          obp = obanks[p2]
                          firsts = [True, True]
                          for c in range(2):
                              b = brot.next()
                              pv = ps[b][:, :].bitcast(BF16)
                              tr(pv[:, 0:128], rT[1][:, c, tsl], [("rT", 1)], [pk(b)])
                              tt("dve", kz[p2 * 2 + c][:, :], pv[:, 0:128], zeta[:, c, :], ALU.mult, [pk(b), "zeta"], [("kz", p2, c)])
                              tt("pool", qxi[p2 * 2 + c][:, :], rT[0][:, c, tsl], xiT[:, c, :], ALU.mult, [("rT", 0), "xiT"], [("qxi", p2, c)])
                              for hl in range(2):
                                  h = 2 * c + hl
                                  rows = slice(hl * 64, (hl + 1) * 64)
                                  b = brot.next()
                                  mm(ps[b][:, 0:128], rT[1][rows, c, tsl], rT[0][rows, c, tsl], True, True, [("rT", 1), ("rT", 0)], [pk(b)])
                                  ai = adrot.next()
                                  tt("dve", attD[ai][:, :], ps[b][:, 0:128], dintraT[:, h, :], ALU.mult, [pk(b), "dintraT"], [("attD", ai)])
                                  mm(ps[obp[hl]][:, c * 64:(c + 1) * 64], attD[ai][:, :], rv[:, t, h * 64:(h + 1) * 64], firsts[hl], t == 0,
                                     [("attD", ai), "rv"], [pk(obp[hl])])
                                  firsts[hl] = False
                              if t < NB - 1:
                                  rb_ = rbk[p2]
                                  mm(ps[rb_][:, c * 128:(c + 1) * 128], kz[p2 * 2 + c][:, :], rv[:, t, c * 128:(c + 1) * 128], True, True, [("kz", p2, c), "rv"], [pk(rb_)])

                      def c_stage2(t):
                          tsl = slice(t * 128, (t + 1) * 128)
                          p2 = t % 2
                          obp = obanks[p2]
                          if t > 0:
                              for hl in range(2):
                                  for c in range(2):
                                      rows = slice(hl * 64, (hl + 1) * 64)
                                      mm(ps[obp[hl]][:, c * 64:(c + 1) * 64], qxi[p2 * 2 + c][rows, :], Rb[rows, c, :], False, True,
                                         [("qxi", p2, c), "Rb"], [pk(obp[hl])])
                          oi = p2
                          ocv = oc[oi][:, :, :].rearrange("p (c hl) e -> p c hl e", hl=2)
                          for hl in range(2):
                              cp("act", ocv[:, :, hl, :], ps[obp[hl]][:, 0:128].rearrange("p (c e) -> p c e", e=64), [pk(obp[hl])], [("oc", oi)])
                          if t < NB - 1:
                              for c in range(2):
                                  for hl in range(2):
                                      rows = slice(hl * 64, (hl + 1) * 64)
                                      stt("dve", Rf[rows, c, :], Rf[rows, c, :], gch[rows, c:c + 1], ps[rbk[p2]][rows, c * 128 + hl * 64:c * 128 + (hl + 1) * 64],
                                          ALU.mult, ALU.add, ["Rf", "gch", pk(rbk[p2])], ["Rf"])
                              cp("act", Rb[:, :, :], Rf[:, :, :], ["Rf"], ["Rb"])
                          tt("pool", ocs[:, :, :], oc[oi][:, :, :], oc[oi][:, :, :], ALU.mult, [("oc", oi)], ["ocs"])
                          reduce(ss4[:, :, :], ocs[:, :, :], ALU.add, ["ocs"], ["c_ss4"])
                          rsqrt_mean("dve", ss4[:, :, :], ss4[:, :, :], 64, "c_ss4", "c_ss4")
                          tt("pool", oc[oi][:, :, :], oc[oi][:, :, :], ss4[:, :, :].to_broadcast([128, 4, 64]), ALU.mult, [("oc", oi), "c_ss4"], [("oc", oi)])
                          tt("pool", ocb[oi][:, :, :], oc[oi][:, :, :], gret[:, l:l + 1, :].to_broadcast([128, 4, 64]), ALU.mult, [("oc", oi), "gret"], [("ocb", oi)])
                          dma("sp", mixed_d[s, tsl, 768:1024], ocb[oi][:, :, :].rearrange("p h e -> p (h e)"), [("ocb", oi)], [("mx", s, t, 2)])

                      c_stage1(0)
                      for t in range(NB):
                          if t + 1 < NB:
                              c_stage1(t + 1)
                          c_stage2(t)
                P.barrier()

                with ExitStack() as ph:
                  if "D" in _SKIP:
                    pass
                  else:
                      wg = sbuf(ph, "d_wg", [128, 8, D], BF16)
                      wo = sbuf(ph, "d_wo", [128, 8, D], BF16)
                      hb = [sbuf(ph, "d_h%d" % i, [128, D]) for i in range(2)]
                      mx = [sbuf(ph, "d_mx%d" % i, [128, D], BF16) for i in range(2)]
                      sg = [sbuf(ph, "d_sg%d" % i, [128, D], BF16) for i in range(2)]
                      yb = [sbuf(ph, "d_y%d" % i, [128, D], BF16) for i in range(2)]
                      yT = [sbuf(ph, "d_yT%d" % i, [128, 8, 128], BF16) for i in range(2)]
                      hn = [sbuf(ph, "d_hn%d" % i, [128, D]) for i in range(2)]
                      fo = [sbuf(ph, "d_fo%d" % i, [128, D]) for i in range(2)]
                      fjunk = sbuf(ph, "d_fjunk", [128, D], BF16)
                      if last_layer:
                          gfin = sbuf(ph, "d_gfin", [128, D])
                          dma("sp", gfin[:, :], gfin_d.partition_broadcast(128), [], ["gfin"])
                      fst = [sbuf(ph, "d_fst%d" % i, [128, 2]) for i in range(2)]
                      for kh in range(2):
                          dma("pool", wg[:, :, kh * 512:(kh + 1) * 512], wT_d[l, :, :, T_GATE[0] + kh * 512:T_GATE[0] + (kh + 1) * 512], [], ["d_wg"])
                          dma("pool", wo[:, :, kh * 512:(kh + 1) * 512], wout_d[l, :, :, kh * 512:(kh + 1) * 512], [], ["d_wo"])
                      gb = Rot([0, 1, 2, 3])
                      tb = Rot([4, 5])
                      ob2 = Rot([6, 7])
                      hb3 = hb + [sbuf(ph, "d_h2", [128, D])]

                      def d_gate(t):
                          i = t % 2
                          tsl = slice(t * 128, (t + 1) * 128)
                          dma("sp", hb3[t % 3][:, :], h_src(l, s, t), [hkey(s, t)], [("dhb", t % 3)])
                          dma("sp", mx[i][:, :], mixed_d[s, tsl, :], [("mx", s, t, 0), ("mx", s, t, 1), ("mx", s, t, 2)], [("dmx", i)])
                          for kh in range(2):
                              b = gb.next()
                              for k in range(8):
                                  mm(ps[b][:, :], uT[:, k, tsl], wg[:, k, kh * 512:(kh + 1) * 512], k == 0, k == 7, ["uT", "d_wg"], [pk(b)])
                              act(sg[i][:, kh * 512:(kh + 1) * 512], ps[b][:, :], AF.Silu, [pk(b)], [("sg", i)])
                          tt("dve", yb[i][:, :], sg[i][:, :], mx[i][:, :], ALU.mult, [("sg", i), ("dmx", i)], [("yb", i)])

                      def d_tr(t):
                          i = t % 2
                          b = tb.next()
                          pv = ps[b][:, :].bitcast(BF16)
                          for k in range(8):
                              tr(pv[:, k * 128:(k + 1) * 128], yb[i][:, k * 128:(k + 1) * 128], [("yb", i)], [pk(b)])
                          cp("act", yT[i][:, :, :], pv[:, :].rearrange("p (k q) -> p k q", q=128), [pk(b)], [("yT", i)])

                      def d_out(t):
                          i = t % 2
                          tsl = slice(t * 128, (t + 1) * 128)
                          for kh in range(2):
                              b = ob2.next()
                              for k in range(8):
                                  mm(ps[b][:, :], yT[i][:, k, :], wo[:, k, kh * 512:(kh + 1) * 512], k == 0, k == 7, [("yT", i), "d_wo"], [pk(b)])
                              tt("dve", hn[i][:, kh * 512:(kh + 1) * 512], ps[b][:, :], hb3[t % 3][:, kh * 512:(kh + 1) * 512], ALU.add, [pk(b), ("dhb", t % 3)], [("dhn", i)])
                          if not last_layer:
                              dma("sp", hbuf_d[s, tsl, :], hn[i][:, :], [("dhn", i)], [hkey(s, t)])
                          else:
                              act(fjunk[:, :], hn[i][:, :], AF.Square, [("dhn", i)], ["fjunk", ("fst", i)], accum_out=fst[i][:, 0:1])
                              rsqrt_mean("dve", fst[i][:, 1:2], fst[i][:, 0:1], D, ("fst", i), ("fst", i))
                              stt("dve", fo[i][:, :], hn[i][:, :], fst[i][:, 1:2], gfin[:, :], ALU.mult, ALU.mult, [("dhn", i), ("fst", i), "gfin"], [("fo", i)])
                              dma("sp", out_d[s, tsl, :], fo[i][:, :], [("fo", i)], [("out", s, t)])

                      for t in range(NB + 2):
                          if t < NB:
                              d_gate(t)
                          if 1 <= t <= NB:
                              d_tr(t - 1)
                          if t >= 2:
                              d_out(t - 2)
                P.barrier()
        P.barrier()
        P.emit()
    return nc


_PROG_CACHE = {}


def kernel(x, attn_norm, w_in, diff_lambda, diff_norm, kv_norm, w_uk, w_uv, ret_norm, w_out, final_norm):
    x = np.asarray(x, dtype=np.float32)
    args = [np.asarray(a, dtype=np.float32) for a in (attn_norm, w_in, diff_lambda, diff_norm, kv_norm, w_uk, w_uv, ret_norm, w_out, final_norm)]
    w = _host_weights(*args)
    consts = _host_consts()
    B = x.shape[0]
    ns = B // NCORES
    if "nc" not in _PROG_CACHE:
        _PROG_CACHE["nc"] = build_program(DEPTH, ns)
    nc = _PROG_CACHE["nc"]
    in_maps = []
    for c in range(NCORES):
        m = {"x": np.ascontiguousarray(x[c * ns:(c + 1) * ns])}
        m.update(w)
        for k, v in consts.items():
            m["c_" + k] = v
        in_maps.append(m)
    res = run_bass_kernel_spmd(nc, in_maps, core_ids=list(range(NCORES)))
    return np.concatenate([r["out"] for r in res.results], axis=0)
```

```python
import math
import numpy as np
from contextlib import ExitStack
import concourse.bass as bass
import concourse.mybir as mybir
from concourse.bass_utils import run_bass_kernel_spmd

F32 = mybir.dt.float32
BF16 = mybir.dt.bfloat16
ALU = mybir.AluOpType
AF = mybir.ActivationFunctionType
AX = mybir.AxisListType

S = 2048
D = 1024
NB = S // 128
DEPTH = 4
NCORES = 8
EPS = 1e-6
NBIS = 14
NEGM = -30000.0
import os
_SKIP = os.environ.get('KSKIP', '')


class Prog:
    ENG = ("pe", "act", "dve", "pool", "sp")
    NDS = 12

    def __init__(self, nc, stack):
        self.nc = nc
        self.q = {e: [] for e in self.ENG}
        self.cnt = {e: 0 for e in self.ENG}
        self.sem = {e: stack.enter_context(nc.semaphore("prog_" + e)) for e in self.ENG}
        self.seen = {e: {f: 0 for f in self.ENG} for e in self.ENG}
        self.dseen = {e: {} for e in self.ENG}
        self.lastw = {}
        self.readers = {}
        self.dsem = {}
        self.drot = {}
        for qn in ("sp", "pool"):
            for i in range(self.NDS):
                nm = "%s%d" % (qn, i)
                self.dsem[nm] = [stack.enter_context(nc.semaphore("d_" + nm)), 0]
            self.drot[qn] = 0

    def _need(self, eng, dep, waits):
        if dep is None:
            return
        if dep[0] == "e":
            _, f, idx = dep
            if self.seen[eng][f] < idx:
                self.seen[eng][f] = idx
                waits[("e", f)] = (self.sem[f], idx)
        else:
            _, name, val = dep
            if self.dseen[eng].get(name, 0) < val:
                self.dseen[eng][name] = val
                waits[("d", name)] = (self.dsem[name][0], val)

    def _deps(self, eng, reads, writes):
        waits = {}
        for k in reads:
            self._need(eng, self.lastw.get(k), waits)
        for k in writes:
            lw = self.lastw.get(k)
            if lw is not None and not (lw[0] == "e" and lw[1] == eng):
                self._need(eng, lw, waits)
            for dep in self.readers.get(k, {}).values():
                if not (dep[0] == "e" and dep[1] == eng):
                    self._need(eng, dep, waits)
        return list(waits.values())

    def op(self, eng, fn, reads=(), writes=()):
        writes = list(writes) + [k for k in reads if isinstance(k, tuple) and k[0] == "ps" and k not in writes]
        waits = self._deps(eng, reads, writes)
        self.cnt[eng] += 1
        idx = self.cnt[eng]
        self.q[eng].append((waits, fn, (self.sem[eng], 1)))
        dep = ("e", eng, idx)
        for k in reads:
            self.readers.setdefault(k, {})[eng] = dep
        for k in writes:
            self.lastw[k] = dep
            self.readers[k] = {}
        return dep

    def dma(self, queue, fn, reads=(), writes=()):
        name = "%s%d" % (queue, self.drot[queue])
        self.drot[queue] = (self.drot[queue] + 1) % self.NDS
        waits = {}
        self._need(queue, ("d", name, self.dsem[name][1]), waits) if self.dsem[name][1] else None
        w2 = self._deps(queue, reads, writes)
        allw = list(waits.values()) + w2
        self.dsem[name][1] += 16
        val = self.dsem[name][1]
        self.q[queue].append((allw, fn, (self.dsem[name][0], 16)))
        dep = ("d", name, val)
        for k in writes:
            self.lastw[k] = dep
            self.readers[k] = {}
        for k in reads:
            self.readers.setdefault(k, {})["dma:" + name] = dep
        return dep

    def barrier(self):
        for e in self.ENG:
            waits = {}
            for f in self.ENG:
                if f != e and self.cnt[f]:
                    self._need(e, ("e", f, self.cnt[f]), waits)
            for name, (h, val) in self.dsem.items():
                if val:
                    self._need(e, ("d", name, val), waits)
            self.q[e].append((list(waits.values()), None, None))

    def emit(self):
        nc = self.nc
        q = self.q
        with nc.Block() as block:
            def replay(name):
                def run(e):
                    for waits, fn, inc in q[name]:
                        for s, v in waits:
                            e.wait_ge(s, v)
                        if fn is not None:
                            fn(e).then_inc(inc[0], inc[1])
                return run
            block.sync(replay("sp"))
            block.tensor(replay("pe"))
            block.scalar(replay("act"))
            block.vector(replay("dve"))
            block.gpsimd(replay("pool"))


class Rot:
    def __init__(self, items):
        self.items = list(items)
        self.i = 0

    def next(self):
        v = self.items[self.i % len(self.items)]
        self.i += 1
        return v


def _col_maps():
    o_dq, o_dk, o_dv, o_sq, o_ckv, o_iq, o_ik, o_iw, o_rq, o_rk, o_rv, o_gate = (
        0, 384, 768, 1152, 1536, 1664, 2176, 2240, 2248, 2504, 2760, 3016)
    colsF = []
    for base in (o_dq, o_dk, o_sq):
        for c in range(3):
            colsF.append(np.arange(base + 128 * c, base + 128 * c + 128))
    for c in range(4):
        colsF.append(np.arange(o_iq + 128 * c, o_iq + 128 * c + 128))
    colsF.append(np.concatenate([np.arange(o_ik, o_ik + 64)] * 2))

    def swap(base, c):
        out = []
        for hl in range(2):
            b = base + 128 * c + 64 * hl
            out += [np.arange(b + 32, b + 64), np.arange(b, b + 32)]
        return np.concatenate(out)
    for base in (o_rq, o_rk):
        for c in range(2):
            colsF.append(np.arange(base + 128 * c, base + 128 * c + 128))
        for c in range(2):
            colsF.append(swap(base, c))
    colsF = np.concatenate(colsF)
    colsT = np.concatenate([np.arange(o_dv, o_dv + 384), np.arange(o_ckv, o_ckv + 128),
                            np.arange(o_iw, o_iw + 8), np.arange(o_rv, o_rv + 256),
                            np.arange(o_gate, o_gate + 1024)])
    return colsF, colsT


NF = 22
T_DV = (0, 384)
T_CKV = (384, 520)
T_RV = (520, 776)
T_GATE = (776, 1800)
NT = 1800


def _host_consts():
    f32 = np.float32
    c = {}
    c["ident"] = np.eye(128, dtype=f32)
    r = np.arange(128)
    c["tri"] = (r[None, :] >= r[:, None]).astype(f32)
    c["cneg"] = np.where(r[None, :] <= r[:, None], 0.0, -1e30).astype(f32)
    inv_freq = (f32(10000.0) ** (-(np.arange(32, dtype=f32)) / f32(32))).astype(f32)
    ang = (np.arange(S, dtype=f32)[:, None] * inv_freq[None, :]).astype(f32)
    cos, sin = np.cos(ang).astype(f32), np.sin(ang).astype(f32)
    rr = np.arange(128)
    cosT = cos[:, rr % 32].T.copy()
    sgn = np.where((rr % 64) < 32, -1.0, 1.0).astype(f32)
    sinT = (sin[:, rr % 32].T * sgn[:, None]).astype(f32)
    c["cosT"], c["sinT"] = np.ascontiguousarray(cosT), np.ascontiguousarray(sinT)
    H = 4
    log_g = np.log(f32(1.0) - f32(2.0) ** (f32(-5.0) - np.arange(H, dtype=f32))).astype(f32)
    pos = np.arange(128, dtype=f32)
    diff = pos[:, None] - pos[None, :]
    d_intra = np.where(diff >= 0, np.exp(np.maximum(diff, 0.0)[None] * log_g[:, None, None]), 0.0).astype(f32)
    xi = np.exp((pos + 1.0)[:, None] * log_g[None, :]).astype(f32)
    zeta = np.exp((127.0 - pos)[:, None] * log_g[None, :]).astype(f32)
    gch = np.exp(128.0 * log_g).astype(f32)
    sc = f32(64.0 ** -0.5)
    c["dintraT"] = np.ascontiguousarray(np.transpose(d_intra, (2, 0, 1)) * sc).astype(f32)
    xiT = np.zeros((128, 2, 128), f32)
    zt = np.zeros((128, 2, 128), f32)
    gc = np.zeros((128, 2), f32)
    for cc in range(2):
        for hl in range(2):
            h = 2 * cc + hl
            xiT[hl * 64:(hl + 1) * 64, cc, :] = xi[None, :, h]
            zt[:, cc, hl * 64:(hl + 1) * 64] = (zeta[:, h] * sc)[:, None]
            gc[hl * 64:(hl + 1) * 64, cc] = gch[h]
    c["xiT"], c["zeta"], c["gch"] = xiT, zt, gc
    return c


_CONST_SHAPES = {"ident": [128, 128], "tri": [128, 128], "cneg": [128, 128], "cosT": [128, S], "sinT": [128, S],
                 "dintraT": [128, 4, 128], "xiT": [128, 2, 128], "zeta": [128, 2, 128], "gch": [128, 2]}


def _host_weights(attn_norm, w_in, diff_lambda, diff_norm, kv_norm, w_uk, w_uv, ret_norm, w_out, final_norm):
    L = w_in.shape[0]
    colsF, colsT = _col_maps()
    w = {}
    wf = w_in[:, :, colsF].reshape(L, 8, 128, NF, 128)
    w["wF"] = np.ascontiguousarray(np.transpose(wf, (0, 3, 2, 1, 4)))
    wt = w_in[:, :, colsT].reshape(L, 8, 128, NT)
    w["wT"] = np.ascontiguousarray(np.transpose(wt, (0, 2, 1, 3)))
    w["wout"] = np.ascontiguousarray(np.transpose(w_out.reshape(L, 8, 128, D), (0, 2, 1, 3)))
    uk = w_uk.reshape(L, 3, 2, 64, 128)
    w["wuk"] = np.ascontiguousarray(np.transpose(uk, (0, 2, 3, 1, 4)).reshape(L, 128, 3, 128))
    w["wuv"] = np.ascontiguousarray(np.transpose(w_uv, (0, 2, 1, 3)).reshape(L, 128, 384))
    w["gattn"] = np.ascontiguousarray(np.transpose(attn_norm.reshape(L, 8, 128), (0, 2, 1)))
    w["gkv"] = np.ascontiguousarray(kv_norm.reshape(L, 128, 1))
    w["gdiff"] = np.ascontiguousarray(diff_norm)
    w["gret"] = np.ascontiguousarray(ret_norm)
    w["gfin"] = np.ascontiguousarray(final_norm)
    w["dlam"] = np.ascontiguousarray(diff_lambda.reshape(L, 128))
    return w


def build_program(NL=DEPTH, NS=2, debug=False):
    nc = bass.Bass("TRN2", target_bir_lowering=False)
    L = NL
    dt = lambda name, shape, dtype=F32, kind="ExternalInput": nc.dram_tensor(name, shape, dtype, kind=kind).ap()
    x_d = dt("x", [NS, S, D])
    wF_d = dt("wF", [L, NF, 128, 8, 128])
    wT_d = dt("wT", [L, 128, 8, NT])
    wout_d = dt("wout", [L, 128, 8, D])
    wuk_d = dt("wuk", [L, 128, 3, 128])
    wuv_d = dt("wuv", [L, 128, 384])
    gattn_d = dt("gattn", [L, 128, 8])
    gkv_d = dt("gkv", [L, 128, 1])
    gdiff_d = dt("gdiff", [L, 64])
    gret_d = dt("gret", [L, 64])
    gfin_d = dt("gfin", [D])
    dlam_d = dt("dlam", [L, 128])
    cd = {k: dt("c_" + k, shp) for k, shp in _CONST_SHAPES.items()}
    out_d = dt("out", [NS, S, D], F32, "ExternalOutput")
    hbuf_d = dt("hbuf", [NS, S, D], F32, "Internal")
    mixed_d = dt("mixed", [NS, S, D], BF16, "Internal")
    dbg_d = dt("dbg", [NS, S, D], F32, "ExternalOutput") if debug else None

    with ExitStack() as st:
        P = Prog(nc, st)
        _uid = [0]

        def sbuf(stack, name, shape, dtype=F32):
            _uid[0] += 1
            return stack.enter_context(nc.sbuf_tensor("s%d_%s" % (_uid[0], name), shape, dtype))
        ps = [st.enter_context(nc.psum_tensor("ps%d" % i, [128, 512], F32)) for i in range(8)]
        pk = lambda b: ("ps", b)

        def mm(out, lhsT, rhs, start, stop, reads, writes, **kw):
            P.op("pe", lambda e: e.matmul(out, lhsT=lhsT, rhs=rhs, start=start, stop=stop,
                                          skip_group_check=True, **kw), reads, writes)

        def tr(out, in_, reads, writes):
            P.op("pe", lambda e: e.transpose(out, in_, ident_b[:, :]), list(reads) + ["ident_b"], writes)

        def act(out, in_, func, reads, writes, **kw):
            P.op("act", lambda e: e.activation(out=out, in_=in_, func=func, **kw), reads, writes)

        def ts(eng, out, in0, s1, s2, op0, op1, reads, writes, **kw):
            if op1 is None:
                P.op(eng, lambda e: e.tensor_scalar(out, in0, s1, None, op0=op0, **kw), reads, writes)
            else:
                P.op(eng, lambda e: e.tensor_scalar(out, in0, s1, s2, op0=op0, op1=op1, **kw), reads, writes)

        def tt(eng, out, in0, in1, op, reads, writes):
            P.op(eng, lambda e: e.tensor_tensor(out, in0, in1, op=op), reads, writes)

        def stt(eng, out, in0, scalar, in1, op0, op1, reads, writes):
            P.op(eng, lambda e: e.scalar_tensor_tensor(out, in0, scalar, in1, op0=op0, op1=op1), reads, writes)

        def cp(eng, out, in_, reads, writes):
            if eng == "act":
                P.op("act", lambda e: e.copy(out, in_), reads, writes)
            else:
                P.op(eng, lambda e: e.tensor_copy(out, in_), reads, writes)

        def recip(out, in_, reads, writes):
            P.op("dve", lambda e: e.reciprocal(out, in_), reads, writes)

        def reduce(out, in_, op, reads, writes):
            P.op("dve", lambda e: e.tensor_reduce(out, in_, axis=AX.X, op=op), reads, writes)

        def memset(eng, ap, val, writes):
            P.op(eng, lambda e: e.memset(ap, val), [], writes)

        def dma(queue, out, in_, reads, writes):
            return P.dma(queue, lambda e: e.dma_start(out=out, in_=in_), reads, writes)

        def rsqrt_mean(eng_unused, out, ss, n, key_out, key_ss):
            ts("dve", out, ss, 1.0 / n, EPS, ALU.mult, ALU.add, [key_ss], [key_out])
            act(out, out, AF.Sqrt, [key_out], [key_out])
            P.op("dve", lambda e: e.reciprocal(out, out), [key_out], [key_out])

        ident_b = sbuf(st, "ident_b", [128, 128], BF16)
        tri_b = sbuf(st, "tri_b", [128, 128], BF16)
        cneg = sbuf(st, "cneg", [128, 128])
        dintraT = sbuf(st, "dintraT", [128, 4, 128])
        xiT = sbuf(st, "xiT", [128, 2, 128])
        zeta = sbuf(st, "zeta", [128, 2, 128])
        gch = sbuf(st, "gch", [128, 2])
        gattn = sbuf(st, "gattn", [128, L, 8])
        gkv = sbuf(st, "gkv", [128, L, 1])
        gdm = sbuf(st, "gdm", [128, L, 64])
        gret = sbuf(st, "gret", [128, L, 64])
        lamneg = sbuf(st, "lamneg", [128, L, 1])
        lamt = sbuf(st, "lamt", [128, 4])
        uT = sbuf(st, "uT", [128, 8, S], BF16)
        tmp0 = ExitStack()
        dlam = sbuf(tmp0, "dlam", [128, L, 128])

        dma("pool", ident_b[:, :], cd["ident"][:, :], [], ["ident_b"])
        dma("pool", tri_b[:, :], cd["tri"][:, :], [], ["tri_b"])
        for nm, t in (("cneg", cneg), ("gch", gch)):
            dma("sp", t[:, :], cd[nm][:, :], [], [nm])
        for nm, t in (("dintraT", dintraT), ("xiT", xiT), ("zeta", zeta)):
            dma("sp", t[:, :, :], cd[nm][:, :, :], [], [nm])
        for l in range(L):
            dma("sp", gattn[:, l, :], gattn_d[l, :, :], [], ["gattn"])
            dma("sp", gkv[:, l, :], gkv_d[l, :, :], [], ["gkv"])
            dma("sp", gdm[:, l, :], gdiff_d[l, :].partition_broadcast(128), [], ["gdm"])
            dma("sp", gret[:, l, :], gret_d[l, :].partition_broadcast(128), [], ["gret"])
            dma("sp", dlam[:, l, :], dlam_d[l, :].partition_broadcast(128), [], ["dlam"])
        for l in range(L):
            lam_init = 0.8 - 0.6 * math.exp(-0.3 * l)
            junk = dlam[:, l, 0:32]
            tt("dve", dlam[:, l, 0:32], dlam[:, l, 0:32], dlam[:, l, 32:64], ALU.mult, ["dlam"], ["dlam"])
            tt("dve", dlam[:, l, 64:96], dlam[:, l, 64:96], dlam[:, l, 96:128], ALU.mult, ["dlam"], ["dlam"])
            reduce(lamt[:, 0:1], dlam[:, l, 0:32], ALU.add, ["dlam"], ["lamt"])
            reduce(lamt[:, 1:2], dlam[:, l, 64:96], ALU.add, ["dlam", "lamt"], ["lamt"])
            act(lamt[:, 2:4], lamt[:, 0:2], AF.Exp, ["lamt"], ["lamt"])
            tt("dve", lamt[:, 0:1], lamt[:, 3:4], lamt[:, 2:3], ALU.subtract, ["lamt"], ["lamt"])
            ts("dve", lamneg[:, l, :], lamt[:, 0:1], -lam_init, None, ALU.add, None, ["lamt"], ["lamneg"])
            ts("dve", gdm[:, l, :], gdm[:, l, :], 1.0 - lam_init, None, ALU.mult, None, ["gdm"], ["gdm"])

        P.barrier()
        tmp0.close()
        wrotF = Rot(range(3))
        wrotT = Rot(range(2))
        evrot = Rot(["act", "dve"])

        def h_src(l, s, t):
            src = x_d if l == 0 else hbuf_d
            return src[s, t * 128:(t + 1) * 128, :]

        def hkey(s, t):
            return ("h", s, t)

        for l in range(L):
            last_layer = (l == L - 1)
            for s in range(NS):
                with ExitStack() as ph:
                    hb = [sbuf(ph, "p0_h%d" % i, [128, D]) for i in range(4)]
                    hn = [sbuf(ph, "p0_hn%d" % i, [128, D], BF16) for i in range(2)]
                    sq_junk = sbuf(ph, "p0_junk", [128, D], BF16)
                    st0 = [sbuf(ph, "p0_st%d" % i, [128, 2]) for i in range(4)]
                    brot = Rot(range(8))
                    gat3 = sbuf(ph, "p0_g3", [128, 8, 1])
                    cp("dve", gat3[:, :, :].rearrange("p k o -> p (k o)"), gattn[:, l, :], ["gattn"], ["gat3"])

                    def p0_s1a(t):
                        i = t % 4
                        dma("sp", hb[i][:, :], h_src(l, s, t), [hkey(s, t)], [("hb", i)])
                        act(sq_junk[:, :], hb[i][:, :], AF.Square, [("hb", i)], ["sqj", ("st0", i)], accum_out=st0[i][:, 0:1])

                    def p0_s1b(t):
                        i = t % 4
                        ts("dve", st0[i][:, 1:2], st0[i][:, 0:1], 1.0 / D, EPS, ALU.mult, ALU.add, [("st0", i)], [("st0", i)])
                        act(st0[i][:, 1:2], st0[i][:, 1:2], AF.Sqrt, [("st0", i)], [("st0", i)])

                    def p0_s1c(t):
                        i = t % 4
                        recip(st0[i][:, 1:2], st0[i][:, 1:2], [("st0", i)], [("st0", i)])
                        act(hn[t % 2][:, :], hb[i][:, :], AF.Copy, [("hb", i), ("st0", i)], [("hn", t % 2)], scale=st0[i][:, 1:2])

                    def p0_stage2(t):
                        i = t % 2
                        b = brot.next()
                        pv = ps[b][:, :].bitcast(BF16)
                        for k in range(8):
                            tr(pv[:, k * 128:(k + 1) * 128], hn[i][:, k * 128:(k + 1) * 128], [("hn", i)], [pk(b)])
                        tt("dve", uT[:, :, t * 128:(t + 1) * 128], pv[:, :].rearrange("p (k q) -> p k q", q=128),
                           gat3[:, :, :].to_broadcast([128, 8, 128]), ALU.mult, [pk(b), "gat3"], ["uT"])
                    for t in range(NB + 3):
                        if t < NB:
                            p0_s1a(t)
                        if 1 <= t <= NB:
                            p0_s1b(t - 1)
                        if 2 <= t <= NB + 1:
                            p0_s1c(t - 2)
                        if t >= 3:
                            p0_stage2(t - 3)
                P.barrier()

                def load_wF(wtiles, f):
                    i = wrotF.next()
                    dma("pool", wtiles[i][:, :, :], wF_d[l, f, :, :, :], [], [("wF", i)])
                    return i

                def proj_F(wtiles, f, evac):
                    i = load_wF(wtiles, f)
                    for tg in range(4):
                        b = brotP.next()
                        for k in range(8):
                            mm(ps[b][:, :], wtiles[i][:, k, :], uT[:, k, tg * 512:(tg + 1) * 512], k == 0, k == 7,
                               [("wF", i), "uT"], [pk(b)])
                        evac(tg, ps[b][:, :], b)

                def proj_T(wt, wkey, ncols, t, c0=0):
                    b = brotP.next()
                    for k in range(8):
                        mm(ps[b][:, 0:ncols], uT[:, k, t * 128:(t + 1) * 128], wt[:, k, c0:c0 + ncols], k == 0, k == 7,
                           [wkey, "uT"], [pk(b)])
                    return b

                def evac_copy(dst3, c):
                    def f(tg, pa, b):
                        ev = evrot.next()
                        cp(ev, dst3[:, c, tg * 512:(tg + 1) * 512], pa, [pk(b)], [dst3.name if hasattr(dst3, "name") else "x"])
                    return f

                with ExitStack() as ph:
                  if "A" in _SKIP:
                    pass
                  else:
                      brotP = Rot(range(8))
                      wFt = [sbuf(ph, "a_wF%d" % i, [128, 8, 128], BF16) for i in range(3)]
                      wTt = sbuf(ph, "a_wT", [128, 8, 384], BF16)
                      dqT = sbuf(ph, "a_dqT", [128, 3, S], BF16)
                      dkT = sbuf(ph, "a_dkT", [128, 3, S], BF16)
                      dva = sbuf(ph, "a_dva", [128, NB, 6, 65], BF16)
                      pt = [sbuf(ph, "a_pt%d" % i, [128, 512], BF16) for i in range(8)]
                      rr = [sbuf(ph, "a_rr%d" % i, [128, 4, 1]) for i in range(3)]
                      tA = sbuf(ph, "a_tA", [128, 4, 64])
                      tB = sbuf(ph, "a_tB", [128, 4, 64])
                      tO = sbuf(ph, "a_tO", [128, 4, 64])
                      tS = sbuf(ph, "a_tS", [128, 4, 64])
                      ss4 = sbuf(ph, "a_ss4", [128, 4, 1])
                      oa = [sbuf(ph, "a_oa%d" % i, [128, 4, 384], BF16) for i in range(2)]

                      dma("pool", wTt[:, :, :], wT_d[l, :, :, T_DV[0]:T_DV[1]], [], ["a_wT"])
                      memset("pool", dva[:, :, :, 64:65], 1.0, ["dva"])
                      for c in range(3):
                          def ev_q(tg, pa, b, c=c):
                              cp(evrot.next(), dqT[:, c, tg * 512:(tg + 1) * 512], pa, [pk(b)], ["dqT"])
                          proj_F(wFt, c, ev_q)
                      for c in range(3):
                          def ev_k(tg, pa, b, c=c):
                              cp(evrot.next(), dkT[:, c, tg * 512:(tg + 1) * 512], pa, [pk(b)], ["dkT"])
                          proj_F(wFt, 3 + c, ev_k)
                      for t in range(NB):
                          b = proj_T(wTt, "a_wT", 384, t)
                          cp(evrot.next(), dva[:, t, :, 0:64], ps[b][:, 0:384].rearrange("p (h e) -> p h e", e=64), [pk(b)], ["dva"])

                      scrot = Rot([0, 1, 2, 3])
                      accrot = Rot([(4, 5), (6, 7)])
                      ptrot = Rot(range(8))
                      scale = 32.0 ** -0.5

                      def a_epilogue(I, h, banks):
                          oab = oa[I % 2]
                          oak = ("oa", I % 2)
                          a0 = ps[banks[0]][:, 0:260].rearrange("p (i e) -> p i e", e=65)
                          a1 = ps[banks[1]][:, 0:260].rearrange("p (i e) -> p i e", e=65)
                          recip(rr[0][:, :, :], a0[:, :, 64:65], [pk(banks[0])], ["rr0"])
                          recip(rr[1][:, :, :], a1[:, :, 64:65], [pk(banks[1])], ["rr1"])
                          ts("dve", rr[2][:, :, :], rr[1][:, :, :], lamneg[:, l, :], None, ALU.mult, None, ["rr1", "lamneg"], ["rr2"])
                          tt("dve", tA[:, :, :], a0[:, :, 0:64], rr[0][:, :, :].to_broadcast([128, 4, 64]), ALU.mult, [pk(banks[0]), "rr0"], ["tA"])
                          tt("dve", tB[:, :, :], a1[:, :, 0:64], rr[2][:, :, :].to_broadcast([128, 4, 64]), ALU.mult, [pk(banks[1]), "rr2"], ["tB"])
                          tt("pool", tO[:, :, :], tA[:, :, :], tB[:, :, :], ALU.add, ["tA", "tB"], ["tO"])
                          tt("pool", tS[:, :, :], tO[:, :, :], tO[:, :, :], ALU.mult, ["tO"], ["tS"])
                          reduce(ss4[:, :, :], tS[:, :, :], ALU.add, ["tS"], ["ss4"])
                          rsqrt_mean("dve", ss4[:, :, :], ss4[:, :, :], 64, "ss4", "ss4")
                          tt("pool", tO[:, :, :], tO[:, :, :], ss4[:, :, :].to_broadcast([128, 4, 64]), ALU.mult, ["tO", "ss4"], ["tO"])
                          tt("pool", oab[:, :, h * 64:(h + 1) * 64], tO[:, :, :], gdm[:, l:l + 1, :].to_broadcast([128, 4, 64]), ALU.mult, ["tO", "gdm"], [oak])
                          if h == 5:
                              for i4 in range(4):
                                  t = 4 * I + i4
                                  dma("sp", mixed_d[s, t * 128:(t + 1) * 128, 0:384], oab[:, i4, :], [oak], [("mx", s, t, 0)])

                      tiles = []
                      for I in range(4):
                          for h in range(6):
                              banks = accrot.next()
                              nj = 4 * I + 4
                              for j in range(nj):
                                  for c2 in range(2):
                                      tiles.append(dict(I=I, h=h, c2=c2, j=j, ab=banks[c2], banks=banks, first=(j == 0),
                                                        last=(c2 == 1 and j == nj - 1)))

                      def a_score(T):
                          I, h, c2, j = T["I"], T["h"], T["c2"], T["j"]
                          r0 = (h % 2) * 64 + c2 * 32
                          kw = dict(tile_position=(96, 0)) if r0 == 96 else {}
                          i0 = max(j, 4 * I)
                          w = (4 * I + 4 - i0) * 128
                          sb_ = scrot.next()
                          pi = ptrot.next()
                          T.update(i0=i0, w=w, pi=pi)
                          mm(ps[sb_][:, 0:w], dkT[r0:r0 + 32, h // 2, j * 128:(j + 1) * 128],
                             dqT[r0:r0 + 32, h // 2, i0 * 128:(4 * I + 4) * 128], True, True,
                             ["dkT", "dqT"], [pk(sb_)], **kw)
                          act(pt[pi][:, 0:w], ps[sb_][:, 0:w], AF.Exp, [pk(sb_)], [("pt", pi)], scale=scale)
                          if j >= 4 * I:
                              tt("pool", pt[pi][:, 0:128], pt[pi][:, 0:128], tri_b[:, :], ALU.mult, [("pt", pi), "tri_b"], [("pt", pi)])

                      def a_av(T):
                          I, h, j, i0, pi, ab = T["I"], T["h"], T["j"], T["i0"], T["pi"], T["ab"]
                          for i in range(i0, 4 * I + 4):
                              mm(ps[ab][:, (i - 4 * I) * 65:(i - 4 * I) * 65 + 65], pt[pi][:, (i - i0) * 128:(i - i0 + 1) * 128],
                                 dva[:, j, h, :], T["first"] and i == i0, j == i, [("pt", pi), "dva"], [pk(ab)])
                          if T["last"]:
                              a_epilogue(I, h, T["banks"])

                      DEP = 4
                      for idx in range(len(tiles) + DEP):
                          if idx < len(tiles):
                              a_score(tiles[idx])
                          if idx >= DEP:
                              a_av(tiles[idx - DEP])
                P.barrier()

                with ExitStack() as ph:
                  if "B" in _SKIP:
                    pass
                  else:
                      brotP = Rot(range(8))
                      qlT = sbuf(ph, "b_qlT", [128, 6, S], BF16)
                      ckvT = sbuf(ph, "b_ckvT", [128, S], BF16)
                      vpa = sbuf(ph, "b_vpa", [128, NB, 6, 65], BF16)
                      iqT = sbuf(ph, "b_iqT", [128, 4, S], BF16)
                      ikT = sbuf(ph, "b_ikT", [128, S], BF16)
                      iw3 = sbuf(ph, "b_iw", [128, NB, 8, 1])
                      iw = iw3[:, :, :, :].rearrange("p t h o -> p t (h o)")
                      ident3 = sbuf(ph, "b_ident3", [128, 1, 128], BF16)
                      cp("pool", ident3[:, 0, :], ident_b[:, :], ["ident_b"], ["ident3"])
                      ph2 = ExitStack()
                      wFt = [sbuf(ph2, "b_wF%d" % i, [128, 8, 128], BF16) for i in range(3)]
                      wTt = sbuf(ph2, "b_wT", [128, 8, 256], BF16)
                      sqt = [sbuf(ph2, "b_sqt%d" % i, [128, 512], BF16) for i in range(2)]
                      ckn = [sbuf(ph2, "b_ckn%d" % i, [128, 128], BF16) for i in range(3)]
                      cst = [sbuf(ph2, "b_cst%d" % i, [128, 2]) for i in range(3)]
                      cjunk = sbuf(ph2, "b_cjunk", [128, 128], BF16)
                      wuk_b = sbuf(ph2, "b_wuk", [128, 1, 3, 128], BF16)
                      wuv_b = sbuf(ph2, "b_wuv", [128, 1, 384], BF16)

                      dma("pool", wTt[:, :, :], wT_d[l, :, :, T_CKV[0]:T_CKV[0] + 256], [], ["b_wT"])
                      dma("pool", wuk_b[:, 0, :, :], wuk_d[l, :, :, :], [], ["wuk_b"])
                      dma("pool", wuv_b[:, 0, :], wuv_d[l, :, :], [], ["wuv_b"])
                      memset("pool", vpa[:, :, :, 64:65], 1.0, ["vpa"])
                      sqrot = Rot(range(2))
                      for c in range(3):
                          def ev_sq(tg, pa, b, c=c):
                              si = sqrot.next()
                              cp(evrot.next(), sqt[si][:, :], pa, [pk(b)], [("sqt", si)])
                              for hl in range(2):
                                  b2 = brotP.next()
                                  mm(ps[b2][:, :], wuk_b[hl * 64:(hl + 1) * 64, 0, c, :], sqt[si][hl * 64:(hl + 1) * 64, :], True, True,
                                     ["wuk_b", ("sqt", si)], [pk(b2)])
                                  cp(evrot.next(), qlT[:, 2 * c + hl, tg * 512:(tg + 1) * 512], ps[b2][:, :], [pk(b2)], ["qlT"])
                          if '1' not in _SKIP:
                              proj_F(wFt, 6 + c, ev_sq)
                      for c in range(4):
                          def ev_iq(tg, pa, b, c=c):
                              cp(evrot.next(), iqT[:, c, tg * 512:(tg + 1) * 512], pa, [pk(b)], ["iqT"])
                          if '2' not in _SKIP:
                              proj_F(wFt, 9 + c, ev_iq)

                      def ev_ik(tg, pa, b):
                          cp(evrot.next(), ikT[:, tg * 512:(tg + 1) * 512], pa, [pk(b)], ["ikT"])
                      if '3' not in _SKIP:
                          proj_F(wFt, 13, ev_ik)
                      def ckv_s1(t):
                          i = t % 3
                          b = proj_T(wTt, "b_wT", 136, t)
                          cp("dve", iw[:, t, :], ps[b][:, 128:136], [pk(b)], ["iw"])
                          act(cjunk[:, :], ps[b][:, 0:128], AF.Square, [pk(b)], ["cjunk", ("cst", i)], accum_out=cst[i][:, 0:1])
                          rsqrt_mean("dve", cst[i][:, 1:2], cst[i][:, 0:1], 128, ("cst", i), ("cst", i))
                          ts("dve", ckn[i][:, :], ps[b][:, 0:128], cst[i][:, 1:2], None, ALU.mult, None, [pk(b), ("cst", i)], [("ckn", i)])

                      def ckv_s2(t):
                          i = t % 3
                          b2 = brotP.next()
                          pv = ps[b2][:, :].bitcast(BF16)
                          tr(pv[:, 0:128], ckn[i][:, :], [("ckn", i)], [pk(b2)])
                          act(ckvT[:, t * 128:(t + 1) * 128], pv[:, 0:128], AF.Copy, [pk(b2), "gkv"], [("ckvT", t), "ckvT"], scale=gkv[:, l, :])

                      def ckv_s3(t):
                          b3 = brotP.next()
                          mm(ps[b3][:, 0:384], ckvT[:, t * 128:(t + 1) * 128], wuv_b[:, 0, :], True, True, [("ckvT", t), "wuv_b"], [pk(b3)])
                          cp(evrot.next(), vpa[:, t, :, 0:64], ps[b3][:, 0:384].rearrange("p (h e) -> p h e", e=64), [pk(b3)], ["vpa"])

                      for t in range(NB + 2):
                          if t < NB:
                              ckv_s1(t)
                          if 1 <= t <= NB:
                              ckv_s2(t - 1)
                          if t >= 2:
                              ckv_s3(t - 2)

                      P.barrier()
                      ph2.close()
                      SCW = [(8 + i + 1) * 128 for i in range(4)] + [(12 + i + 1) * 128 for i in range(4)]
                      score = [sbuf(ph, "b_score%d" % i, [128, SCW[i]]) for i in range(8)]
                      negm = [sbuf(ph, "b_negm%d" % i, [128, SCW[i]], BF16) for i in range(8)]
                      rel = [sbuf(ph, "b_rel%d" % i, [128, 512], BF16) for i in range(6)]
                      dg = [sbuf(ph, "b_dg%d" % i, [128, 8, 128], BF16) for i in range(4)]
                      bis = [sbuf(ph, "b_bis%d" % i, [128, 8]) for i in range(8)]
                      bjunk = sbuf(ph, "b_bjunk", [128, S], BF16)
                      pt = [sbuf(ph, "b_pt%d" % i, [128, 512], BF16) for i in range(4)]
                      acs = [sbuf(ph, "b_acs%d" % i, [128, 4, 65]) for i in range(2)]
                      acsrot = Rot(range(2))
                      rrp = [sbuf(ph, "b_rrp%d" % i, [128, 4, 1]) for i in range(2)]
                      negone = sbuf(ph, "b_negone", [128, 4, 1])
                      memset("pool", negone[:, :, :], -1.0, ["negone"])
                      ob = [sbuf(ph, "b_ob%d" % i, [128, 4, 384], BF16) for i in range(2)]
                      relbank = Rot([0, 1, 2, 3])
                      idxacc = Rot([4])
                      relrot = Rot(range(6))
                      scrot = Rot([5, 6])
                      accrot = Rot([7])
                      ptrot = Rot(range(4))
                      scale = 64.0 ** -0.5

                      QORD = [1, 3, 2, 0]
                      QPAR = {1: 0, 3: 1, 2: 0, 0: 1}

                      def index_phase(I):
                          par = QPAR[I]
                          chains = []
                          for i4 in range(4):
                              i = 4 * I + i4
                              if i >= 2:
                                  tt("pool", dg[i4][:, :, :], ident3[:, :, :].to_broadcast([128, 8, 128]), iw3[:, i, :, :].to_broadcast([128, 8, 128]),
                                     ALU.mult, ["ident3", "iw"], [("dg", i4)])
                          for i4 in range(4):
                              i = 4 * I + i4
                              n = (i + 1) * 128
                              si_ = par * 4 + i4
                              sk = ("score", si_)
                              bk = ("bis", si_)
                              bs = bis[si_]
                              if i < 2 or 'i' in _SKIP:
                                  memset("dve", score[si_][:, 0:n], 0.0, [sk])
                                  tt("dve", score[si_][:, n - 128:n], score[si_][:, n - 128:n], cneg[:, :], ALU.add, [sk, "cneg"], [sk])
                                  memset("dve", bs[:, 0:1], -1e29, [bk])
                              else:
                                  dgi = i4
                                  for kc in range((n + 511) // 512):
                                      w = min(512, n - kc * 512)
                                      ab = idxacc.next()
                                      pend = []
                                      for hp in range(5):
                                          cur = []
                                          if hp < 4:
                                              for hl in range(2):
                                                  hh = 2 * hp + hl
                                                  rb = relbank.next()
                                                  r0 = hl * 64
                                                  mm(ps[rb][:, 0:w], iqT[r0:r0 + 64, hp, i * 128:(i + 1) * 128], ikT[r0:r0 + 64, kc * 512:kc * 512 + w],
                                                     True, True, ["iqT", "ikT"], [pk(rb)])
                                                  ri = relrot.next()
                                                  act(rel[ri][:, 0:w], ps[rb][:, 0:w], AF.Relu, [pk(rb)], [("rel", ri)])
                                                  cur.append((hh, ri))
                                          for (ph_, pri) in pend:
                                              mm(ps[ab][:, 0:w], dg[dgi][:, ph_, :], rel[pri][:, 0:w], ph_ == 0, ph_ == 7, [("dg", dgi), ("rel", pri)], [pk(ab)])
                                          pend = cur
                                      cp("act", score[si_][:, kc * 512:kc * 512 + w], ps[ab][:, 0:w], [pk(ab)], [sk])
                                  tt("pool", score[si_][:, n - 128:n], score[si_][:, n - 128:n], cneg[:, :], ALU.add, [sk, "cneg"], [sk])
                                  reduce(bs[:, 5:6], score[si_][:, 0:n], ALU.max, [sk], [bk])
                                  reduce(bs[:, 0:1], score[si_][:, 0:256], ALU.min, [sk, bk], [bk])
                                  tt("dve", bs[:, 1:2], bs[:, 5:6], bs[:, 0:1], ALU.subtract, [bk], [bk])
                                  chains.append((si_, n, sk, bk, bs))
                          for it in range(NBIS):
                              for (si_, n, sk, bk, bs) in chains:
                                  ts("dve", bs[:, 2:3], bs[:, 1:2], 0.5 ** (it + 1), bs[:, 0:1], ALU.mult, ALU.add, [bk], [bk])
                              for (si_, n, sk, bk, bs) in chains:
                                  ts("dve", bjunk[:, 0:n], score[si_][:, 0:n], bs[:, 2:3], 0.0, ALU.is_ge, ALU.add, [sk, bk], ["bjunk", bk], accum_out=bs[:, 3:4])
                              for (si_, n, sk, bk, bs) in chains:
                                  ts("dve", bs[:, 4:5], bs[:, 3:4], 255.5, 1e30, ALU.is_lt, ALU.mult, [bk], [bk])
                              for (si_, n, sk, bk, bs) in chains:
                                  stt("dve", bs[:, 0:1], bs[:, 2:3], bs[:, 4:5], bs[:, 0:1], ALU.subtract, ALU.max, [bk], [bk])
                          for i4 in range(4):
                              n = (4 * I + i4 + 1) * 128
                              si_ = par * 4 + i4
                              ts("dve", negm[si_][:, 0:n], score[si_][:, 0:n], bis[si_][:, 0:1], NEGM, ALU.is_lt, ALU.mult,
                                 [("score", si_), ("bis", si_)], [("negm", si_)])

                      def d_epilogue(I, h, ab):
                          obb = ob[I % 2]
                          obk = ("ob", I % 2)
                          a0 = ps[ab][:, 0:260].rearrange("p (i e) -> p i e", e=65)
                          ai = acsrot.next()
                          cp("act", acs[ai][:, :, :], a0, [pk(ab)], [("acs", ai)])
                          tt("pool", rrp[ai][:, :, :], acs[ai][:, :, 64:65], negone[:, :, :], ALU.pow, [("acs", ai), "negone"], [("rrp", ai)])
                          tt("pool", obb[:, :, h * 64:(h + 1) * 64], acs[ai][:, :, 0:64], rrp[ai][:, :, :].to_broadcast([128, 4, 64]), ALU.mult,
                             [("acs", ai), ("rrp", ai)], [obk])
                          if h == 5:
                              for i4 in range(4):
                                  t = 4 * I + i4
                                  dma("sp", mixed_d[s, t * 128:(t + 1) * 128, 384:768], obb[:, i4, :], [obk], [("mx", s, t, 1)])

                      def d_score(T):
                          I, h, j = T["I"], T["h"], T["j"]
                          par = QPAR[I]
                          i0 = max(j, 4 * I)
                          w = (4 * I + 4 - i0) * 128
                          sb_ = scrot.next()
                          pi = ptrot.next()
                          T.update(i0=i0, w=w, pi=pi)
                          mm(ps[sb_][:, 0:w], ckvT[:, j * 128:(j + 1) * 128], qlT[:, h, i0 * 128:(4 * I + 4) * 128], True, False,
                             ["ckvT", "qlT"], [pk(sb_)])
                          for i in range(i0, 4 * I + 4):
                              i4 = i - 4 * I
                              mm(ps[sb_][:, (i - i0) * 128:(i - i0 + 1) * 128], negm[par * 4 + i4][:, j * 128:(j + 1) * 128], ident_b[:, :], False,
                                 i == 4 * I + 3, [("negm", par * 4 + i4), "ident_b"], [pk(sb_)])
                          act(pt[pi][:, 0:w], ps[sb_][:, 0:w], AF.Exp, [pk(sb_)], [("pt", pi)], scale=scale)

                      def d_av(T):
                          I, h, j, i0, pi, ab = T["I"], T["h"], T["j"], T["i0"], T["pi"], T["ab"]
                          for i in range(i0, 4 * I + 4):
                              mm(ps[ab][:, (i - 4 * I) * 65:(i - 4 * I) * 65 + 65], pt[pi][:, (i - i0) * 128:(i - i0 + 1) * 128],
                                 vpa[:, j, h, :], T["first"] and i == i0, j == i, [("pt", pi), "vpa"], [pk(ab)])
                          if T["last"]:
                              d_epilogue(I, h, ab)

                      def dsa_phase(I):
                          tiles = []
                          for h in range(6):
                              ab = accrot.next()
                              nj = 4 * I + 4
                              for j in range(nj):
                                  tiles.append(dict(I=I, h=h, j=j, ab=ab, first=(j == 0), last=(j == nj - 1)))
                          DEP = 1
                          for idx in range(len(tiles) + DEP):
                              if idx < len(tiles):
                                  d_score(tiles[idx])
                              if idx >= DEP:
                                  d_av(tiles[idx - DEP])

                      for pos, I in enumerate(QORD):
                          if pos == 0:
                              index_phase(I)
                          if pos + 1 < 4:
                              index_phase(QORD[pos + 1])
                          dsa_phase(I)
                P.barrier()

                with ExitStack() as ph:
                  if "C" in _SKIP:
                    pass
                  else:
                      brotP = Rot(range(8))
                      wFt = [sbuf(ph, "c_wF%d" % i, [128, 8, 128], BF16) for i in range(3)]
                      wTt = sbuf(ph, "c_wT", [128, 8, 256], BF16)
                      rT = [sbuf(ph, "c_rqT", [128, 2, S], BF16), sbuf(ph, "c_rkT", [128, 2, S], BF16)]
                      rv = sbuf(ph, "c_rv", [128, NB, 256], BF16)
                      t1 = sbuf(ph, "c_t1", [128, 2, S])
                      t2 = [sbuf(ph, "c_t2_%d" % i, [128, 512]) for i in range(2)]
                      kz = [sbuf(ph, "c_kz%d" % i, [128, 128], BF16) for i in range(4)]
                      qxi = [sbuf(ph, "c_qxi%d" % i, [128, 128], BF16) for i in range(4)]
                      attD = [sbuf(ph, "c_attD%d" % i, [128, 128], BF16) for i in range(4)]
                      Rf = sbuf(ph, "c_Rf", [128, 2, 64])
                      Rb = sbuf(ph, "c_Rb", [128, 2, 64], BF16)
                      oc = [sbuf(ph, "c_oc%d" % i, [128, 4, 64]) for i in range(3)]
                      ocs = sbuf(ph, "c_ocs", [128, 4, 64])
                      ocb = [sbuf(ph, "c_ocb%d" % i, [128, 4, 64], BF16) for i in range(3)]
                      ss4 = sbuf(ph, "c_ss4", [128, 4, 1])

                      dma("pool", wTt[:, :, :], wT_d[l, :, :, T_RV[0]:T_RV[1]], [], ["c_wT"])
                      cosT = sbuf(ph, "c_cosT", [128, S])
                      sinT = sbuf(ph, "c_sinT", [128, S])
                      dma("sp", cosT[:, :], cd["cosT"][:, :], [], ["cosT"])
                      dma("sp", sinT[:, :], cd["sinT"][:, :], [], ["sinT"])
                      for qk in range(2):
                          for c in range(2):
                              def ev_x(tg, pa, b, c=c, qk=qk):
                                  i = tg % 2
                                  tt("dve", t1[:, c, tg * 512:(tg + 1) * 512], pa, cosT[:, tg * 512:(tg + 1) * 512], ALU.mult, [pk(b), "cosT"], [("t1", tg, c)])
                              proj_F(wFt, 14 + 4 * qk + c, ev_x)
                          for c in range(2):
                              def ev_xs(tg, pa, b, c=c, qk=qk):
                                  i = tg % 2
                                  tt("dve", t2[i][:, :], pa, sinT[:, tg * 512:(tg + 1) * 512], ALU.mult, [pk(b), "sinT"], [("t2", i)])
                                  tt("pool", rT[qk][:, c, tg * 512:(tg + 1) * 512], t1[:, c, tg * 512:(tg + 1) * 512], t2[i][:, :], ALU.add, [("t1", tg, c), ("t2", i)], [("rT", qk)])
                              proj_F(wFt, 16 + 4 * qk + c, ev_xs)
                      for t in range(NB):
                          b = proj_T(wTt, "c_wT", 256, t)
                          cp(evrot.next(), rv[:, t, :], ps[b][:, 0:256], [pk(b)], ["rv"])

                      memset("dve", Rf[:, :, :], 0.0, ["Rf"])
                      memset("dve", Rb[:, :, :], 0.0, ["Rb"])
                      brot = Rot([0, 1])
                      adrot = Rot(range(4))
                      obanks = [(4, 5), (2, 3)]
                      rbk = [6, 7]

                      def c_stage1(t):
                          tsl = slice(t * 128, (t + 1) * 128)
                          p2 = t % 2
                          obp = obanks[p2]
                          firsts = [True, True]
                          for c in range(2):
                              b = brot.next()
                              pv = ps[b][:, :].bitcast(BF16)
                              tr(pv[:, 0:128], rT[1][:, c, tsl], [("rT", 1)], [pk(b)])
                              tt("dve", kz[p2 * 2 + c][:, :], pv[:, 0:128], zeta[:, c, :], ALU.mult, [pk(b), "zeta"], [("kz", p2, c)])
                              tt("pool", qxi[p2 * 2 + c][:, :], rT[0][:, c, tsl], xiT[:, c, :], ALU.mult, [("rT", 0), "xiT"], [("qxi", p2, c)])
                              for hl in range(2):
                                  h = 2 * c + hl
                                  rows = slice(hl * 64, (hl + 1) * 64)
                                  b = brot.next()
                                  mm(ps[b][:, 0:128], rT[1][rows, c, tsl], rT[0][rows, c, tsl], True, True, [("rT", 1), ("rT", 0)], [pk(b)])
                                  ai = adrot.next()
                                  tt("dve", attD[ai][:, :], ps[b][:, 0:128], dintraT[:, h, :], ALU.mult, [pk(b), "dintraT"], [("attD", ai)])
                                  mm(ps[obp[hl]][:, c * 64:(c + 1) * 64], attD[ai][:, :], rv[:, t, h * 64:(h + 1) * 64], firsts[hl], t == 0,
                                     [("attD", ai), "rv"], [pk(obp[hl])])
                                  firsts[hl] = False
                              if t < NB - 1:
                                  rb_ = rbk[p2]
                                  mm(ps[rb_][:, c * 128:(c + 1) * 128], kz[p2 * 2 + c][:, :], rv[:, t, c * 128:(c + 1) * 128], True, True, [("kz", p2, c), "rv"], [pk(rb_)])

                      def c_stage2(t):
                          tsl = slice(t * 128, (t + 1) * 128)
                          p2 = t % 2
                          obp = obanks[p2]
                          if t > 0:
                              for hl in range(2):
                                  for c in range(2):
                                      rows = slice(hl * 64, (hl + 1) * 64)
                                      mm(ps[obp[hl]][:, c * 64:(c + 1) * 64], qxi[p2 * 2 + c][rows, :], Rb[rows, c, :], False, True,
                                         [("qxi", p2, c), "Rb"], [pk(obp[hl])])
                          oi = t % 3
                          ocv = oc[oi][:, :, :].rearrange("p (c hl) e -> p c hl e", hl=2)
                          for hl in range(2):
                              cp("act", ocv[:, :, hl, :], ps[obp[hl]][:, 0:128].rearrange("p (c e) -> p c e", e=64), [pk(obp[hl])], [("oc", oi)])
                          if t < NB - 1:
                              for c in range(2):
                                  for hl in range(2):
                                      rows = slice(hl * 64, (hl + 1) * 64)
                                      stt("dve", Rf[rows, c, :], Rf[rows, c, :], gch[rows, c:c + 1], ps[rbk[p2]][rows, c * 128 + hl * 64:c * 128 + (hl + 1) * 64],
                                          ALU.mult, ALU.add, ["Rf", "gch", pk(rbk[p2])], ["Rf"])
                              cp("act", Rb[:, :, :], Rf[:, :, :], ["Rf"], ["Rb"])

                      def c_stage3(t):
                          tsl = slice(t * 128, (t + 1) * 128)
                          oi = t % 3
                          tt("pool", ocs[:, :, :], oc[oi][:, :, :], oc[oi][:, :, :], ALU.mult, [("oc", oi)], ["ocs"])
                          reduce(ss4[:, :, :], ocs[:, :, :], ALU.add, ["ocs"], ["c_ss4"])
                          rsqrt_mean("dve", ss4[:, :, :], ss4[:, :, :], 64, "c_ss4", "c_ss4")
                          tt("pool", oc[oi][:, :, :], oc[oi][:, :, :], ss4[:, :, :].to_broadcast([128, 4, 64]), ALU.mult, [("oc", oi), "c_ss4"], [("oc", oi)])
                          tt("pool", ocb[oi][:, :, :], oc[oi][:, :, :], gret[:, l:l + 1, :].to_broadcast([128, 4, 64]), ALU.mult, [("oc", oi), "gret"], [("ocb", oi)])
                          dma("sp", mixed_d[s, tsl, 768:1024], ocb[oi][:, :, :].rearrange("p h e -> p (h e)"), [("ocb", oi)], [("mx", s, t, 2)])

                      c_stage1(0)
                      for t in range(NB + 1):
                          if t + 1 < NB:
                              c_stage1(t + 1)
                          if t < NB:
                              c_stage2(t)
                          if t >= 1:
                              c_stage3(t - 1)
                P.barrier()

                with ExitStack() as ph:
                  if "D" in _SKIP:
                    pass
                  else:
                      wg = sbuf(ph, "d_wg", [128, 8, D], BF16)
                      wo = sbuf(ph, "d_wo", [128, 8, D], BF16)
                      hb = [sbuf(ph, "d_h%d" % i, [128, D]) for i in range(2)]
                      mx = [sbuf(ph, "d_mx%d" % i, [128, D], BF16) for i in range(2)]
                      sg = [sbuf(ph, "d_sg%d" % i, [128, D], BF16) for i in range(2)]
                      yb = [sbuf(ph, "d_y%d" % i, [128, D], BF16) for i in range(2)]
                      yT = [sbuf(ph, "d_yT%d" % i, [128, 8, 128], BF16) for i in range(2)]
                      hn = [sbuf(ph, "d_hn%d" % i, [128, D]) for i in range(2)]
                      fo = [sbuf(ph, "d_fo%d" % i, [128, D]) for i in range(2)]
                      fjunk = sbuf(ph, "d_fjunk", [128, D], BF16)
                      if last_layer:
                          gfin = sbuf(ph, "d_gfin", [128, D])
                          dma("sp", gfin[:, :], gfin_d.partition_broadcast(128), [], ["gfin"])
                      fst = [sbuf(ph, "d_fst%d" % i, [128, 2]) for i in range(2)]
                      for kh in range(2):
                          dma("pool", wg[:, :, kh * 512:(kh + 1) * 512], wT_d[l, :, :, T_GATE[0] + kh * 512:T_GATE[0] + (kh + 1) * 512], [], ["d_wg"])
                          dma("pool", wo[:, :, kh * 512:(kh + 1) * 512], wout_d[l, :, :, kh * 512:(kh + 1) * 512], [], ["d_wo"])
                      gb = Rot([0, 1, 2, 3])
                      tb = Rot([4, 5])
                      ob2 = Rot([6, 7])
                      hb3 = hb + [sbuf(ph, "d_h2", [128, D])]

                      def d_gate(t):
                          i = t % 2
                          tsl = slice(t * 128, (t + 1) * 128)
                          dma("sp", hb3[t % 3][:, :], h_src(l, s, t), [hkey(s, t)], [("dhb", t % 3)])
                          dma("sp", mx[i][:, :], mixed_d[s, tsl, :], [("mx", s, t, 0), ("mx", s, t, 1), ("mx", s, t, 2)], [("dmx", i)])
                          for kh in range(2):
                              b = gb.next()
                              for k in range(8):
                                  mm(ps[b][:, :], uT[:, k, tsl], wg[:, k, kh * 512:(kh + 1) * 512], k == 0, k == 7, ["uT", "d_wg"], [pk(b)])
                              act(sg[i][:, kh * 512:(kh + 1) * 512], ps[b][:, :], AF.Silu, [pk(b)], [("sg", i)])
                          tt("dve", yb[i][:, :], sg[i][:, :], mx[i][:, :], ALU.mult, [("sg", i), ("dmx", i)], [("yb", i)])

                      def d_tr(t):
                          i = t % 2
                          b = tb.next()
                          pv = ps[b][:, :].bitcast(BF16)
                          for k in range(8):
                              tr(pv[:, k * 128:(k + 1) * 128], yb[i][:, k * 128:(k + 1) * 128], [("yb", i)], [pk(b)])
                          cp("act", yT[i][:, :, :], pv[:, :].rearrange("p (k q) -> p k q", q=128), [pk(b)], [("yT", i)])

                      def d_out(t):
                          i = t % 2
                          tsl = slice(t * 128, (t + 1) * 128)
                          for kh in range(2):
                              b = ob2.next()
                              for k in range(8):
                                  mm(ps[b][:, :], yT[i][:, k, :], wo[:, k, kh * 512:(kh + 1) * 512], k == 0, k == 7, [("yT", i), "d_wo"], [pk(b)])
                              tt("dve", hn[i][:, kh * 512:(kh + 1) * 512], ps[b][:, :], hb3[t % 3][:, kh * 512:(kh + 1) * 512], ALU.add, [pk(b), ("dhb", t % 3)], [("dhn", i)])
                          if not last_layer:
                              dma("sp", hbuf_d[s, tsl, :], hn[i][:, :], [("dhn", i)], [hkey(s, t)])
                          else:
                              act(fjunk[:, :], hn[i][:, :], AF.Square, [("dhn", i)], ["fjunk", ("fst", i)], accum_out=fst[i][:, 0:1])
                              rsqrt_mean("dve", fst[i][:, 1:2], fst[i][:, 0:1], D, ("fst", i), ("fst", i))
                              stt("dve", fo[i][:, :], hn[i][:, :], fst[i][:, 1:2], gfin[:, :], ALU.mult, ALU.mult, [("dhn", i), ("fst", i), "gfin"], [("fo", i)])
                              dma("sp", out_d[s, tsl, :], fo[i][:, :], [("fo", i)], [("out", s, t)])

                      for t in range(NB + 2):
                          if t < NB:
                              d_gate(t)
                          if 1 <= t <= NB:
                              d_tr(t - 1)
                          if t >= 2:
                              d_out(t - 2)
                P.barrier()
        P.barrier()
        P.emit()
    return nc


_PROG_CACHE = {}


def kernel(x, attn_norm, w_in, diff_lambda, diff_norm, kv_norm, w_uk, w_uv, ret_norm, w_out, final_norm):
    x = np.asarray(x, dtype=np.float32)
    args = [np.asarray(a, dtype=np.float32) for a in (attn_norm, w_in, diff_lambda, diff_norm, kv_norm, w_uk, w_uv, ret_norm, w_out, final_norm)]
    w = _host_weights(*args)
    consts = _host_consts()
    B = x.shape[0]
    ns = B // NCORES
    if "nc" not in _PROG_CACHE:
        _PROG_CACHE["nc"] = build_program(DEPTH, ns)
    nc = _PROG_CACHE["nc"]
    in_maps = []
    for c in range(NCORES):
        m = {"x": np.ascontiguousarray(x[c * ns:(c + 1) * ns])}
        m.update(w)
        for k, v in consts.items():
            m["c_" + k] = v
        in_maps.append(m)
    res = run_bass_kernel_spmd(nc, in_maps, core_ids=list(range(NCORES)))
    return np.concatenate([r["out"] for r in res.results], axis=0)
```

```python
import math
import numpy as np
from contextlib import ExitStack
import concourse.bass as bass
import concourse.mybir as mybir
from concourse.bass_utils import run_bass_kernel_spmd

F32 = mybir.dt.float32
BF16 = mybir.dt.bfloat16
ALU = mybir.AluOpType
AF = mybir.ActivationFunctionType
AX = mybir.AxisListType

S = 2048
D = 1024
NB = S // 128
DEPTH = 4
NCORES = 8
EPS = 1e-6
NBIS = 14
NEGM = -30000.0
_SKIP = ''


class Prog:
    ENG = ("pe", "act", "dve", "pool", "sp")
    NDS = 12

    def __init__(self, nc, stack):
        self.nc = nc
        self.q = {e: [] for e in self.ENG}
        self.cnt = {e: 0 for e in self.ENG}
        self.sem = {e: stack.enter_context(nc.semaphore("prog_" + e)) for e in self.ENG}
        self.seen = {e: {f: 0 for f in self.ENG} for e in self.ENG}
        self.dseen = {e: {} for e in self.ENG}
        self.lastw = {}
        self.readers = {}
        self.dsem = {}
        self.drot = {}
        for qn in ("sp", "pool"):
            for i in range(self.NDS):
                nm = "%s%d" % (qn, i)
                self.dsem[nm] = [stack.enter_context(nc.semaphore("d_" + nm)), 0]
            self.drot[qn] = 0

    def _need(self, eng, dep, waits):
        if dep is None:
            return
        if dep[0] == "e":
            _, f, idx = dep
            if self.seen[eng][f] < idx:
                self.seen[eng][f] = idx
                waits[("e", f)] = (self.sem[f], idx)
        else:
            _, name, val = dep
            if self.dseen[eng].get(name, 0) < val:
                self.dseen[eng][name] = val
                waits[("d", name)] = (self.dsem[name][0], val)

    def _deps(self, eng, reads, writes):
        waits = {}
        for k in reads:
            self._need(eng, self.lastw.get(k), waits)
        for k in writes:
            lw = self.lastw.get(k)
            if lw is not None and not (lw[0] == "e" and lw[1] == eng):
                self._need(eng, lw, waits)
            for dep in self.readers.get(k, {}).values():
                if not (dep[0] == "e" and dep[1] == eng):
                    self._need(eng, dep, waits)
        return list(waits.values())

    def op(self, eng, fn, reads=(), writes=()):
        writes = list(writes) + [k for k in reads if isinstance(k, tuple) and k[0] == "ps" and k not in writes]
        waits = self._deps(eng, reads, writes)
        self.cnt[eng] += 1
        idx = self.cnt[eng]
        self.q[eng].append((waits, fn, (self.sem[eng], 1)))
        dep = ("e", eng, idx)
        for k in reads:
            self.readers.setdefault(k, {})[eng] = dep
        for k in writes:
            self.lastw[k] = dep
            self.readers[k] = {}
        return dep

    def dma(self, queue, fn, reads=(), writes=()):
        name = "%s%d" % (queue, self.drot[queue])
        self.drot[queue] = (self.drot[queue] + 1) % self.NDS
        waits = {}
        self._need(queue, ("d", name, self.dsem[name][1]), waits) if self.dsem[name][1] else None
        w2 = self._deps(queue, reads, writes)
        allw = list(waits.values()) + w2
        self.dsem[name][1] += 16
        val = self.dsem[name][1]
        self.q[queue].append((allw, fn, (self.dsem[name][0], 16)))
        dep = ("d", name, val)
        for k in writes:
            self.lastw[k] = dep
            self.readers[k] = {}
        for k in reads:
            self.readers.setdefault(k, {})["dma:" + name] = dep
        return dep

    def barrier(self):
        for e in self.ENG:
            waits = {}
            for f in self.ENG:
                if f != e and self.cnt[f]:
                    self._need(e, ("e", f, self.cnt[f]), waits)
            for name, (h, val) in self.dsem.items():
                if val:
                    self._need(e, ("d", name, val), waits)
            self.q[e].append((list(waits.values()), None, None))

    def emit(self):
        nc = self.nc
        q = self.q
        with nc.Block() as block:
            def replay(name):
                def run(e):
                    for waits, fn, inc in q[name]:
                        for s, v in waits:
                            e.wait_ge(s, v)
                        if fn is not None:
                            fn(e).then_inc(inc[0], inc[1])
                return run
            block.sync(replay("sp"))
            block.tensor(replay("pe"))
            block.scalar(replay("act"))
            block.vector(replay("dve"))
            block.gpsimd(replay("pool"))


class Rot:
    def __init__(self, items):
        self.items = list(items)
        self.i = 0

    def next(self):
        v = self.items[self.i % len(self.items)]
        self.i += 1
        return v


def _col_maps():
    o_dq, o_dk, o_dv, o_sq, o_ckv, o_iq, o_ik, o_iw, o_rq, o_rk, o_rv, o_gate = (
        0, 384, 768, 1152, 1536, 1664, 2176, 2240, 2248, 2504, 2760, 3016)
    colsF = []
    for base in (o_dq, o_dk, o_sq):
        for c in range(3):
            colsF.append(np.arange(base + 128 * c, base + 128 * c + 128))
    for c in range(4):
        colsF.append(np.arange(o_iq + 128 * c, o_iq + 128 * c + 128))
    colsF.append(np.concatenate([np.arange(o_ik, o_ik + 64)] * 2))

    def swap(base, c):
        out = []
        for hl in range(2):
            b = base + 128 * c + 64 * hl
            out += [np.arange(b + 32, b + 64), np.arange(b, b + 32)]
        return np.concatenate(out)
    for base in (o_rq, o_rk):
        for c in range(2):
            colsF.append(np.arange(base + 128 * c, base + 128 * c + 128))
        for c in range(2):
            colsF.append(swap(base, c))
    colsF = np.concatenate(colsF)
    colsT = np.concatenate([np.arange(o_dv, o_dv + 384), np.arange(o_ckv, o_ckv + 128),
                            np.arange(o_iw, o_iw + 8), np.arange(o_rv, o_rv + 256),
                            np.arange(o_gate, o_gate + 1024)])
    return colsF, colsT


NF = 22
T_DV = (0, 384)
T_CKV = (384, 520)
T_RV = (520, 776)
T_GATE = (776, 1800)
NT = 1800


def _host_consts():
    f32 = np.float32
    c = {}
    c["ident"] = np.eye(128, dtype=f32)
    r = np.arange(128)
    c["tri"] = (r[None, :] >= r[:, None]).astype(f32)
    c["cneg"] = np.where(r[None, :] <= r[:, None], 0.0, -1e30).astype(f32)
    inv_freq = (f32(10000.0) ** (-(np.arange(32, dtype=f32)) / f32(32))).astype(f32)
    ang = (np.arange(S, dtype=f32)[:, None] * inv_freq[None, :]).astype(f32)
    cos, sin = np.cos(ang).astype(f32), np.sin(ang).astype(f32)
    rr = np.arange(128)
    cosT = cos[:, rr % 32].T.copy()
    sgn = np.where((rr % 64) < 32, -1.0, 1.0).astype(f32)
    sinT = (sin[:, rr % 32].T * sgn[:, None]).astype(f32)
    c["cosT"], c["sinT"] = np.ascontiguousarray(cosT), np.ascontiguousarray(sinT)
    H = 4
    log_g = np.log(f32(1.0) - f32(2.0) ** (f32(-5.0) - np.arange(H, dtype=f32))).astype(f32)
    pos = np.arange(128, dtype=f32)
    diff = pos[:, None] - pos[None, :]
    d_intra = np.where(diff >= 0, np.exp(np.maximum(diff, 0.0)[None] * log_g[:, None, None]), 0.0).astype(f32)
    xi = np.exp((pos + 1.0)[:, None] * log_g[None, :]).astype(f32)
    zeta = np.exp((127.0 - pos)[:, None] * log_g[None, :]).astype(f32)
    gch = np.exp(128.0 * log_g).astype(f32)
    sc = f32(64.0 ** -0.5)
    c["dintraT"] = np.ascontiguousarray(np.transpose(d_intra, (2, 0, 1)) * sc).astype(f32)
    xiT = np.zeros((128, 2, 128), f32)
    zt = np.zeros((128, 2, 128), f32)
    gc = np.zeros((128, 2), f32)
    for cc in range(2):
        for hl in range(2):
            h = 2 * cc + hl
            xiT[hl * 64:(hl + 1) * 64, cc, :] = xi[None, :, h]
            zt[:, cc, hl * 64:(hl + 1) * 64] = (zeta[:, h] * sc)[:, None]
            gc[hl * 64:(hl + 1) * 64, cc] = gch[h]
    c["xiT"], c["zeta"], c["gch"] = xiT, zt, gc
    return c


_CONST_SHAPES = {"ident": [128, 128], "tri": [128, 128], "cneg": [128, 128], "cosT": [128, S], "sinT": [128, S],
                 "dintraT": [128, 4, 128], "xiT": [128, 2, 128], "zeta": [128, 2, 128], "gch": [128, 2]}


def _host_weights(attn_norm, w_in, diff_lambda, diff_norm, kv_norm, w_uk, w_uv, ret_norm, w_out, final_norm):
    L = w_in.shape[0]
    colsF, colsT = _col_maps()
    w = {}
    wf = w_in[:, :, colsF].reshape(L, 8, 128, NF, 128)
    w["wF"] = np.ascontiguousarray(np.transpose(wf, (0, 3, 2, 1, 4)))
    wt = w_in[:, :, colsT].reshape(L, 8, 128, NT)
    w["wT"] = np.ascontiguousarray(np.transpose(wt, (0, 2, 1, 3)))
    w["wout"] = np.ascontiguousarray(np.transpose(w_out.reshape(L, 8, 128, D), (0, 2, 1, 3)))
    uk = w_uk.reshape(L, 3, 2, 64, 128)
    w["wuk"] = np.ascontiguousarray(np.transpose(uk, (0, 2, 3, 1, 4)).reshape(L, 128, 3, 128))
    w["wuv"] = np.ascontiguousarray(np.transpose(w_uv, (0, 2, 1, 3)).reshape(L, 128, 384))
    w["gattn"] = np.ascontiguousarray(np.transpose(attn_norm.reshape(L, 8, 128), (0, 2, 1)))
    w["gkv"] = np.ascontiguousarray(kv_norm.reshape(L, 128, 1))
    w["gdiff"] = np.ascontiguousarray(diff_norm)
    w["gret"] = np.ascontiguousarray(ret_norm)
    w["gfin"] = np.ascontiguousarray(final_norm)
    w["dlam"] = np.ascontiguousarray(diff_lambda.reshape(L, 128))
    return w


def build_program(NL=DEPTH, NS=2, debug=False):
    nc = bass.Bass("TRN2", target_bir_lowering=False)
    L = NL
    dt = lambda name, shape, dtype=F32, kind="ExternalInput": nc.dram_tensor(name, shape, dtype, kind=kind).ap()
    x_d = dt("x", [NS, S, D])
    wF_d = dt("wF", [L, NF, 128, 8, 128])
    wT_d = dt("wT", [L, 128, 8, NT])
    wout_d = dt("wout", [L, 128, 8, D])
    wuk_d = dt("wuk", [L, 128, 3, 128])
    wuv_d = dt("wuv", [L, 128, 384])
    gattn_d = dt("gattn", [L, 128, 8])
    gkv_d = dt("gkv", [L, 128, 1])
    gdiff_d = dt("gdiff", [L, 64])
    gret_d = dt("gret", [L, 64])
    gfin_d = dt("gfin", [D])
    dlam_d = dt("dlam", [L, 128])
    cd = {k: dt("c_" + k, shp) for k, shp in _CONST_SHAPES.items()}
    out_d = dt("out", [NS, S, D], F32, "ExternalOutput")
    hbuf_d = dt("hbuf", [NS, S, D], F32, "Internal")
    mixed_d = dt("mixed", [NS, S, D], BF16, "Internal")
    dbg_d = dt("dbg", [NS, S, D], F32, "ExternalOutput") if debug else None

    with ExitStack() as st:
        P = Prog(nc, st)
        _uid = [0]

        def sbuf(stack, name, shape, dtype=F32):
            _uid[0] += 1
            return stack.enter_context(nc.sbuf_tensor("s%d_%s" % (_uid[0], name), shape, dtype))
        ps = [st.enter_context(nc.psum_tensor("ps%d" % i, [128, 512], F32)) for i in range(8)]
        pk = lambda b: ("ps", b)

        def mm(out, lhsT, rhs, start, stop, reads, writes, **kw):
            P.op("pe", lambda e: e.matmul(out, lhsT=lhsT, rhs=rhs, start=start, stop=stop,
                                          skip_group_check=True, **kw), reads, writes)

        def tr(out, in_, reads, writes):
            P.op("pe", lambda e: e.transpose(out, in_, ident_b[:, :]), list(reads) + ["ident_b"], writes)

        def act(out, in_, func, reads, writes, **kw):
            P.op("act", lambda e: e.activation(out=out, in_=in_, func=func, **kw), reads, writes)

        def ts(eng, out, in0, s1, s2, op0, op1, reads, writes, **kw):
            if op1 is None:
                P.op(eng, lambda e: e.tensor_scalar(out, in0, s1, None, op0=op0, **kw), reads, writes)
            else:
                P.op(eng, lambda e: e.tensor_scalar(out, in0, s1, s2, op0=op0, op1=op1, **kw), reads, writes)

        def tt(eng, out, in0, in1, op, reads, writes):
            P.op(eng, lambda e: e.tensor_tensor(out, in0, in1, op=op), reads, writes)

        def stt(eng, out, in0, scalar, in1, op0, op1, reads, writes):
            P.op(eng, lambda e: e.scalar_tensor_tensor(out, in0, scalar, in1, op0=op0, op1=op1), reads, writes)

        def cp(eng, out, in_, reads, writes):
            if eng == "act":
                P.op("act", lambda e: e.copy(out, in_), reads, writes)
            else:
                P.op(eng, lambda e: e.tensor_copy(out, in_), reads, writes)

        def recip(out, in_, reads, writes):
            P.op("dve", lambda e: e.reciprocal(out, in_), reads, writes)

        def reduce(out, in_, op, reads, writes):
            P.op("dve", lambda e: e.tensor_reduce(out, in_, axis=AX.X, op=op), reads, writes)

        def memset(eng, ap, val, writes):
            P.op(eng, lambda e: e.memset(ap, val), [], writes)

        def dma(queue, out, in_, reads, writes):
            return P.dma(queue, lambda e: e.dma_start(out=out, in_=in_), reads, writes)

        def rsqrt_mean(eng_unused, out, ss, n, key_out, key_ss):
            ts("dve", out, ss, 1.0 / n, EPS, ALU.mult, ALU.add, [key_ss], [key_out])
            act(out, out, AF.Sqrt, [key_out], [key_out])
            P.op("dve", lambda e: e.reciprocal(out, out), [key_out], [key_out])

        ident_b = sbuf(st, "ident_b", [128, 128], BF16)
        tri_b = sbuf(st, "tri_b", [128, 128], BF16)
        cneg = sbuf(st, "cneg", [128, 128])
        dintraT = sbuf(st, "dintraT", [128, 4, 128])
        xiT = sbuf(st, "xiT", [128, 2, 128])
        zeta = sbuf(st, "zeta", [128, 2, 128])
        gch = sbuf(st, "gch", [128, 2])
        gattn = sbuf(st, "gattn", [128, L, 8])
        gkv = sbuf(st, "gkv", [128, L, 1])
        gdm = sbuf(st, "gdm", [128, L, 64])
        gret = sbuf(st, "gret", [128, L, 64])
        lamneg = sbuf(st, "lamneg", [128, L, 1])
        lamt = sbuf(st, "lamt", [128, 4])
        uT = sbuf(st, "uT", [128, 8, S], BF16)
        tmp0 = ExitStack()
        dlam = sbuf(tmp0, "dlam", [128, L, 128])

        dma("pool", ident_b[:, :], cd["ident"][:, :], [], ["ident_b"])
        dma("pool", tri_b[:, :], cd["tri"][:, :], [], ["tri_b"])
        for nm, t in (("cneg", cneg), ("gch", gch)):
            dma("sp", t[:, :], cd[nm][:, :], [], [nm])
        for nm, t in (("dintraT", dintraT), ("xiT", xiT), ("zeta", zeta)):
            dma("sp", t[:, :, :], cd[nm][:, :, :], [], [nm])
        for l in range(L):
            dma("sp", gattn[:, l, :], gattn_d[l, :, :], [], ["gattn"])
            dma("sp", gkv[:, l, :], gkv_d[l, :, :], [], ["gkv"])
            dma("sp", gdm[:, l, :], gdiff_d[l, :].partition_broadcast(128), [], ["gdm"])
            dma("sp", gret[:, l, :], gret_d[l, :].partition_broadcast(128), [], ["gret"])
            dma("sp", dlam[:, l, :], dlam_d[l, :].partition_broadcast(128), [], ["dlam"])
        for l in range(L):
            lam_init = 0.8 - 0.6 * math.exp(-0.3 * l)
            junk = dlam[:, l, 0:32]
            tt("dve", dlam[:, l, 0:32], dlam[:, l, 0:32], dlam[:, l, 32:64], ALU.mult, ["dlam"], ["dlam"])
            tt("dve", dlam[:, l, 64:96], dlam[:, l, 64:96], dlam[:, l, 96:128], ALU.mult, ["dlam"], ["dlam"])
            reduce(lamt[:, 0:1], dlam[:, l, 0:32], ALU.add, ["dlam"], ["lamt"])
            reduce(lamt[:, 1:2], dlam[:, l, 64:96], ALU.add, ["dlam", "lamt"], ["lamt"])
            act(lamt[:, 2:4], lamt[:, 0:2], AF.Exp, ["lamt"], ["lamt"])
            tt("dve", lamt[:, 0:1], lamt[:, 3:4], lamt[:, 2:3], ALU.subtract, ["lamt"], ["lamt"])
            ts("dve", lamneg[:, l, :], lamt[:, 0:1], -lam_init, None, ALU.add, None, ["lamt"], ["lamneg"])
            ts("dve", gdm[:, l, :], gdm[:, l, :], 1.0 - lam_init, None, ALU.mult, None, ["gdm"], ["gdm"])

        P.barrier()
        tmp0.close()
        wrotF = Rot(range(3))
        wrotT = Rot(range(2))
        evrot = Rot(["act", "dve"])

        def h_src(l, s, t):
            src = x_d if l == 0 else hbuf_d
            return src[s, t * 128:(t + 1) * 128, :]

        def hkey(s, t):
            return ("h", s, t)

        for l in range(L):
            last_layer = (l == L - 1)
            for s in range(NS):
                with ExitStack() as ph:
                    hb = [sbuf(ph, "p0_h%d" % i, [128, D]) for i in range(4)]
                    hn = [sbuf(ph, "p0_hn%d" % i, [128, D], BF16) for i in range(2)]
                    sq_junk = sbuf(ph, "p0_junk", [128, D], BF16)
                    st0 = [sbuf(ph, "p0_st%d" % i, [128, 2]) for i in range(4)]
                    brot = Rot(range(8))
                    gat3 = sbuf(ph, "p0_g3", [128, 8, 1])
                    cp("dve", gat3[:, :, :].rearrange("p k o -> p (k o)"), gattn[:, l, :], ["gattn"], ["gat3"])

                    def p0_s1a(t):
                        i = t % 4
                        dma("sp", hb[i][:, :], h_src(l, s, t), [hkey(s, t)], [("hb", i)])
                        act(sq_junk[:, :], hb[i][:, :], AF.Square, [("hb", i)], ["sqj", ("st0", i)], accum_out=st0[i][:, 0:1])

                    def p0_s1b(t):
                        i = t % 4
                        ts("dve", st0[i][:, 1:2], st0[i][:, 0:1], 1.0 / D, EPS, ALU.mult, ALU.add, [("st0", i)], [("st0", i)])
                        act(st0[i][:, 1:2], st0[i][:, 1:2], AF.Sqrt, [("st0", i)], [("st0", i)])

                    def p0_s1c(t):
                        i = t % 4
                        recip(st0[i][:, 1:2], st0[i][:, 1:2], [("st0", i)], [("st0", i)])
                        act(hn[t % 2][:, :], hb[i][:, :], AF.Copy, [("hb", i), ("st0", i)], [("hn", t % 2)], scale=st0[i][:, 1:2])

                    def p0_stage2(t):
                        i = t % 2
                        b = brot.next()
                        pv = ps[b][:, :].bitcast(BF16)
                        for k in range(8):
                            tr(pv[:, k * 128:(k + 1) * 128], hn[i][:, k * 128:(k + 1) * 128], [("hn", i)], [pk(b)])
                        tt("dve", uT[:, :, t * 128:(t + 1) * 128], pv[:, :].rearrange("p (k q) -> p k q", q=128),
                           gat3[:, :, :].to_broadcast([128, 8, 128]), ALU.mult, [pk(b), "gat3"], ["uT"])
                    for t in range(NB + 3):
                        if t < NB:
                            p0_s1a(t)
                        if 1 <= t <= NB:
                            p0_s1b(t - 1)
                        if 2 <= t <= NB + 1:
                            p0_s1c(t - 2)
                        if t >= 3:
                            p0_stage2(t - 3)
                P.barrier()

                def load_wF(wtiles, f):
                    i = wrotF.next()
                    dma("pool", wtiles[i][:, :, :], wF_d[l, f, :, :, :], [], [("wF", i)])
                    return i

                def proj_F(wtiles, f, evac):
                    i = load_wF(wtiles, f)
                    for tg in range(4):
                        b = brotP.next()
                        for k in range(8):
                            mm(ps[b][:, :], wtiles[i][:, k, :], uT[:, k, tg * 512:(tg + 1) * 512], k == 0, k == 7,
                               [("wF", i), "uT"], [pk(b)])
                        evac(tg, ps[b][:, :], b)

                def proj_T(wt, wkey, ncols, t, c0=0):
                    b = brotP.next()
                    for k in range(8):
                        mm(ps[b][:, 0:ncols], uT[:, k, t * 128:(t + 1) * 128], wt[:, k, c0:c0 + ncols], k == 0, k == 7,
                           [wkey, "uT"], [pk(b)])
                    return b

                def evac_copy(dst3, c):
                    def f(tg, pa, b):
                        ev = evrot.next()
                        cp(ev, dst3[:, c, tg * 512:(tg + 1) * 512], pa, [pk(b)], [dst3.name if hasattr(dst3, "name") else "x"])
                    return f

                with ExitStack() as ph:
                  if "A" in _SKIP:
                    pass
                  else:
                      brotP = Rot(range(8))
                      wFt = [sbuf(ph, "a_wF%d" % i, [128, 8, 128], BF16) for i in range(3)]
                      wTt = sbuf(ph, "a_wT", [128, 8, 384], BF16)
                      dqT = sbuf(ph, "a_dqT", [128, 3, S], BF16)
                      dkT = sbuf(ph, "a_dkT", [128, 3, S], BF16)
                      dva = sbuf(ph, "a_dva", [128, NB, 6, 65], BF16)
                      pt = [sbuf(ph, "a_pt%d" % i, [128, 512], BF16) for i in range(8)]
                      rr = [sbuf(ph, "a_rr%d" % i, [128, 4, 1]) for i in range(3)]
                      tA = sbuf(ph, "a_tA", [128, 4, 64])
                      tB = sbuf(ph, "a_tB", [128, 4, 64])
                      tO = sbuf(ph, "a_tO", [128, 4, 64])
                      tS = sbuf(ph, "a_tS", [128, 4, 64])
                      ss4 = sbuf(ph, "a_ss4", [128, 4, 1])
                      oa = [sbuf(ph, "a_oa%d" % i, [128, 4, 384], BF16) for i in range(2)]

                      dma("pool", wTt[:, :, :], wT_d[l, :, :, T_DV[0]:T_DV[1]], [], ["a_wT"])
                      memset("pool", dva[:, :, :, 64:65], 1.0, ["dva"])
                      for c in range(3):
                          def ev_q(tg, pa, b, c=c):
                              cp(evrot.next(), dqT[:, c, tg * 512:(tg + 1) * 512], pa, [pk(b)], ["dqT"])
                          proj_F(wFt, c, ev_q)
                      for c in range(3):
                          def ev_k(tg, pa, b, c=c):
                              cp(evrot.next(), dkT[:, c, tg * 512:(tg + 1) * 512], pa, [pk(b)], ["dkT"])
                          proj_F(wFt, 3 + c, ev_k)
                      for t in range(NB):
                          b = proj_T(wTt, "a_wT", 384, t)
                          cp(evrot.next(), dva[:, t, :, 0:64], ps[b][:, 0:384].rearrange("p (h e) -> p h e", e=64), [pk(b)], ["dva"])

                      scrot = Rot([0, 1, 2, 3])
                      accrot = Rot([(4, 5), (6, 7)])
                      ptrot = Rot(range(8))
                      scale = 32.0 ** -0.5

                      def a_epilogue(I, h, banks):
                          oab = oa[I % 2]
                          oak = ("oa", I % 2)
                          a0 = ps[banks[0]][:, 0:260].rearrange("p (i e) -> p i e", e=65)
                          a1 = ps[banks[1]][:, 0:260].rearrange("p (i e) -> p i e", e=65)
                          recip(rr[0][:, :, :], a0[:, :, 64:65], [pk(banks[0])], ["rr0"])
                          recip(rr[1][:, :, :], a1[:, :, 64:65], [pk(banks[1])], ["rr1"])
                          ts("dve", rr[2][:, :, :], rr[1][:, :, :], lamneg[:, l, :], None, ALU.mult, None, ["rr1", "lamneg"], ["rr2"])
                          tt("dve", tA[:, :, :], a0[:, :, 0:64], rr[0][:, :, :].to_broadcast([128, 4, 64]), ALU.mult, [pk(banks[0]), "rr0"], ["tA"])
                          tt("dve", tB[:, :, :], a1[:, :, 0:64], rr[2][:, :, :].to_broadcast([128, 4, 64]), ALU.mult, [pk(banks[1]), "rr2"], ["tB"])
                          tt("pool", tO[:, :, :], tA[:, :, :], tB[:, :, :], ALU.add, ["tA", "tB"], ["tO"])
                          tt("pool", tS[:, :, :], tO[:, :, :], tO[:, :, :], ALU.mult, ["tO"], ["tS"])
                          reduce(ss4[:, :, :], tS[:, :, :], ALU.add, ["tS"], ["ss4"])
                          rsqrt_mean("dve", ss4[:, :, :], ss4[:, :, :], 64, "ss4", "ss4")
                          tt("pool", tO[:, :, :], tO[:, :, :], ss4[:, :, :].to_broadcast([128, 4, 64]), ALU.mult, ["tO", "ss4"], ["tO"])
                          tt("pool", oab[:, :, h * 64:(h + 1) * 64], tO[:, :, :], gdm[:, l:l + 1, :].to_broadcast([128, 4, 64]), ALU.mult, ["tO", "gdm"], [oak])
                          if h == 5:
                              for i4 in range(4):
                                  t = 4 * I + i4
                                  dma("sp", mixed_d[s, t * 128:(t + 1) * 128, 0:384], oab[:, i4, :], [oak], [("mx", s, t, 0)])

                      tiles = []
                      for I in range(4):
                          for h in range(6):
                              banks = accrot.next()
                              nj = 4 * I + 4
                              for j in range(nj):
                                  for c2 in range(2):
                                      tiles.append(dict(I=I, h=h, c2=c2, j=j, ab=banks[c2], banks=banks, first=(j == 0),
                                                        last=(c2 == 1 and j == nj - 1)))

                      def a_score(T):
                          I, h, c2, j = T["I"], T["h"], T["c2"], T["j"]
                          r0 = (h % 2) * 64 + c2 * 32
                          kw = dict(tile_position=(96, 0)) if r0 == 96 else {}
                          i0 = max(j, 4 * I)
                          w = (4 * I + 4 - i0) * 128
                          sb_ = scrot.next()
                          pi = ptrot.next()
                          T.update(i0=i0, w=w, pi=pi)
                          mm(ps[sb_][:, 0:w], dkT[r0:r0 + 32, h // 2, j * 128:(j + 1) * 128],
                             dqT[r0:r0 + 32, h // 2, i0 * 128:(4 * I + 4) * 128], True, True,
                             ["dkT", "dqT"], [pk(sb_)], **kw)
                          act(pt[pi][:, 0:w], ps[sb_][:, 0:w], AF.Exp, [pk(sb_)], [("pt", pi)], scale=scale)
                          if j >= 4 * I:
                              tt("pool", pt[pi][:, 0:128], pt[pi][:, 0:128], tri_b[:, :], ALU.mult, [("pt", pi), "tri_b"], [("pt", pi)])

                      def a_av(T):
                          I, h, j, i0, pi, ab = T["I"], T["h"], T["j"], T["i0"], T["pi"], T["ab"]
                          for i in range(i0, 4 * I + 4):
                              mm(ps[ab][:, (i - 4 * I) * 65:(i - 4 * I) * 65 + 65], pt[pi][:, (i - i0) * 128:(i - i0 + 1) * 128],
                                 dva[:, j, h, :], T["first"] and i == i0, j == i, [("pt", pi), "dva"], [pk(ab)])
                          if T["last"]:
                              a_epilogue(I, h, T["banks"])

                      DEP = 4
                      for idx in range(len(tiles) + DEP):
                          if idx < len(tiles):
                              a_score(tiles[idx])
                          if idx >= DEP:
                              a_av(tiles[idx - DEP])
                P.barrier()

                with ExitStack() as ph:
                  if "B" in _SKIP:
                    pass
                  else:
                      brotP = Rot(range(8))
                      qlT = sbuf(ph, "b_qlT", [128, 6, S], BF16)
                      ckvT = sbuf(ph, "b_ckvT", [128, S], BF16)
                      vpa = sbuf(ph, "b_vpa", [128, NB, 6, 65], BF16)
                      iqT = sbuf(ph, "b_iqT", [128, 4, S], BF16)
                      ikT = sbuf(ph, "b_ikT", [128, S], BF16)
                      iw3 = sbuf(ph, "b_iw", [128, NB, 8, 1])
                      iw = iw3[:, :, :, :].rearrange("p t h o -> p t (h o)")
                      ident3 = sbuf(ph, "b_ident3", [128, 1, 128], BF16)
                      cp("pool", ident3[:, 0, :], ident_b[:, :], ["ident_b"], ["ident3"])
                      ph2 = ExitStack()
                      wFt = [sbuf(ph2, "b_wF%d" % i, [128, 8, 128], BF16) for i in range(3)]
                      wTt = sbuf(ph2, "b_wT", [128, 8, 256], BF16)
                      sqt = [sbuf(ph2, "b_sqt%d" % i, [128, 512], BF16) for i in range(2)]
                      ckn = [sbuf(ph2, "b_ckn%d" % i, [128, 128], BF16) for i in range(3)]
                      cst = [sbuf(ph2, "b_cst%d" % i, [128, 2]) for i in range(3)]
                      cjunk = sbuf(ph2, "b_cjunk", [128, 128], BF16)
                      wuk_b = sbuf(ph2, "b_wuk", [128, 1, 3, 128], BF16)
                      wuv_b = sbuf(ph2, "b_wuv", [128, 1, 384], BF16)

                      dma("pool", wTt[:, :, :], wT_d[l, :, :, T_CKV[0]:T_CKV[0] + 256], [], ["b_wT"])
                      dma("pool", wuk_b[:, 0, :, :], wuk_d[l, :, :, :], [], ["wuk_b"])
                      dma("pool", wuv_b[:, 0, :], wuv_d[l, :, :], [], ["wuv_b"])
                      memset("pool", vpa[:, :, :, 64:65], 1.0, ["vpa"])
                      sqrot = Rot(range(2))
                      for c in range(3):
                          def ev_sq(tg, pa, b, c=c):
                              si = sqrot.next()
                              cp(evrot.next(), sqt[si][:, :], pa, [pk(b)], [("sqt", si)])
                              for hl in range(2):
                                  b2 = brotP.next()
                                  mm(ps[b2][:, :], wuk_b[hl * 64:(hl + 1) * 64, 0, c, :], sqt[si][hl * 64:(hl + 1) * 64, :], True, True,
                                     ["wuk_b", ("sqt", si)], [pk(b2)])
                                  cp(evrot.next(), qlT[:, 2 * c + hl, tg * 512:(tg + 1) * 512], ps[b2][:, :], [pk(b2)], ["qlT"])
                          if '1' not in _SKIP:
                              proj_F(wFt, 6 + c, ev_sq)
                      for c in range(4):
                          def ev_iq(tg, pa, b, c=c):
                              cp(evrot.next(), iqT[:, c, tg * 512:(tg + 1) * 512], pa, [pk(b)], ["iqT"])
                          if '2' not in _SKIP:
                              proj_F(wFt, 9 + c, ev_iq)

                      def ev_ik(tg, pa, b):
                          cp(evrot.next(), ikT[:, tg * 512:(tg + 1) * 512], pa, [pk(b)], ["ikT"])
                      if '3' not in _SKIP:
                          proj_F(wFt, 13, ev_ik)
                      def ckv_s1(t):
                          i = t % 3
                          b = proj_T(wTt, "b_wT", 136, t)
                          cp("dve", iw[:, t, :], ps[b][:, 128:136], [pk(b)], ["iw"])
                          act(cjunk[:, :], ps[b][:, 0:128], AF.Square, [pk(b)], ["cjunk", ("cst", i)], accum_out=cst[i][:, 0:1])
                          rsqrt_mean("dve", cst[i][:, 1:2], cst[i][:, 0:1], 128, ("cst", i), ("cst", i))
                          ts("dve", ckn[i][:, :], ps[b][:, 0:128], cst[i][:, 1:2], None, ALU.mult, None, [pk(b), ("cst", i)], [("ckn", i)])

                      def ckv_s2(t):
                          i = t % 3
                          b2 = brotP.next()
                          pv = ps[b2][:, :].bitcast(BF16)
                          tr(pv[:, 0:128], ckn[i][:, :], [("ckn", i)], [pk(b2)])
                          act(ckvT[:, t * 128:(t + 1) * 128], pv[:, 0:128], AF.Copy, [pk(b2), "gkv"], [("ckvT", t), "ckvT"], scale=gkv[:, l, :])

                      def ckv_s3(t):
                          b3 = brotP.next()
                          mm(ps[b3][:, 0:384], ckvT[:, t * 128:(t + 1) * 128], wuv_b[:, 0, :], True, True, [("ckvT", t), "wuv_b"], [pk(b3)])
                          cp(evrot.next(), vpa[:, t, :, 0:64], ps[b3][:, 0:384].rearrange("p (h e) -> p h e", e=64), [pk(b3)], ["vpa"])

                      for t in range(NB + 2):
                          if t < NB:
                              ckv_s1(t)
                          if 1 <= t <= NB:
                              ckv_s2(t - 1)
                          if t >= 2:
                              ckv_s3(t - 2)

                      P.barrier()
                      ph2.close()
                      SCW = [(8 + i + 1) * 128 for i in range(4)] + [(12 + i + 1) * 128 for i in range(4)]
                      score = [sbuf(ph, "b_score%d" % i, [128, SCW[i]]) for i in range(8)]
                      negm = [sbuf(ph, "b_negm%d" % i, [128, SCW[i]], BF16) for i in range(8)]
                      rel = [sbuf(ph, "b_rel%d" % i, [128, 512], BF16) for i in range(6)]
                      dg = [sbuf(ph, "b_dg%d" % i, [128, 8, 128], BF16) for i in range(4)]
                      bis = [sbuf(ph, "b_bis%d" % i, [128, 8]) for i in range(8)]
                      bjunk = sbuf(ph, "b_bjunk", [128, S], BF16)
                      pt = [sbuf(ph, "b_pt%d" % i, [128, 512], BF16) for i in range(4)]
                      acs = [sbuf(ph, "b_acs%d" % i, [128, 4, 65]) for i in range(2)]
                      acsrot = Rot(range(2))
                      rrp = [sbuf(ph, "b_rrp%d" % i, [128, 4, 1]) for i in range(2)]
                      negone = sbuf(ph, "b_negone", [128, 4, 1])
                      memset("pool", negone[:, :, :], -1.0, ["negone"])
                      ob = [sbuf(ph, "b_ob%d" % i, [128, 4, 384], BF16) for i in range(2)]
                      relbank = Rot([0, 1, 2, 3])
                      idxacc = Rot([4])
                      relrot = Rot(range(6))
                      scrot = Rot([5, 6])
                      accrot = Rot([7])
                      ptrot = Rot(range(4))
                      scale = 64.0 ** -0.5

                      QORD = [1, 3, 2, 0]
                      QPAR = {1: 0, 3: 1, 2: 0, 0: 1}

                      def index_phase(I):
                          par = QPAR[I]
                          chains = []
                          for i4 in range(4):
                              i = 4 * I + i4
                              if i >= 2:
                                  tt("pool", dg[i4][:, :, :], ident3[:, :, :].to_broadcast([128, 8, 128]), iw3[:, i, :, :].to_broadcast([128, 8, 128]),
                                     ALU.mult, ["ident3", "iw"], [("dg", i4)])
                          for i4 in range(4):
                              i = 4 * I + i4
                              n = (i + 1) * 128
                              si_ = par * 4 + i4
                              sk = ("score", si_)
                              bk = ("bis", si_)
                              bs = bis[si_]
                              if i < 2 or 'i' in _SKIP:
                                  memset("dve", score[si_][:, 0:n], 0.0, [sk])
                                  tt("dve", score[si_][:, n - 128:n], score[si_][:, n - 128:n], cneg[:, :], ALU.add, [sk, "cneg"], [sk])
                                  memset("dve", bs[:, 0:1], -1e29, [bk])
                              else:
                                  dgi = i4
                                  for kc in range((n + 511) // 512):
                                      w = min(512, n - kc * 512)
                                      ab = idxacc.next()
                                      pend = []
                                      for hp in range(5):
                                          cur = []
                                          if hp < 4:
                                              for hl in range(2):
                                                  hh = 2 * hp + hl
                                                  rb = relbank.next()
                                                  r0 = hl * 64
                                                  mm(ps[rb][:, 0:w], iqT[r0:r0 + 64, hp, i * 128:(i + 1) * 128], ikT[r0:r0 + 64, kc * 512:kc * 512 + w],
                                                     True, True, ["iqT", "ikT"], [pk(rb)])
                                                  ri = relrot.next()
                                                  act(rel[ri][:, 0:w], ps[rb][:, 0:w], AF.Relu, [pk(rb)], [("rel", ri)])
                                                  cur.append((hh, ri))
                                          for (ph_, pri) in pend:
                                              mm(ps[ab][:, 0:w], dg[dgi][:, ph_, :], rel[pri][:, 0:w], ph_ == 0, ph_ == 7, [("dg", dgi), ("rel", pri)], [pk(ab)])
                                          pend = cur
                                      cp("act", score[si_][:, kc * 512:kc * 512 + w], ps[ab][:, 0:w], [pk(ab)], [sk])
                                  tt("pool", score[si_][:, n - 128:n], score[si_][:, n - 128:n], cneg[:, :], ALU.add, [sk, "cneg"], [sk])
                                  reduce(bs[:, 5:6], score[si_][:, 0:n], ALU.max, [sk], [bk])
                                  reduce(bs[:, 0:1], score[si_][:, 0:256], ALU.min, [sk, bk], [bk])
                                  tt("dve", bs[:, 1:2], bs[:, 5:6], bs[:, 0:1], ALU.subtract, [bk], [bk])
                                  chains.append((si_, n, sk, bk, bs))
                          for it in range(NBIS):
                              for (si_, n, sk, bk, bs) in chains:
                                  ts("dve", bs[:, 2:3], bs[:, 1:2], 0.5 ** (it + 1), bs[:, 0:1], ALU.mult, ALU.add, [bk], [bk])
                              for (si_, n, sk, bk, bs) in chains:
                                  ts("dve", bjunk[:, 0:n], score[si_][:, 0:n], bs[:, 2:3], 0.0, ALU.is_ge, ALU.add, [sk, bk], ["bjunk", bk], accum_out=bs[:, 3:4])
                              for (si_, n, sk, bk, bs) in chains:
                                  ts("dve", bs[:, 4:5], bs[:, 3:4], 255.5, 1e30, ALU.is_lt, ALU.mult, [bk], [bk])
                              for (si_, n, sk, bk, bs) in chains:
                                  stt("dve", bs[:, 0:1], bs[:, 2:3], bs[:, 4:5], bs[:, 0:1], ALU.subtract, ALU.max, [bk], [bk])
                          for i4 in range(4):
                              n = (4 * I + i4 + 1) * 128
                              si_ = par * 4 + i4
                              ts("dve", negm[si_][:, 0:n], score[si_][:, 0:n], bis[si_][:, 0:1], NEGM, ALU.is_lt, ALU.mult,
                                 [("score", si_), ("bis", si_)], [("negm", si_)])

                      def d_epilogue(I, h, ab):
                          obb = ob[I % 2]
                          obk = ("ob", I % 2)
                          a0 = ps[ab][:, 0:260].rearrange("p (i e) -> p i e", e=65)
                          ai = acsrot.next()
                          cp("act", acs[ai][:, :, :], a0, [pk(ab)], [("acs", ai)])
                          tt("pool", rrp[ai][:, :, :], acs[ai][:, :, 64:65], negone[:, :, :], ALU.pow, [("acs", ai), "negone"], [("rrp", ai)])
                          tt("pool", obb[:, :, h * 64:(h + 1) * 64], acs[ai][:, :, 0:64], rrp[ai][:, :, :].to_broadcast([128, 4, 64]), ALU.mult,
                             [("acs", ai), ("rrp", ai)], [obk])
                          if h == 5:
                              for i4 in range(4):
                                  t = 4 * I + i4
                                  dma("sp", mixed_d[s, t * 128:(t + 1) * 128, 384:768], obb[:, i4, :], [obk], [("mx", s, t, 1)])

                      def d_score(T):
                          I, h, j = T["I"], T["h"], T["j"]
                          par = QPAR[I]
                          i0 = max(j, 4 * I)
                          w = (4 * I + 4 - i0) * 128
                          sb_ = scrot.next()
                          pi = ptrot.next()
                          T.update(i0=i0, w=w, pi=pi)
                          mm(ps[sb_][:, 0:w], ckvT[:, j * 128:(j + 1) * 128], qlT[:, h, i0 * 128:(4 * I + 4) * 128], True, False,
                             ["ckvT", "qlT"], [pk(sb_)])
                          for i in range(i0, 4 * I + 4):
                              i4 = i - 4 * I
                              mm(ps[sb_][:, (i - i0) * 128:(i - i0 + 1) * 128], negm[par * 4 + i4][:, j * 128:(j + 1) * 128], ident_b[:, :], False,
                                 i == 4 * I + 3, [("negm", par * 4 + i4), "ident_b"], [pk(sb_)])
                          act(pt[pi][:, 0:w], ps[sb_][:, 0:w], AF.Exp, [pk(sb_)], [("pt", pi)], scale=scale)

                      def d_av(T):
                          I, h, j, i0, pi, ab = T["I"], T["h"], T["j"], T["i0"], T["pi"], T["ab"]
                          for i in range(i0, 4 * I + 4):
                              mm(ps[ab][:, (i - 4 * I) * 65:(i - 4 * I) * 65 + 65], pt[pi][:, (i - i0) * 128:(i - i0 + 1) * 128],
                                 vpa[:, j, h, :], T["first"] and i == i0, j == i, [("pt", pi), "vpa"], [pk(ab)])
                          if T["last"]:
                              d_epilogue(I, h, ab)

                      def dsa_phase(I):
                          tiles = []
                          for h in range(6):
                              ab = accrot.next()
                              nj = 4 * I + 4
                              for j in range(nj):
                                  tiles.append(dict(I=I, h=h, j=j, ab=ab, first=(j == 0), last=(j == nj - 1)))
                          DEP = 1
                          for idx in range(len(tiles) + DEP):
                              if idx < len(tiles):
                                  d_score(tiles[idx])
                              if idx >= DEP:
                                  d_av(tiles[idx - DEP])

                      for pos, I in enumerate(QORD):
                          if pos == 0:
                              index_phase(I)
                          if pos + 1 < 4:
                              index_phase(QORD[pos + 1])
                          dsa_phase(I)
                P.barrier()

                with ExitStack() as ph:
                  if "C" in _SKIP:
                    pass
                  else:
                      brotP = Rot(range(8))
                      wFt = [sbuf(ph, "c_wF%d" % i, [128, 8, 128], BF16) for i in range(3)]
                      wTt = sbuf(ph, "c_wT", [128, 8, 256], BF16)
                      rT = [sbuf(ph, "c_rqT", [128, 2, S], BF16), sbuf(ph, "c_rkT", [128, 2, S], BF16)]
                      rv = sbuf(ph, "c_rv", [128, NB, 256], BF16)
                      t1 = sbuf(ph, "c_t1", [128, 2, S])
                      t2 = [sbuf(ph, "c_t2_%d" % i, [128, 512]) for i in range(2)]
                      kz = [sbuf(ph, "c_kz%d" % i, [128, 128], BF16) for i in range(4)]
                      qxi = [sbuf(ph, "c_qxi%d" % i, [128, 128], BF16) for i in range(4)]
                      attD = [sbuf(ph, "c_attD%d" % i, [128, 128], BF16) for i in range(4)]
                      Rf = sbuf(ph, "c_Rf", [128, 2, 64])
                      Rb = sbuf(ph, "c_Rb", [128, 2, 64], BF16)
                      oc = [sbuf(ph, "c_oc%d" % i, [128, 4, 64]) for i in range(3)]
                      ocs = sbuf(ph, "c_ocs", [128, 4, 64])
                      ocb = [sbuf(ph, "c_ocb%d" % i, [128, 4, 64], BF16) for i in range(3)]
                      ss4 = sbuf(ph, "c_ss4", [128, 4, 1])

                      dma("pool", wTt[:, :, :], wT_d[l, :, :, T_RV[0]:T_RV[1]], [], ["c_wT"])
                      cosT = sbuf(ph, "c_cosT", [128, S])
                      sinT = sbuf(ph, "c_sinT", [128, S])
                      dma("sp", cosT[:, :], cd["cosT"][:, :], [], ["cosT"])
                      dma("sp", sinT[:, :], cd["sinT"][:, :], [], ["sinT"])
                      for qk in range(2):
                          for c in range(2):
                              def ev_x(tg, pa, b, c=c, qk=qk):
                                  i = tg % 2
                                  tt("dve", t1[:, c, tg * 512:(tg + 1) * 512], pa, cosT[:, tg * 512:(tg + 1) * 512], ALU.mult, [pk(b), "cosT"], [("t1", tg, c)])
                              proj_F(wFt, 14 + 4 * qk + c, ev_x)
                          for c in range(2):
                              def ev_xs(tg, pa, b, c=c, qk=qk):
                                  i = tg % 2
                                  tt("dve", t2[i][:, :], pa, sinT[:, tg * 512:(tg + 1) * 512], ALU.mult, [pk(b), "sinT"], [("t2", i)])
                                  tt("pool", rT[qk][:, c, tg * 512:(tg + 1) * 512], t1[:, c, tg * 512:(tg + 1) * 512], t2[i][:, :], ALU.add, [("t1", tg, c), ("t2", i)], [("rT", qk)])
                              proj_F(wFt, 16 + 4 * qk + c, ev_xs)
                      for t in range(NB):
                          b = proj_T(wTt, "c_wT", 256, t)
                          cp(evrot.next(), rv[:, t, :], ps[b][:, 0:256], [pk(b)], ["rv"])

                      memset("dve", Rf[:, :, :], 0.0, ["Rf"])
                      memset("dve", Rb[:, :, :], 0.0, ["Rb"])
                      brot = Rot([0, 1])
                      adrot = Rot(range(4))
                      obanks = [(4, 5), (2, 3)]
                      rbk = [6, 7]

                      def c_stage1(t):
                          tsl = slice(t * 128, (t + 1) * 128)
                          p2 = t % 2
                          obp = obanks[p2]
                          firsts = [True, True]
                          for c in range(2):
                              b = brot.next()
                              pv = ps[b][:, :].bitcast(BF16)
                              tr(pv[:, 0:128], rT[1][:, c, tsl], [("rT", 1)], [pk(b)])
                              tt("dve", kz[p2 * 2 + c][:, :], pv[:, 0:128], zeta[:, c, :], ALU.mult, [pk(b), "zeta"], [("kz", p2, c)])
                              tt("pool", qxi[p2 * 2 + c][:, :], rT[0][:, c, tsl], xiT[:, c, :], ALU.mult, [("rT", 0), "xiT"], [("qxi", p2, c)])
                              for hl in range(2):
                                  h = 2 * c + hl
                                  rows = slice(hl * 64, (hl + 1) * 64)
                                  b = brot.next()
                                  mm(ps[b][:, 0:128], rT[1][rows, c, tsl], rT[0][rows, c, tsl], True, True, [("rT", 1), ("rT", 0)], [pk(b)])
                                  ai = adrot.next()
                                  tt("dve", attD[ai][:, :], ps[b][:, 0:128], dintraT[:, h, :], ALU.mult, [pk(b), "dintraT"], [("attD", ai)])
                                  mm(ps[obp[hl]][:, c * 64:(c + 1) * 64], attD[ai][:, :], rv[:, t, h * 64:(h + 1) * 64], firsts[hl], t == 0,
                                     [("attD", ai), "rv"], [pk(obp[hl])])
                                  firsts[hl] = False
                              if t < NB - 1:
                                  rb_ = rbk[p2]
                                  mm(ps[rb_][:, c * 128:(c + 1) * 128], kz[p2 * 2 + c][:, :], rv[:, t, c * 128:(c + 1) * 128], True, True, [("kz", p2, c), "rv"], [pk(rb_)])

                      def c_stage2(t):
                          tsl = slice(t * 128, (t + 1) * 128)
                          p2 = t % 2
                          obp = obanks[p2]
                          if t > 0:
                              for hl in range(2):
                                  for c in range(2):
                                      rows = slice(hl * 64, (hl + 1) * 64)
                                      mm(ps[obp[hl]][:, c * 64:(c + 1) * 64], qxi[p2 * 2 + c][rows, :], Rb[rows, c, :], False, True,
                                         [("qxi", p2, c), "Rb"], [pk(obp[hl])])
                          oi = t % 3
                          ocv = oc[oi][:, :, :].rearrange("p (c hl) e -> p c hl e", hl=2)
                          for hl in range(2):
                              cp("act", ocv[:, :, hl, :], ps[obp[hl]][:, 0:128].rearrange("p (c e) -> p c e", e=64), [pk(obp[hl])], [("oc", oi)])
                          if t < NB - 1:
                              for c in range(2):
                                  for hl in range(2):
                                      rows = slice(hl * 64, (hl + 1) * 64)
                                      stt("dve", Rf[rows, c, :], Rf[rows, c, :], gch[rows, c:c + 1], ps[rbk[p2]][rows, c * 128 + hl * 64:c * 128 + (hl + 1) * 64],
                                          ALU.mult, ALU.add, ["Rf", "gch", pk(rbk[p2])], ["Rf"])
                              cp("act", Rb[:, :, :], Rf[:, :, :], ["Rf"], ["Rb"])

                      def c_stage3(t):
                          tsl = slice(t * 128, (t + 1) * 128)
                          oi = t % 3
                          tt("pool", ocs[:, :, :], oc[oi][:, :, :], oc[oi][:, :, :], ALU.mult, [("oc", oi)], ["ocs"])
                          reduce(ss4[:, :, :], ocs[:, :, :], ALU.add, ["ocs"], ["c_ss4"])
                          rsqrt_mean("dve", ss4[:, :, :], ss4[:, :, :], 64, "c_ss4", "c_ss4")
                          tt("pool", oc[oi][:, :, :], oc[oi][:, :, :], ss4[:, :, :].to_broadcast([128, 4, 64]), ALU.mult, [("oc", oi), "c_ss4"], [("oc", oi)])
                          tt("pool", ocb[oi][:, :, :], oc[oi][:, :, :], gret[:, l:l + 1, :].to_broadcast([128, 4, 64]), ALU.mult, [("oc", oi), "gret"], [("ocb", oi)])
                          dma("sp", mixed_d[s, tsl, 768:1024], ocb[oi][:, :, :].rearrange("p h e -> p (h e)"), [("ocb", oi)], [("mx", s, t, 2)])

                      c_stage1(0)
                      for t in range(NB + 1):
                          if t + 1 < NB:
                              c_stage1(t + 1)
                          if t < NB:
                              c_stage2(t)
                          if t >= 1:
                              c_stage3(t - 1)
                P.barrier()

                with ExitStack() as ph:
                  if "D" in _SKIP:
                    pass
                  else:
                      wg = sbuf(ph, "d_wg", [128, 8, D], BF16)
                      wo = sbuf(ph, "d_wo", [128, 8, D], BF16)
                      hb = [sbuf(ph, "d_h%d" % i, [128, D]) for i in range(2)]
                      mx = [sbuf(ph, "d_mx%d" % i, [128, D], BF16) for i in range(2)]
                      sg = [sbuf(ph, "d_sg%d" % i, [128, D], BF16) for i in range(2)]
                      yb = [sbuf(ph, "d_y%d" % i, [128, D], BF16) for i in range(2)]
                      yT = [sbuf(ph, "d_yT%d" % i, [128, 8, 128], BF16) for i in range(2)]
                      hn = [sbuf(ph, "d_hn%d" % i, [128, D]) for i in range(2)]
                      fo = [sbuf(ph, "d_fo%d" % i, [128, D]) for i in range(2)]
                      fjunk = sbuf(ph, "d_fjunk", [128, D], BF16)
                      if last_layer:
                          gfin = sbuf(ph, "d_gfin", [128, D])
                          dma("sp", gfin[:, :], gfin_d.partition_broadcast(128), [], ["gfin"])
                      fst = [sbuf(ph, "d_fst%d" % i, [128, 2]) for i in range(2)]
                      for kh in range(2):
                          dma("pool", wg[:, :, kh * 512:(kh + 1) * 512], wT_d[l, :, :, T_GATE[0] + kh * 512:T_GATE[0] + (kh + 1) * 512], [], ["d_wg"])
                          dma("pool", wo[:, :, kh * 512:(kh + 1) * 512], wout_d[l, :, :, kh * 512:(kh + 1) * 512], [], ["d_wo"])
                      gb = Rot([0, 1, 2, 3])
                      tb = Rot([4, 5])
                      ob2 = Rot([6, 7])
                      hb3 = hb + [sbuf(ph, "d_h2", [128, D])]

                      def d_gate(t):
                          i = t % 2
                          tsl = slice(t * 128, (t + 1) * 128)
                          dma("sp", hb3[t % 3][:, :], h_src(l, s, t), [hkey(s, t)], [("dhb", t % 3)])
                          dma("sp", mx[i][:, :], mixed_d[s, tsl, :], [("mx", s, t, 0), ("mx", s, t, 1), ("mx", s, t, 2)], [("dmx", i)])
                          for kh in range(2):
                              b = gb.next()
                              for k in range(8):
                                  mm(ps[b][:, :], uT[:, k, tsl], wg[:, k, kh * 512:(kh + 1) * 512], k == 0, k == 7, ["uT", "d_wg"], [pk(b)])
                              act(sg[i][:, kh * 512:(kh + 1) * 512], ps[b][:, :], AF.Silu, [pk(b)], [("sg", i)])
                          tt("dve", yb[i][:, :], sg[i][:, :], mx[i][:, :], ALU.mult, [("sg", i), ("dmx", i)], [("yb", i)])

                      def d_tr(t):
                          i = t % 2
                          b = tb.next()
                          pv = ps[b][:, :].bitcast(BF16)
                          for k in range(8):
                              tr(pv[:, k * 128:(k + 1) * 128], yb[i][:, k * 128:(k + 1) * 128], [("yb", i)], [pk(b)])
                          cp("act", yT[i][:, :, :], pv[:, :].rearrange("p (k q) -> p k q", q=128), [pk(b)], [("yT", i)])

                      def d_out(t):
                          i = t % 2
                          tsl = slice(t * 128, (t + 1) * 128)
                          for kh in range(2):
                              b = ob2.next()
                              for k in range(8):
                                  mm(ps[b][:, :], yT[i][:, k, :], wo[:, k, kh * 512:(kh + 1) * 512], k == 0, k == 7, [("yT", i), "d_wo"], [pk(b)])
                              tt("dve", hn[i][:, kh * 512:(kh + 1) * 512], ps[b][:, :], hb3[t % 3][:, kh * 512:(kh + 1) * 512], ALU.add, [pk(b), ("dhb", t % 3)], [("dhn", i)])
                          if not last_layer:
                              dma("sp", hbuf_d[s, tsl, :], hn[i][:, :], [("dhn", i)], [hkey(s, t)])
                          else:
                              act(fjunk[:, :], hn[i][:, :], AF.Square, [("dhn", i)], ["fjunk", ("fst", i)], accum_out=fst[i][:, 0:1])
                              rsqrt_mean("dve", fst[i][:, 1:2], fst[i][:, 0:1], D, ("fst", i), ("fst", i))
                              stt("dve", fo[i][:, :], hn[i][:, :], fst[i][:, 1:2], gfin[:, :], ALU.mult, ALU.mult, [("dhn", i), ("fst", i), "gfin"], [("fo", i)])
                              dma("sp", out_d[s, tsl, :], fo[i][:, :], [("fo", i)], [("out", s, t)])

                      for t in range(NB + 2):
                          if t < NB:
                              d_gate(t)
                          if 1 <= t <= NB:
                              d_tr(t - 1)
                          if t >= 2:
                              d_out(t - 2)
                P.barrier()
        P.barrier()
        P.emit()
    return nc


_PROG_CACHE = {}


def kernel(x, attn_norm, w_in, diff_lambda, diff_norm, kv_norm, w_uk, w_uv, ret_norm, w_out, final_norm):
    x = np.asarray(x, dtype=np.float32)
    args = [np.asarray(a, dtype=np.float32) for a in (attn_norm, w_in, diff_lambda, diff_norm, kv_norm, w_uk, w_uv, ret_norm, w_out, final_norm)]
    w = _host_weights(*args)
    consts = _host_consts()
    B = x.shape[0]
    ns = B // NCORES
    if "nc" not in _PROG_CACHE:
        _PROG_CACHE["nc"] = build_program(DEPTH, ns)
    nc = _PROG_CACHE["nc"]
    in_maps = []
    for c in range(NCORES):
        m = {"x": np.ascontiguousarray(x[c * ns:(c + 1) * ns])}
        m.update(w)
        for k, v in consts.items():
            m["c_" + k] = v
        in_maps.append(m)
    res = run_bass_kernel_spmd(nc, in_maps, core_ids=list(range(NCORES)))
    return np.concatenate([r["out"] for r in res.results], axis=0)
```
